# Optimizing a Trainium2 kernel written in Bass

```python
import math
import jax, jax.numpy as jnp
from jax import lax
import numpy as np

D_MODEL = 1024
BATCH = 4
SEQ = 4096
DEPTH = 1

MEM_LEN = 256

MLA_HEADS = 8
QK_NOPE = 64
QK_ROPE = 32
V_HEAD = 64
Q_LORA = 256
KV_LORA = 256
ROPE_THETA = 10000.0
Q_BLOCK = 128

S5_GROUP_CH = 16
S5_WIDTH = 512
S5_GROUPS = S5_WIDTH // S5_GROUP_CH
S5_STATE = 64
DT_MIN = 1e-3
DT_MAX = 1e-1

XATTN_HEADS = 4
XATTN_HEAD_DIM = 128

N_EXPERTS = 64
TOP_K = 8
N_EXPERT_GROUPS = 8
TOPK_GROUPS = 4
EXPERT_FF = 256
SHARED_FF = 256
ROUTED_SCALE = 2.5
MOE_BLOCK = 128

LN_EPS = 1e-5
RMS_EPS = 1e-6

DEEPNORM_ALPHA = (2.0 * DEPTH) ** 0.25
DEEPNORM_BETA = (8.0 * DEPTH) ** -0.25

IN_SIZES = (Q_LORA, KV_LORA, QK_ROPE, S5_WIDTH, D_MODEL, D_MODEL)
IN_WIDTH = sum(IN_SIZES)
IN_OFFSETS = tuple(int(v) for v in np.cumsum(IN_SIZES)[:-1])

kernel_name = "hybrid_mla_s5_gated_deepnorm_moe"


def layer_norm(x, g, b):
    xf = x.astype(jnp.float32)
    mu = jnp.mean(xf, axis=-1, keepdims=True)
    var = jnp.mean(jnp.square(xf - mu), axis=-1, keepdims=True)
    y = (xf - mu) * lax.rsqrt(var + LN_EPS) * g.astype(jnp.float32) + b.astype(jnp.float32)
    return y.astype(x.dtype)


def rms_norm(x, g):
    xf = x.astype(jnp.float32)
    y = xf * lax.rsqrt(jnp.mean(jnp.square(xf), axis=-1, keepdims=True) + RMS_EPS)
    return (y * g.astype(jnp.float32)).astype(x.dtype)


def rope_cos_sin(positions):
    inv_freq = ROPE_THETA ** (-jnp.arange(0, QK_ROPE, 2, dtype=jnp.float32) / QK_ROPE)
    ang = positions.astype(jnp.float32)[..., None] * inv_freq
    return jnp.cos(ang), jnp.sin(ang)


def apply_rope(x, cos, sin):
    cos = cos.astype(x.dtype)
    sin = sin.astype(x.dtype)
    x1, x2 = jnp.split(x, 2, axis=-1)
    return jnp.concatenate([x1 * cos - x2 * sin, x2 * cos + x1 * sin], axis=-1)


def causal_block_attention(q_nope, q_rope, k_nope, k_rope, v):
    B, S, H, _ = q_nope.shape
    nb = S // Q_BLOCK
    scale = (QK_NOPE + QK_ROPE) ** -0.5
    qn_b = q_nope.reshape(B, nb, Q_BLOCK, H, QK_NOPE).transpose(1, 0, 2, 3, 4)
    qr_b = q_rope.reshape(B, nb, Q_BLOCK, H, QK_ROPE).transpose(1, 0, 2, 3, 4)
    key_pos = jnp.arange(S)

    def one_block(args):
        i, qn, qr = args
        s = (jnp.einsum('bqhd,bkhd->bhqk', qn, k_nope)
             + jnp.einsum('bqhr,bkr->bhqk', qr, k_rope)).astype(jnp.float32) * scale
        q_pos = i * Q_BLOCK + jnp.arange(Q_BLOCK)
        mask = key_pos[None, :] <= q_pos[:, None]
        s = jnp.where(mask[None, None], s, -jnp.inf)
        p = jax.nn.softmax(s, axis=-1).astype(v.dtype)
        return jnp.einsum('bhqk,bkhd->bqhd', p, v)

    out = lax.map(one_block, (jnp.arange(nb), qn_b, qr_b))
    return out.transpose(1, 0, 2, 3, 4).reshape(B, S, H, V_HEAD)


def mla_branch(h_q, h_kv, k_rope_raw, positions, q_norm_g, kv_norm_g, w_uq, w_ukv, w_mla_o):
    B, S, _ = h_q.shape
    q = (rms_norm(h_q, q_norm_g) @ w_uq).reshape(B, S, MLA_HEADS, QK_NOPE + QK_ROPE)
    q_nope, q_rope = q[..., :QK_NOPE], q[..., QK_NOPE:]
    kv = (rms_norm(h_kv, kv_norm_g) @ w_ukv).reshape(B, S, MLA_HEADS, QK_NOPE + V_HEAD)
    k_nope, v = kv[..., :QK_NOPE], kv[..., QK_NOPE:]
    cos, sin = rope_cos_sin(positions)
    q_rope = apply_rope(q_rope, cos[:, :, None, :], sin[:, :, None, :])
    k_rope = apply_rope(k_rope_raw, cos, sin)
    attn = causal_block_attention(q_nope, q_rope, k_nope, k_rope, v)
    return attn.reshape(B, S, MLA_HEADS * V_HEAD) @ w_mla_o


def _complex_affine_combine(c1, c2):
    a1r, a1i, b1r, b1i = c1
    a2r, a2i, b2r, b2i = c2
    ar = a2r * a1r - a2i * a1i
    ai = a2r * a1i + a2i * a1r
    br = a2r * b1r - a2i * b1i + b2r
    bi = a2r * b1i + a2i * b1r + b2i
    return (ar, ai, br, bi)


def s5_branch(u, a_re, a_im, log_dt, b_re, b_im, c_re, c_im, d, w_glu):
    B, S, _ = u.shape
    f32 = jnp.float32
    uf = u.astype(f32).reshape(B, S, S5_GROUPS, S5_GROUP_CH)
    dt = jnp.exp(log_dt.astype(f32))[:, None]
    ar, ai = a_re.astype(f32), a_im.astype(f32)
    mag = jnp.exp(ar * dt)
    abar_r, abar_i = mag * jnp.cos(ai * dt), mag * jnp.sin(ai * dt)
    den = ar * ar + ai * ai
    nr, ni = abar_r - 1.0, abar_i
    coef_r = ((nr * ar + ni * ai) / den)[..., None]
    coef_i = ((ni * ar - nr * ai) / den)[..., None]
    br, bi = b_re.astype(f32), b_im.astype(f32)
    bbar_r = coef_r * br - coef_i * bi
    bbar_i = coef_r * bi + coef_i * br
    bu_r = jnp.einsum('bsgh,gph->bsgp', uf, bbar_r)
    bu_i = jnp.einsum('bsgh,gph->bsgp', uf, bbar_i)
    a_r = jnp.broadcast_to(abar_r, bu_r.shape)
    a_i = jnp.broadcast_to(abar_i, bu_i.shape)
    _, _, xr, xi = lax.associative_scan(_complex_affine_combine, (a_r, a_i, bu_r, bu_i), axis=1)
    y = (jnp.einsum('bsgp,ghp->bsgh', xr, c_re.astype(f32))
         - jnp.einsum('bsgp,ghp->bsgh', xi, c_im.astype(f32)))
    y = y.reshape(B, S, S5_WIDTH) + d.astype(f32) * uf.reshape(B, S, S5_WIDTH)
    y = jax.nn.gelu(y).astype(u.dtype)
    val, gate = jnp.split(y @ w_glu, 2, axis=-1)
    return val * jax.nn.sigmoid(gate)


def hybrid_mixer(x, positions, w_in, q_norm_g, kv_norm_g, w_uq, w_ukv, w_mla_o,
                 s5_a_re, s5_a_im, s5_log_dt, s5_b_re, s5_b_im, s5_c_re, s5_c_im,
                 s5_d, w_s5_glu, w_out):
    proj = x @ w_in
    h_q, h_kv, k_rope_raw, u, g_mla, g_s5 = jnp.split(proj, IN_OFFSETS, axis=-1)
    y_mla = mla_branch(h_q, h_kv, k_rope_raw, positions, q_norm_g, kv_norm_g, w_uq, w_ukv, w_mla_o)
    y_s5 = s5_branch(u, s5_a_re, s5_a_im, s5_log_dt, s5_b_re, s5_b_im, s5_c_re, s5_c_im, s5_d, w_s5_glu)
    merged = jax.nn.sigmoid(g_mla) * y_mla + jax.nn.sigmoid(g_s5) * y_s5
    return merged @ w_out


def memory_cross_attention(x, mem, mem_ln_g, mem_ln_b, w_xq, w_xkv, w_xo):
    B, S, _ = x.shape
    M = mem.shape[1]
    m = layer_norm(mem, mem_ln_g, mem_ln_b)
    q = (x @ w_xq).reshape(B, S, XATTN_HEADS, XATTN_HEAD_DIM)
    kv = (m @ w_xkv).reshape(B, M, 2, XATTN_HEADS, XATTN_HEAD_DIM)
    k, v = kv[:, :, 0], kv[:, :, 1]
    s = jnp.einsum('bshd,bmhd->bhsm', q, k).astype(jnp.float32) * (XATTN_HEAD_DIM ** -0.5)
    p = jax.nn.softmax(s, axis=-1).astype(v.dtype)
    o = jnp.einsum('bhsm,bmhd->bshd', p, v).reshape(B, S, XATTN_HEADS * XATTN_HEAD_DIM)
    return o @ w_xo


def swiglu(x, w_gu, w_down):
    g, u = jnp.split(x @ w_gu, 2, axis=-1)
    return (jax.nn.silu(g) * u) @ w_down


def moe_ffn(x2d, w_router, router_bias, w_exp_gu, w_exp_down, w_sh_gu, w_sh_down):
    T, D = x2d.shape
    scores = jax.nn.sigmoid((x2d @ w_router).astype(jnp.float32))
    sel = scores + router_bias.astype(jnp.float32)
    per_group = N_EXPERTS // N_EXPERT_GROUPS
    grp_score = lax.top_k(sel.reshape(T, N_EXPERT_GROUPS, per_group), 2)[0].sum(-1)
    _, grp_idx = lax.top_k(grp_score, TOPK_GROUPS)
    grp_mask = jax.nn.one_hot(grp_idx, N_EXPERT_GROUPS, dtype=jnp.float32).sum(1) > 0
    sel = jnp.where(jnp.repeat(grp_mask, per_group, axis=1), sel, -jnp.inf)
    _, top_idx = lax.top_k(sel, TOP_K)
    w = jnp.take_along_axis(scores, top_idx, axis=1)
    w = w / jnp.sum(w, axis=-1, keepdims=True) * ROUTED_SCALE

    A = T * TOP_K
    flat_e = top_idx.reshape(A).astype(jnp.int32)
    flat_tok = jnp.repeat(jnp.arange(T, dtype=jnp.int32), TOP_K)
    flat_w = w.reshape(A).astype(x2d.dtype)
    order = jnp.argsort(flat_e)
    se = flat_e[order]
    counts = jnp.zeros((N_EXPERTS,), jnp.int32).at[flat_e].add(1)
    starts = jnp.cumsum(counts) - counts
    padded = (counts + MOE_BLOCK - 1) // MOE_BLOCK * MOE_BLOCK
    pends = jnp.cumsum(padded)
    pstarts = pends - padded
    dest = pstarts[se] + (jnp.arange(A, dtype=jnp.int32) - starts[se])
    n_blocks = (A + N_EXPERTS * (MOE_BLOCK - 1) + MOE_BLOCK - 1) // MOE_BLOCK
    P = n_blocks * MOE_BLOCK
    buf_tok = jnp.full((P,), T, jnp.int32).at[dest].set(flat_tok[order])
    buf_w = jnp.zeros((P,), x2d.dtype).at[dest].set(flat_w[order])
    block_start = jnp.arange(n_blocks, dtype=jnp.int32) * MOE_BLOCK
    block_e = jnp.minimum(jnp.searchsorted(pends, block_start, side='right'), N_EXPERTS - 1)
    x_pad = jnp.concatenate([x2d, jnp.zeros((1, D), x2d.dtype)], axis=0)

    def expert_block(args):
        tok, e = args
        return swiglu(x_pad[tok], w_exp_gu[e], w_exp_down[e])

    out = lax.map(expert_block, (buf_tok.reshape(n_blocks, MOE_BLOCK), block_e))
    out = out.reshape(P, D) * buf_w[:, None]
    routed = jnp.zeros((T + 1, D), x2d.dtype).at[buf_tok].add(out)[:T]
    return routed + swiglu(x2d, w_sh_gu, w_sh_down)


def setup_inputs(seed: int = 0) -> dict:
    key = jax.random.key(seed)
    ks = iter(jax.random.split(key, 48))
    L, D = DEPTH, D_MODEL
    f32 = jnp.float32

    def nrm(shape, scale):
        return jax.random.normal(next(ks), shape, f32) * scale

    def gain(shape):
        return 1.0 + 0.02 * jax.random.normal(next(ks), shape, f32)

    x = jax.random.normal(next(ks), (BATCH, SEQ, D), f32)
    mem = jax.random.normal(next(ks), (BATCH, MEM_LEN, D), f32)
    positions = (jnp.arange(SEQ, dtype=jnp.int32)[None, :]
                 + jax.random.randint(next(ks), (BATCH, 1), 0, 1024, jnp.int32))
    a_re = -0.5 + 0.01 * jax.random.normal(next(ks), (L, S5_GROUPS, S5_STATE), f32)
    a_im = (jnp.pi * jnp.arange(S5_STATE, dtype=f32))[None, None, :] \
        + 0.01 * jax.random.normal(next(ks), (L, S5_GROUPS, S5_STATE), f32)
    log_dt = jax.random.uniform(next(ks), (L, S5_GROUPS), f32, math.log(DT_MIN), math.log(DT_MAX))
    beta = DEEPNORM_BETA
    return {
        "x": x,
        "mem": mem,
        "positions": positions,
        "w_in": nrm((L, D, IN_WIDTH), D ** -0.5),
        "q_norm_g": gain((L, Q_LORA)),
        "kv_norm_g": gain((L, KV_LORA)),
        "w_uq": nrm((L, Q_LORA, MLA_HEADS * (QK_NOPE + QK_ROPE)), Q_LORA ** -0.5),
        "w_ukv": nrm((L, KV_LORA, MLA_HEADS * (QK_NOPE + V_HEAD)), KV_LORA ** -0.5),
        "w_mla_o": nrm((L, MLA_HEADS * V_HEAD, D), (MLA_HEADS * V_HEAD) ** -0.5),
        "s5_a_re": a_re,
        "s5_a_im": a_im,
        "s5_log_dt": log_dt,
        "s5_b_re": nrm((L, S5_GROUPS, S5_STATE, S5_GROUP_CH), (2 * S5_GROUP_CH) ** -0.5),
        "s5_b_im": nrm((L, S5_GROUPS, S5_STATE, S5_GROUP_CH), (2 * S5_GROUP_CH) ** -0.5),
        "s5_c_re": nrm((L, S5_GROUPS, S5_GROUP_CH, S5_STATE), (2 * S5_STATE) ** -0.5 * 4.0),
        "s5_c_im": nrm((L, S5_GROUPS, S5_GROUP_CH, S5_STATE), (2 * S5_STATE) ** -0.5 * 4.0),
        "s5_d": nrm((L, S5_WIDTH), 1.0),
        "w_s5_glu": nrm((L, S5_WIDTH, 2 * D), S5_WIDTH ** -0.5),
        "w_out": nrm((L, D, D), D ** -0.5 * beta),
        "ln1_g": gain((L, D)),
        "ln1_b": nrm((L, D), 0.02),
        "mem_ln_g": gain((L, D)),
        "mem_ln_b": nrm((L, D), 0.02),
        "w_xq": nrm((L, D, XATTN_HEADS * XATTN_HEAD_DIM), D ** -0.5),
        "w_xkv": nrm((L, D, 2 * XATTN_HEADS * XATTN_HEAD_DIM), D ** -0.5),
        "w_xo": nrm((L, XATTN_HEADS * XATTN_HEAD_DIM, D), (XATTN_HEADS * XATTN_HEAD_DIM) ** -0.5 * beta),
        "ln2_g": gain((L, D)),
        "ln2_b": nrm((L, D), 0.02),
        "w_router": nrm((L, D, N_EXPERTS), D ** -0.5),
        "router_bias": nrm((L, N_EXPERTS), 0.01),
        "w_exp_gu": nrm((L, N_EXPERTS, D, 2 * EXPERT_FF), D ** -0.5),
        "w_exp_down": nrm((L, N_EXPERTS, EXPERT_FF, D), EXPERT_FF ** -0.5 * beta),
        "w_sh_gu": nrm((L, D, 2 * SHARED_FF), D ** -0.5),
        "w_sh_down": nrm((L, SHARED_FF, D), SHARED_FF ** -0.5 * beta),
        "ln3_g": gain((L, D)),
        "ln3_b": nrm((L, D), 0.02),
    }


def reference(x, mem, positions, w_in, q_norm_g, kv_norm_g, w_uq, w_ukv, w_mla_o,
              s5_a_re, s5_a_im, s5_log_dt, s5_b_re, s5_b_im, s5_c_re, s5_c_im, s5_d,
              w_s5_glu, w_out, ln1_g, ln1_b, mem_ln_g, mem_ln_b, w_xq, w_xkv, w_xo,
              ln2_g, ln2_b, w_router, router_bias, w_exp_gu, w_exp_down, w_sh_gu,
              w_sh_down, ln3_g, ln3_b):
    B, S, D = x.shape
    for l in range(DEPTH):
        mix = hybrid_mixer(x, positions, w_in[l], q_norm_g[l], kv_norm_g[l], w_uq[l], w_ukv[l],
                           w_mla_o[l], s5_a_re[l], s5_a_im[l], s5_log_dt[l], s5_b_re[l],
                           s5_b_im[l], s5_c_re[l], s5_c_im[l], s5_d[l], w_s5_glu[l], w_out[l])
        x = layer_norm(DEEPNORM_ALPHA * x + mix, ln1_g[l], ln1_b[l])
        xa = memory_cross_attention(x, mem, mem_ln_g[l], mem_ln_b[l], w_xq[l], w_xkv[l], w_xo[l])
        x = layer_norm(DEEPNORM_ALPHA * x + xa, ln2_g[l], ln2_b[l])
        ff = moe_ffn(x.reshape(B * S, D), w_router[l], router_bias[l], w_exp_gu[l],
                     w_exp_down[l], w_sh_gu[l], w_sh_down[l]).reshape(B, S, D)
        x = layer_norm(DEEPNORM_ALPHA * x + ff, ln3_g[l], ln3_b[l])
    return x
```

```python
import numpy as np
from contextlib import ExitStack
import concourse.bass as bass
import concourse.mybir as mybir
from concourse.bass_utils import run_bass_kernel_spmd

F32 = mybir.dt.float32
BF16 = mybir.dt.bfloat16
I32 = mybir.dt.int32
U8 = mybir.dt.uint8
ALU = mybir.AluOpType
AF = mybir.ActivationFunctionType
AX = mybir.AxisListType

ENGS = ["pe", "act", "dve", "pool", "sp"]
NDMA_SLOTS = 12
DEBUG = False
STAGE = 99
NOMASK = False
XA_CUT = 99
N_EXPERTS_RUN = 64
LN_GB_ENG = "pool"
ST_Q = "pool"
CAP = 512
NS = 64 * CAP

ALPHA = 2.0 ** 0.25
MAGIC = 12582912.0
TWO_PI = 2.0 * np.pi


class Prog:
    def __init__(self, nc, stack):
        self.nc = nc
        self.lists = {e: [] for e in ENGS}
        self.cnt = {e: 0 for e in ENGS}
        self.seen = {e: {} for e in ENGS}
        self.tok = {}
        self.sem = {}
        for e in ENGS:
            self.sem[e] = stack.enter_context(nc.semaphore("s_" + e))
        self.dma_slots = {}
        self.dma_rr = {}
        self.dma_uses = {}
        for q in ["sp", "pool"]:
            self.dma_slots[q] = []
            for i in range(NDMA_SLOTS):
                k = "d_%s_%d" % (q, i)
                self.sem[k] = stack.enter_context(nc.semaphore(k))
                self.dma_slots[q].append(k)
                self.dma_uses[k] = 0
            self.dma_rr[q] = 0
        self.n_inst = 0

    def _need(self, E, deps):
        for (k, v) in deps:
            if k == E and E == "pe":
                continue
            if self.seen[E].get(k, 0) >= v:
                continue
            self.seen[E][k] = v
            sem = self.sem[k]
            self.lists[E].append(lambda h, sem=sem, v=v: h.wait_ge(sem, v))

    def _collect(self, reads, writes):
        deps = {}

        def add(d):
            if d is None:
                return
            k, v = d
            if deps.get(k, 0) < v:
                deps[k] = v
        for t in reads:
            st = self.tok.get(t)
            if st is not None:
                add(st["w"])
        for t in writes:
            st = self.tok.get(t)
            if st is not None:
                add(st["w"])
                for k, v in st["r"].items():
                    add((k, v))
        return list(deps.items())

    def _update(self, dep, reads, writes):
        k, v = dep
        for t in reads:
            st = self.tok.setdefault(t, {"w": None, "r": {}})
            if st["r"].get(k, 0) < v:
                st["r"][k] = v
        for t in writes:
            self.tok[t] = {"w": dep, "r": {}}

    def op(self, E, fn, reads=(), writes=()):
        deps = self._collect(reads, writes)
        self._need(E, deps)
        self.cnt[E] += 1
        idx = self.cnt[E]
        sem = self.sem[E]
        self.lists[E].append(lambda h, fn=fn, sem=sem: fn(h).then_inc(sem, 1))
        self._update((E, idx), reads, writes)
        self.n_inst += 1

    def dma(self, Q, out, in_, reads=(), writes=(), **kw):
        slots = self.dma_slots[Q]
        s = slots[self.dma_rr[Q] % len(slots)]
        self.dma_rr[Q] += 1
        deps = self._collect(reads, writes)
        if self.dma_uses[s] > 0:
            deps.append((s, 16 * self.dma_uses[s]))
        self._need(Q, deps)
        self.dma_uses[s] += 1
        v = 16 * self.dma_uses[s]
        sem = self.sem[s]
        self.lists[Q].append(lambda h, out=out, in_=in_, kw=kw, sem=sem: h.dma_start(out=out, in_=in_, allow_slow_non_contiguous=True, **kw).then_inc(sem, 16))
        self._update((s, v), reads, writes)
        self.n_inst += 1
        return (s, v)

    def idma(self, out, in_, idx_ap, gather, reads=(), writes=()):
        Q = "pool"
        slots = self.dma_slots[Q]
        s = slots[self.dma_rr[Q] % len(slots)]
        self.dma_rr[Q] += 1
        deps = self._collect(reads, writes)
        if self.dma_uses[s] > 0:
            deps.append((s, 16 * self.dma_uses[s]))
        self._need(Q, deps)
        self.dma_uses[s] += 1
        v = 16 * self.dma_uses[s]
        sem = self.sem[s]
        off = bass.IndirectOffsetOnAxis(idx_ap, 0)
        if gather:
            self.lists[Q].append(lambda h: h.indirect_dma_start(out=out, out_offset=None, in_=in_, in_offset=off).then_inc(sem, 16))
        else:
            self.lists[Q].append(lambda h: h.indirect_dma_start(out=out, out_offset=off, in_=in_, in_offset=None).then_inc(sem, 16))
        self._update((s, v), reads, writes)
        self.n_inst += 1

    def barrier(self):
        deps = [(e, self.cnt[e]) for e in ENGS if self.cnt[e] > 0]
        for s, u in self.dma_uses.items():
            if u > 0:
                deps.append((s, 16 * u))
        for E in ENGS:
            self._need(E, deps)
        self.tok = {}

    def emit(self):
        print("inst counts", self.cnt, {k: v for k, v in self.dma_uses.items() if v}, flush=True)
        nc = self.nc
        L = self.lists
        with nc.Block() as block:
            @block.tensor
            def _(h):
                for f in L["pe"]:
                    f(h)

            @block.scalar
            def _(h):
                for f in L["act"]:
                    f(h)

            @block.vector
            def _(h):
                for f in L["dve"]:
                    f(h)

            @block.gpsimd
            def _(h):
                for f in L["pool"]:
                    f(h)

            @block.sync
            def _(h):
                for f in L["sp"]:
                    f(h)


class Arena:
    def __init__(self, nc, stack, nbytes):
        self.t = stack.enter_context(nc.sbuf_tensor("arena", [128, nbytes], U8))
        self.nbytes = nbytes
        self.top = 0
        self.marks = []
        self.hi = nbytes

    def alloc(self, shape_free, dtype, top=False):
        esz = {F32: 4, BF16: 2, I32: 4, U8: 1, mybir.dt.uint32: 4}[dtype]
        n = int(np.prod(shape_free)) * esz
        if top:
            off = (self.hi - n) // 64 * 64
            assert off >= self.top, ("arena overflow(top)", off, n, self.top)
            self.hi = off
        else:
            off = (self.top + 63) // 64 * 64
            assert off + n <= self.hi, ("arena overflow", off, n, self.hi)
            self.top = off + n
        ap = self.t[:, off:off + n].bitcast(dtype)
        if len(shape_free) > 1:
            names = " ".join("d%d" % i for i in range(len(shape_free)))
            kw = {"d%d" % i: int(s) for i, s in enumerate(shape_free)}
            ap = ap.rearrange("p (%s) -> p %s" % (names, names), **kw)
        return ap

    def mark(self):
        self.marks.append(self.top)

    def release(self):
        self.top = self.marks.pop()


def build(debug_names=(), sparse=True):
    nc = bass.Bass("TRN2", target_bir_lowering=False)

    def din(name, shape, dt=F32):
        return nc.dram_tensor(name, list(shape), dt, kind="ExternalInput").ap()

    x_own = din("x_own", [2048, 1024])
    x_full = din("x_full", [4096, 1024])
    mem_in = din("mem", [256, 1024])
    pos_full = din("pos_full", [4096], I32)
    pos_own = din("pos_own", [2048], I32)
    masks_in = din("masks", [128, 256])
    rflag_in = din("rflag", [128, 2])
    ident_in = din("ident", [128, 128])
    blockmask_in = din("blockmask", [128, 128])
    parity_in = din("parity", [128, 2])
    invf_in = din("invf", [128, 1])
    w_in = din("w_in", [1024, 3104])
    q_norm_g = din("q_norm_g", [256])
    kv_norm_g = din("kv_norm_g", [256])
    w_uq = din("w_uq", [256, 768])
    w_ukv = din("w_ukv", [256, 1024])
    w_mla_o = din("w_mla_o", [512, 1024])
    s5_a_re = din("s5_a_re", [32, 64])
    s5_a_im = din("s5_a_im", [32, 64])
    s5_log_dt = din("s5_log_dt", [32])
    s5_b_re = din("s5_b_re", [32, 64, 16])
    s5_b_im = din("s5_b_im", [32, 64, 16])
    s5_c_re = din("s5_c_re", [32, 16, 64])
    s5_c_im = din("s5_c_im", [32, 16, 64])
    s5_d = din("s5_d", [512])
    w_s5_glu = din("w_s5_glu", [512, 2048])
    w_out = din("w_out", [1024, 1024])
    ln_g = {}
    ln_b = {}
    for nm in ["ln1", "mem_ln", "ln2", "ln3"]:
        ln_g[nm] = din(nm + "_g", [1024])
        ln_b[nm] = din(nm + "_b", [1024])
    w_xq = din("w_xq", [1024, 512])
    w_xkv = din("w_xkv", [1024, 1024])
    w_xo = din("w_xo", [512, 1024])
    w_router = din("w_router", [1024, 64])
    router_bias = din("router_bias", [64])
    w_exp_gu = din("w_exp_gu", [64, 1024, 512])
    w_exp_down = din("w_exp_down", [64, 256, 1024])
    w_sh_gu = din("w_sh_gu", [1024, 512])
    w_sh_down = din("w_sh_down", [256, 1024])

    out_d = nc.dram_tensor("out", [2048, 1024], F32, kind="ExternalOutput").ap()
    ovf_d = nc.dram_tensor("ovf", [128, 1], F32, kind="ExternalOutput").ap()
    x2b_d = nc.dram_tensor("x2b_scr", [2049, 1024], BF16, kind="Internal").ap()
    tbl_d = nc.dram_tensor("tbl_scr", [NS + 16384, 2], F32, kind="Internal").ap()
    y_d = nc.dram_tensor("y_scr", [NS + 16384, 1024], BF16, kind="Internal").ap()
    triu_in = din("triu", [128, 128])
    iota_in = din("iota64", [128, 64])
    dum_in = din("dum8", [128, 16 * 8])
    x1_d = nc.dram_tensor("x1_scr", [2048, 1024], F32, kind="Internal").ap()
    x2_d = nc.dram_tensor("x2_scr", [2048, 1024], F32, kind="Internal").ap()
    dbg = {}

    with ExitStack() as stack:
        P = Prog(nc, stack)
        A = Arena(nc, stack, 206 * 1024)
        pst = [stack.enter_context(nc.psum_tensor("ps%d" % i, [128, 512], F32)) for i in range(8)]
        ps = [p_[:, :] for p_ in pst]
        psb = [p_.bitcast(BF16)[:, :] for p_ in pst]
        ps_rr = [0]

        def nps():
            i = ps_rr[0] % 8
            ps_rr[0] += 1
            return i

        def tap(name, ap, shape, reads, dt=F32):
            if name not in debug_names:
                return
            d = nc.dram_tensor("dbg_" + name, list(shape), dt, kind="ExternalOutput").ap()
            P.dma("sp", d, ap, reads=reads, writes=["dbg_" + name])
            dbg[name] = d

        def mm(out, lhsT, rhs, start, stop, reads, writes):
            P.op("pe", lambda h: h.matmul(out, lhsT=lhsT, rhs=rhs, start=start, stop=stop, skip_group_check=True), reads=reads, writes=writes)

        def tr(out, in_, ident, reads, writes):
            P.op("pe", lambda h: h.transpose(out=out, in_=in_, identity=ident), reads=reads, writes=writes)

        evac_rr = [0]

        def evac(out, in_, reads, writes, eng=None):
            if eng is None:
                eng = "act" if evac_rr[0] % 2 == 0 else "dve"
                evac_rr[0] += 1
            if eng == "act":
                P.op("act", lambda h: h.copy(out=out, in_=in_), reads=reads, writes=writes)
            else:
                P.op(eng, lambda h: h.tensor_copy(out=out, in_=in_), reads=reads, writes=writes)

        def tt(E, out, in0, in1, op, reads, writes):
            P.op(E, lambda h: h.tensor_tensor(out=out, in0=in0, in1=in1, op=op), reads=reads, writes=writes)

        def ts(E, out, in0, s1, s2, op0, op1, reads, writes):
            if op1 is None:
                P.op(E, lambda h: h.tensor_scalar(out=out, in0=in0, scalar1=s1, scalar2=None, op0=op0), reads=reads, writes=writes)
            else:
                P.op(E, lambda h: h.tensor_scalar(out=out, in0=in0, scalar1=s1, scalar2=s2, op0=op0, op1=op1), reads=reads, writes=writes)

        def stt(out, in0, scalar, in1, op0, op1, reads, writes):
            P.op("dve", lambda h: h.scalar_tensor_tensor(out=out, in0=in0, scalar=scalar, in1=in1, op0=op0, op1=op1), reads=reads, writes=writes)

        def act(out, in_, func, reads, writes, scale=1.0, bias=None, accum_out=None):
            kw = {}
            if bias is not None:
                kw["bias"] = bias
            if accum_out is not None:
                kw["accum_out"] = accum_out
            P.op("act", lambda h: h.activation(out=out, in_=in_, func=func, scale=scale, **kw), reads=reads, writes=writes)

        def load_w_bf16(dst, src, kt, tokname):
            for k in range(kt):
                P.dma("pool", dst[:, k, :], src[k * 128:(k + 1) * 128, :], writes=[(tokname, k)])

        def sincos(E_tmp, yin, n, parts, toks_in, sin_out, cos_out, tokname, tmps=None):
            p0, p1 = parts
            if tmps is None:
                tmps = (A.alloc([n], F32), A.alloc([n], F32))
            t1 = tmps[0][p0:p1]
            t2 = tmps[1][p0:p1]
            for which, outap, off in (("s", sin_out, 0.0), ("c", cos_out, 0.25)):
                ts("dve", t1, yin, off, MAGIC, ALU.add, ALU.add, toks_in, [tokname + "t1"])
                ts("dve", t2, t1, -MAGIC, None, ALU.add, None, [tokname + "t1"], [tokname + "t2"])
                stt(t1, yin, off, t2, ALU.add, ALU.subtract, toks_in + [tokname + "t2"], [tokname + "t1"])
                act(outap, t1, AF.Sin, [tokname + "t1"], [tokname + which], scale=TWO_PI)

        identf = A.alloc([128], F32)
        identb = A.alloc([128], BF16)
        P.dma("sp", identf, ident_in, writes=["identf"])
        P.dma("pool", identb, ident_in, writes=["identb"])
        rflag = A.alloc([2], F32)
        P.dma("sp", rflag, rflag_in, writes=["rflag"])
        ones_b = A.alloc([128], BF16)
        P.op("dve", lambda h: h.memset(ones_b, 1.0), writes=["ones_b"])
        eps_ln = A.alloc([1], F32)
        P.op("dve", lambda h: h.memset(eps_ln, 1e-5), writes=["eps"])

        def ln_bcast_load(nm):
            g = A.alloc([1024], F32)
            b = A.alloc([1024], F32)
            P.dma("sp", g, ln_g[nm].partition_broadcast(128), writes=[nm + "_g"])
            P.dma("sp", b, ln_b[nm].partition_broadcast(128), writes=[nm + "_b"])
            return g, b

        ln_par = [0]

        def layer_norm_a(src, nm0, reads, tmp_stats):
            par = ln_par[0] % 4
            ln_par[0] += 1
            st6a, mva, rstda = tmp_stats
            st6, mv, rstd = st6a[:, par, :], mva[:, par, :], rstda[:, par, :]
            nm = (nm0, par)
            for i in range(2):
                P.op("dve", lambda h, i=i: h.bn_stats(out=st6[:, i * 6:(i + 1) * 6], in_=src[:, i * 512:(i + 1) * 512]), reads=reads, writes=[("lnst", nm, i)])
            P.op("dve", lambda h: h.bn_aggr(out=mv, in_=st6), reads=[("lnst", nm, 0), ("lnst", nm, 1)], writes=[("lnmv", nm)])
            act(rstd, mv[:, 1:2], AF.Ln, [("lnmv", nm)], [("lnr", nm)], bias=eps_ln[:, 0:1])
            act(rstd, rstd, AF.Exp, [("lnr", nm)], [("lnr", nm)], scale=-0.5)
            return (mv, rstd, nm, nm0)

        def layer_norm_b(hnd, src, dst, g, b, reads, writes, gb_eng=None):
            mv, rstd, nm, nm0 = hnd
            gb_eng = gb_eng or LN_GB_ENG
            ts("dve", dst, src, mv[:, 0:1], rstd, ALU.subtract, ALU.mult, list(reads) + [("lnmv", nm), ("lnr", nm)], writes)
            tt(gb_eng, dst, dst, g, ALU.mult, list(writes) + [nm0 + "_g"], writes)
            tt(gb_eng, dst, dst, b, ALU.add, list(writes) + [nm0 + "_b"], writes)

        def layer_norm(src, dst, g, b, nm0, reads, writes, tmp_stats):
            hnd = layer_norm_a(src, nm0, reads, tmp_stats)
            layer_norm_b(hnd, src, dst, g, b, reads, writes)

        def load_xT_block(src_rows, dstT, col0, tokbase, xin_bufs, blk, eng=None):
            xb = xin_bufs[blk % len(xin_bufs)]
            xtok = ("xin", blk % len(xin_bufs))
            for t in range(4):
                P.dma("pool", xb[:, t, :], src_rows[t * 128:(t + 1) * 128, :], writes=[xtok + (t,)])
            for t in range(4):
                b_ = nps()
                for dt in range(8):
                    tr(psb[b_][:, dt * 128:(dt + 1) * 128], xb[:, t, dt * 128:(dt + 1) * 128], identb,
                       [xtok + (t,), "identb"], ["ps%d" % b_])
                evac(dstT[:, :, col0 + t * 128:col0 + (t + 1) * 128], psb[b_].rearrange("p (a b) -> p a b", a=8),
                     ["ps%d" % b_], [(tokbase, (col0 // 128) + t)], eng=eng)

        A.mark()
        XownS = A.alloc([32, 128], BF16)
        hi_save = A.hi
        W1 = A.alloc([4, 2, 16, 128], BF16, top=True)
        KB = A.alloc([4, 16, 128], BF16)
        Cs = A.alloc([17, 32, 16], BF16)
        w_u = A.alloc([8, 512], BF16)
        sd = A.alloc([4], F32)
        AA1 = A.alloc([2, 16], F32)
        AA2 = A.alloc([2, 16], F32)
        with nc.allow_non_contiguous_dma(reason="tiny param loads"):
            P.dma("sp", sd, s5_d.rearrange("(t r) -> r t", r=128), writes=["sd"])
        A.mark()
        load_w_bf16(w_u, w_in[:, 544:1056], 8, "w_u")
        wutoks = [("w_u", k) for k in range(8)]
        uTf = A.alloc([4, 16, 128], BF16)
        xin_bufs = [A.alloc([4, 1024], BF16)]
        xTblk = [A.alloc([8, 512], BF16) for _ in range(2)]

        def u_blocks(half, eng):
            for bl in range(4):
                blk = half * 4 + bl
                xt = xTblk[blk % 2]
                tb = "xTb%d" % (blk % 2)
                load_xT_block(x_full[blk * 512:(blk + 1) * 512, :], xt, 0, tb, xin_bufs, blk, eng=eng)
                for T in range(4):
                    b_ = nps()
                    for dt in range(8):
                        mm(ps[b_], w_u[:, dt, T * 128:(T + 1) * 128], xt[:, dt, :], dt == 0, dt == 7,
                           wutoks + [(tb, t) for t in range(4)], ["ps%d" % b_])
                    evac(uTf[:, T, :, bl * 32:(bl + 1) * 32], ps[b_].rearrange("p (c j) -> p j c", j=16), ["ps%d" % b_], [("uTf", T)], eng=eng)

        A.mark()
        are = A.alloc([32], F32)
        aim = A.alloc([32], F32)
        ldt = A.alloc([32], F32)
        bre = A.alloc([32, 16], F32)
        bim = A.alloc([32, 16], F32)
        cre = A.alloc([32, 16], F32)
        cim = A.alloc([32, 16], F32)
        craw = A.alloc([2, 4, 2, 64], F32)
        with nc.allow_non_contiguous_dma(reason="tiny param loads"):
            for hf in range(2):
                sl = slice(hf * 64, hf * 64 + 64)
                P.dma("sp", are[sl, :], s5_a_re.rearrange("g p -> p g"), writes=[("are", hf)])
                P.dma("sp", aim[sl, :], s5_a_im.rearrange("g p -> p g"), writes=[("aim", hf)])
                P.dma("sp", bre[sl], s5_b_re.rearrange("g p h -> p g h"), writes=[("bre", hf)])
                P.dma("sp", bim[sl], s5_b_im.rearrange("g p h -> p g h"), writes=[("bim", hf)])
            P.dma("sp", ldt, s5_log_dt.partition_broadcast(128), writes=["ldt"])
            for ri, src in enumerate((s5_c_re, s5_c_im)):
                for dup in range(2):
                    P.dma("sp", craw[:, ri, :, dup, :], src.rearrange("g h p -> (g h) p").rearrange("(t r) p -> r t p", r=128),
                          writes=[("craw", ri, dup)])
        for ri, dstc in enumerate((cre, cim)):
            for T in range(4):
                b_ = nps()
                tr(ps[b_][:, 0:128], craw[:, ri, T].rearrange("p a b -> p (a b)"), identf,
                   [("craw", ri, 0), ("craw", ri, 1), "identf"], ["ps%d" % b_])
                evac(dstc[:, T * 8:(T + 1) * 8, :], ps[b_][:, 0:128].rearrange("p (a b) -> p a b", a=8), ["ps%d" % b_], [("c%d" % ri, T)])
        ctoks = [("c0", T) for T in range(4)] + [("c1", T) for T in range(4)]
        atoks = [("are", 0), ("are", 1), ("aim", 0), ("aim", 1)]
        btoks = [("bre", 0), ("bre", 1), ("bim", 0), ("bim", 1)]
        dtv = A.alloc([32], F32)
        mag = A.alloc([32], F32)
        yang = A.alloc([32], F32)
        sn = A.alloc([32], F32)
        cs = A.alloc([32], F32)
        act(dtv, ldt, AF.Exp, ["ldt"], ["dtv"])
        tt("dve", mag, are, dtv, ALU.mult, atoks + ["dtv"], ["mag"])
        act(mag, mag, AF.Exp, ["mag"], ["mag"])
        tt("dve", yang, aim, dtv, ALU.mult, atoks + ["dtv"], ["yang"])
        ts("dve", yang, yang, 1.0 / TWO_PI, None, ALU.mult, None, ["yang"], ["yang"])
        A.mark()
        sincos("dve", yang, 32, (0, 128), ["yang"], sn, cs, "s5sc")
        A.release()
        abr = A.alloc([32], F32)
        abi = A.alloc([32], F32)
        tt("dve", abr, mag, cs, ALU.mult, ["mag", "s5scc"], ["abr"])
        tt("dve", abi, mag, sn, ALU.mult, ["mag", "s5scs"], ["abi"])
        u_blocks(0, "act")
        den = A.alloc([32], F32)
        tmpa = A.alloc([32], F32)
        tmpb = A.alloc([32], F32)
        cr = A.alloc([32], F32)
        ci = A.alloc([32], F32)
        tt("dve", den, are, are, ALU.mult, atoks, ["den"])
        tt("dve", tmpa, aim, aim, ALU.mult, atoks, ["tmpa"])
        tt("dve", den, den, tmpa, ALU.add, ["den", "tmpa"], ["den"])
        P.op("dve", lambda h: h.reciprocal(out=den, in_=den), reads=["den"], writes=["den"])
        nr = A.alloc([32], F32)
        ts("dve", nr, abr, -1.0, None, ALU.add, None, ["abr"], ["nr"])
        tt("dve", tmpa, nr, are, ALU.mult, ["nr"] + atoks, ["tmpa"])
        tt("dve", tmpb, abi, aim, ALU.mult, ["abi"] + atoks, ["tmpb"])
        tt("dve", tmpa, tmpa, tmpb, ALU.add, ["tmpa", "tmpb"], ["tmpa"])
        tt("dve", cr, tmpa, den, ALU.mult, ["tmpa", "den"], ["cr"])
        tt("dve", tmpa, abi, are, ALU.mult, ["abi"] + atoks, ["tmpa"])
        tt("dve", tmpb, nr, aim, ALU.mult, ["nr"] + atoks, ["tmpb"])
        tt("dve", tmpa, tmpa, tmpb, ALU.subtract, ["tmpa", "tmpb"], ["tmpa"])
        tt("dve", ci, tmpa, den, ALU.mult, ["tmpa", "den"], ["ci"])
        bbr = A.alloc([32, 16], F32)
        bbi = A.alloc([32, 16], F32)
        tb1 = A.alloc([32, 16], F32)
        crB = cr.unsqueeze(2).to_broadcast([128, 32, 16])
        ciB = ci.unsqueeze(2).to_broadcast([128, 32, 16])
        tt("dve", bbr, bre, crB, ALU.mult, btoks + ["cr"], ["bbr"])
        tt("dve", tb1, bim, ciB, ALU.mult, btoks + ["ci"], ["tb1"])
        tt("dve", bbr, bbr, tb1, ALU.subtract, ["bbr", "tb1"], ["bbr"])
        tt("dve", bbi, bim, crB, ALU.mult, btoks + ["cr"], ["bbi"])
        tt("dve", tb1, bre, ciB, ALU.mult, btoks + ["ci"], ["tb1"])
        tt("dve", bbi, bbi, tb1, ALU.add, ["bbi", "tb1"], ["bbi"])
        pwr = A.alloc([17, 32], F32)
        pwi = A.alloc([17, 32], F32)
        P.op("dve", lambda h: h.memset(pwr[:, 0, :], 1.0), writes=[("pw", 0)])
        P.op("dve", lambda h: h.memset(pwi[:, 0, :], 0.0), writes=[("pw", 0)])
        for k in range(1, 17):
            rd = [("pw", k - 1), "abr", "abi"]
            tt("dve", tmpa, pwr[:, k - 1, :], abr, ALU.mult, rd, ["tmpa"])
            tt("dve", tmpb, pwi[:, k - 1, :], abi, ALU.mult, rd, ["tmpb"])
            tt("dve", pwr[:, k, :], tmpa, tmpb, ALU.subtract, ["tmpa", "tmpb"], [("pw", k)])
            tt("dve", tmpa, pwr[:, k - 1, :], abi, ALU.mult, rd, ["tmpa"])
            tt("dve", tmpb, pwi[:, k - 1, :], abr, ALU.mult, rd, ["tmpb"])
            tt("dve", pwi[:, k, :], tmpa, tmpb, ALU.add, ["tmpa", "tmpb"], [("pw", k)])
        pwtoks = [("pw", k) for k in range(17)]
        for hp in range(2):
            pp = slice(hp * 64, hp * 64 + 64)
            gg = slice(hp * 16, hp * 16 + 16)
            for ri in range(2):
                P.op("dve", lambda h, pp=pp, gg=gg, ri=ri: h.tensor_copy(out=AA1[pp, ri, :], in_=pwr[pp, 16, gg]), reads=pwtoks, writes=["AA1"])
            ts("dve", AA2[pp, 0, :], pwi[pp, 16, gg], -1.0, None, ALU.mult, None, pwtoks, ["AA2"])
            P.op("dve", lambda h, pp=pp, gg=gg: h.tensor_copy(out=AA2[pp, 1, :], in_=pwi[pp, 16, gg]), reads=pwtoks, writes=["AA2"])
        Bs = A.alloc([16, 32, 16], BF16)
        t4a = A.alloc([2, 32, 16], F32)
        t4b = A.alloc([2, 32, 16], F32)
        lo, hi = slice(0, 64), slice(64, 128)

        def bc_pw(pw, k0, nk, sl):
            return pw[sl, k0:k0 + nk, :].unsqueeze(3).to_broadcast([64, nk, 32, 16])

        def bc_x(xx, nk, sl):
            return xx[sl].unsqueeze(1).to_broadcast([64, nk, 32, 16])

        for k0 in range(0, 16, 2):
            tt("dve", t4a[lo], bc_pw(pwr, k0, 2, lo), bc_x(bbr, 2, lo), ALU.mult, pwtoks + ["bbr"], ["t4a"])
            tt("dve", t4b[lo], bc_pw(pwi, k0, 2, lo), bc_x(bbi, 2, lo), ALU.mult, pwtoks + ["bbi"], ["t4b"])
            tt("dve", Bs[lo, k0:k0 + 2], t4a[lo], t4b[lo], ALU.subtract, ["t4a", "t4b"], [("Bs", k0)])
            tt("dve", t4a[hi], bc_pw(pwr, k0, 2, hi), bc_x(bbi, 2, hi), ALU.mult, pwtoks + ["bbi"], ["t4a"])
            tt("dve", t4b[hi], bc_pw(pwi, k0, 2, hi), bc_x(bbr, 2, hi), ALU.mult, pwtoks + ["bbr"], ["t4b"])
            tt("dve", Bs[hi, k0:k0 + 2], t4a[hi], t4b[hi], ALU.add, ["t4a", "t4b"], [("Bs", k0)])
        for k0, nk in ((0, 2), (2, 2), (4, 2), (6, 2), (8, 2), (10, 2), (12, 2), (14, 2), (16, 1)):
            tt("dve", t4a[lo, 0:nk], bc_pw(pwr, k0, nk, lo), bc_x(cre, nk, lo), ALU.mult, pwtoks + ctoks, ["t4a"])
            tt("dve", t4b[lo, 0:nk], bc_pw(pwi, k0, nk, lo), bc_x(cim, nk, lo), ALU.mult, pwtoks + ctoks, ["t4b"])
            tt("dve", Cs[lo, k0:k0 + nk], t4a[lo, 0:nk], t4b[lo, 0:nk], ALU.subtract, ["t4a", "t4b"], [("Cs", k0)])
            tt("dve", t4a[hi, 0:nk], bc_pw(pwi, k0, nk, hi), bc_x(cre, nk, hi), ALU.mult, pwtoks + ctoks, ["t4a"])
            tt("dve", t4b[hi, 0:nk], bc_pw(pwr, k0, nk, hi), bc_x(cim, nk, hi), ALU.mult, pwtoks + ctoks, ["t4b"])
            stt(Cs[hi, k0:k0 + nk], t4a[hi, 0:nk], -1.0, t4b[hi, 0:nk], ALU.mult, ALU.subtract, ["t4a", "t4b"], [("Cs", k0)])
        Bstoks = [("Bs", k0) for k0 in range(0, 16, 2)]
        Cstoks = [("Cs", k0) for k0 in range(0, 17, 2)]
        parity = A.alloc([2], F32)
        blockmask = A.alloc([128], F32)
        P.dma("sp", parity, parity_in, writes=["parity"])
        P.dma("sp", blockmask, blockmask_in, writes=["blockmask"])
        for T in range(4):
            for j in range(16):
                b_ = nps()
                tr(psb[b_][:, 0:128], Bs[:, 15 - j, T * 8:(T + 1) * 8, :].rearrange("p a b -> p (a b)"), identb, Bstoks + ["identb"], ["ps%d" % b_])
                for e in range(2):
                    ts("dve", W1[:, T, e, j, :], psb[b_][:, 0:128], parity[:, e:e + 1], None, ALU.mult, None,
                       ["ps%d" % b_, "parity"], [("W1", T, e, j)])
        for T in range(4):
            for tau in range(16):
                b_ = nps()
                mm(ps[b_][:, 0:128], Bs[:, tau, T * 8:(T + 1) * 8, :].rearrange("p a b -> p (a b)"),
                   Cs[:, 0, T * 8:(T + 1) * 8, :].rearrange("p a b -> p (a b)"), True, True, Bstoks + Cstoks, ["ps%d" % b_])
                tt("dve", KB[:, T, tau, :], ps[b_][:, 0:128], blockmask, ALU.mult, ["ps%d" % b_, "blockmask"], [("KB", T)])
        tap("KB", KB, [128, 4, 16, 128], [("KB", T) for T in range(4)], BF16)
        tap("W1", W1, [128, 4, 2, 16, 128], [("W1", T, e, j) for T in range(4) for e in range(2) for j in range(16)], BF16)
        P.barrier()
        A.release()

        Cstoks = ["CsB"]
        Bp = A.alloc([2, 16, 257], F32)
        P.op("pool", lambda h: h.memset(Bp[:, :, :, 0:1], 0.0), writes=["Bp0"])
        for half in range(2):
            if half == 1:
                u_blocks(1, None)
            if half == 0:
                tap("uTf", uTf, [128, 4, 16, 128], [("uTf", T) for T in range(4)], BF16)
            for T in range(4):
                for s_ in range(4):
                    for e in range(2):
                        g = T * 8 + 2 * s_ + e
                        hp, gg = g // 16, g % 16
                        b_ = nps()
                        rows = slice(32 * s_, 32 * s_ + 32) if s_ < 3 else slice(64, 128)
                        for j in range(16):
                            for hf in range(2):
                                mm(ps[b_][hp * 64:hp * 64 + 64, hf * 128:(hf + 1) * 128], W1[rows, T, e, j, hf * 64:(hf + 1) * 64], uTf[rows, T, j, :],
                                   (j == 0 and hf == 0), (j == 15 and hf == 1), [("W1", T, e, j), ("uTf", T)], ["ps%d" % b_])
                        dst = Bp[hp * 64:hp * 64 + 64, :, gg, 1 + half * 128:1 + (half + 1) * 128]
                        src = ps[b_][hp * 64:hp * 64 + 64, 0:256].rearrange("p (a b) -> p a b", a=2)
                        if s_ < 3:
                            evac(dst, src, ["ps%d" % b_], [("Bp", g)])
                        else:
                            oth = Bp[hp * 64:hp * 64 + 64, :, gg - 2, 1 + half * 128:1 + (half + 1) * 128]
                            tt("dve", dst, src, oth, ALU.subtract, ["ps%d" % b_, ("Bp", g - 2)], [("Bp", g)])
        P.barrier()
        A.hi = hi_save
        Bq = Bp[:, :, :, 1:257].rearrange("p r g (s k) -> p r g s k", k=16)
        l1a = A.alloc([2, 16, 16], F32)
        l1b = A.alloc([2, 16, 16], F32)
        AA1b = AA1.unsqueeze(3).to_broadcast([128, 2, 16, 16])
        for k in range(1, 16):
            tt("dve", l1a, AA1b, Bq[:, :, :, :, k - 1], ALU.mult, ["Bq"], ["l1a"])
            tt("dve", l1b[:, 0], AA2[:, 0, :].unsqueeze(2).to_broadcast([128, 16, 16]), Bq[:, 1, :, :, k - 1], ALU.mult, ["Bq"], ["l1b"])
            tt("dve", l1b[:, 1], AA2[:, 1, :].unsqueeze(2).to_broadcast([128, 16, 16]), Bq[:, 0, :, :, k - 1], ALU.mult, ["Bq"], ["l1b"])
            tt("dve", l1a, l1a, l1b, ALU.add, ["l1a", "l1b"], ["l1a"])
            tt("dve", Bq[:, :, :, :, k], Bq[:, :, :, :, k], l1a, ALU.add, ["l1a", "Bq"], ["Bq"])
        pwj = A.alloc([5, 2, 16], F32)
        sq1 = A.alloc([16], F32)
        sq2 = A.alloc([16], F32)
        P.op("dve", lambda h: h.tensor_copy(out=pwj[:, 0, 0, :], in_=AA1[:, 0, :]), reads=["AA1"], writes=[("pwj", 0)])
        P.op("dve", lambda h: h.tensor_copy(out=pwj[:, 0, 1, :], in_=AA2[:, 1, :]), reads=["AA2"], writes=[("pwj", 0)])
        for j in range(1, 5):
            r_, i_ = pwj[:, j - 1, 0, :], pwj[:, j - 1, 1, :]
            tt("dve", sq1, r_, r_, ALU.mult, [("pwj", j - 1)], ["sq1"])
            tt("dve", sq2, i_, i_, ALU.mult, [("pwj", j - 1)], ["sq2"])
            tt("dve", pwj[:, j, 0, :], sq1, sq2, ALU.subtract, ["sq1", "sq2"], [("pwj", j)])
            tt("dve", sq1, r_, i_, ALU.mult, [("pwj", j - 1)], ["sq1"])
            ts("dve", pwj[:, j, 1, :], sq1, 2.0, None, ALU.mult, None, ["sq1"], [("pwj", j)])
        BB1 = A.alloc([2, 16], F32)
        BB2 = A.alloc([2, 16], F32)
        for ri in range(2):
            P.op("dve", lambda h, ri=ri: h.tensor_copy(out=BB1[:, ri, :], in_=pwj[:, 4, 0, :]), reads=[("pwj", 4)], writes=["BB1"])
        ts("dve", BB2[:, 0, :], pwj[:, 4, 1, :], -1.0, None, ALU.mult, None, [("pwj", 4)], ["BB2"])
        P.op("dve", lambda h: h.tensor_copy(out=BB2[:, 1, :], in_=pwj[:, 4, 1, :]), reads=[("pwj", 4)], writes=["BB2"])
        Sb = A.alloc([2, 16, 17], F32)
        P.op("dve", lambda h: h.memset(Sb[:, :, :, 0:1], 0.0), writes=["Sb"])
        s2a = A.alloc([2, 16], F32)
        s2b = A.alloc([2, 16], F32)
        for s_ in range(15):
            tt("dve", s2a, BB1, Sb[:, :, :, s_], ALU.mult, ["Sb", "BB1"], ["s2a"])
            tt("dve", s2b[:, 0, :], BB2[:, 0, :], Sb[:, 1, :, s_], ALU.mult, ["Sb", "BB2"], ["s2b"])
            tt("dve", s2b[:, 1, :], BB2[:, 1, :], Sb[:, 0, :, s_], ALU.mult, ["Sb", "BB2"], ["s2b"])
            tt("dve", s2a, s2a, s2b, ALU.add, ["s2a", "s2b"], ["s2a"])
            tt("dve", Sb[:, :, :, s_ + 1], s2a, Bq[:, :, :, s_, 15], ALU.add, ["s2a", "Bq"], ["Sb"])
        Ap = A.alloc([2, 16, 16], F32)
        d1 = A.alloc([16, 8], F32)
        d2 = A.alloc([16, 8], F32)
        P.op("dve", lambda h: h.tensor_copy(out=Ap[:, :, :, 0], in_=pwj[:, 0]), reads=[("pwj", 0)], writes=["Ap"])
        for j in range(4):
            n = 1 << j
            pr = pwj[:, j, 0, :].unsqueeze(2).to_broadcast([128, 16, n])
            pi = pwj[:, j, 1, :].unsqueeze(2).to_broadcast([128, 16, n])
            o_r, o_i = Ap[:, 0, :, 0:n], Ap[:, 1, :, 0:n]
            n_r, n_i = Ap[:, 0, :, n:2 * n], Ap[:, 1, :, n:2 * n]
            rd = ["Ap", ("pwj", j)]
            tt("dve", d1[:, :, 0:n], o_r, pr, ALU.mult, rd, ["d1"])
            tt("dve", d2[:, :, 0:n], o_i, pi, ALU.mult, rd, ["d2"])
            tt("dve", n_r, d1[:, :, 0:n], d2[:, :, 0:n], ALU.subtract, ["d1", "d2", "Ap"], ["Ap"])
            tt("dve", d1[:, :, 0:n], o_r, pi, ALU.mult, rd, ["d1"])
            tt("dve", d2[:, :, 0:n], o_i, pr, ALU.mult, rd, ["d2"])
            tt("dve", n_i, d1[:, :, 0:n], d2[:, :, 0:n], ALU.add, ["d1", "d2", "Ap"], ["Ap"])
        tA = A.alloc([16, 16, 16], F32)
        tB = A.alloc([16, 16, 16], F32)
        Apr_b = Ap[:, 0].unsqueeze(2).to_broadcast([128, 16, 16, 16])
        Api_b = Ap[:, 1].unsqueeze(2).to_broadcast([128, 16, 16, 16])
        Sr_b = Sb[:, 0, :, 0:16].unsqueeze(3).to_broadcast([128, 16, 16, 16])
        Si_b = Sb[:, 1, :, 0:16].unsqueeze(3).to_broadcast([128, 16, 16, 16])
        tt("dve", tA, Apr_b, Sr_b, ALU.mult, ["Ap", "Sb"], ["tA"])
        tt("dve", tB, Api_b, Si_b, ALU.mult, ["Ap", "Sb"], ["tB"])
        tt("dve", tA, tA, tB, ALU.subtract, ["tA", "tB"], ["tA"])
        tt("dve", Bq[:, 0], Bq[:, 0], tA, ALU.add, ["tA", "Bq"], ["Bq"])
        tt("dve", tA, Apr_b, Si_b, ALU.mult, ["Ap", "Sb"], ["tA"])
        tt("dve", tB, Api_b, Sr_b, ALU.mult, ["Ap", "Sb"], ["tB"])
        tt("dve", tA, tA, tB, ALU.add, ["tA", "tB"], ["tA"])
        tt("dve", Bq[:, 1], Bq[:, 1], tA, ALU.add, ["tA", "Bq"], ["Bpc"])
        tap("Bp", Bp, [128, 2, 16, 257], ["Bpc"])
        Xsel = A.alloc([2, 16, 128], F32)
        Xodd = A.alloc([2, 16, 128], F32)
        Xsb = A.alloc([2, 16, 128], BF16)
        Bv = Bp[:, :, :, 0:256].rearrange("p r g (m two c) -> p r g m two c", two=2, c=8)
        for ri in range(2):
            xs = Xsel[:, ri].rearrange("p g (m c) -> p g m c", c=8)
            ts("dve", xs, Bv[:, ri, :, :, 0, :], rflag[:, 0:1], None, ALU.mult, None, ["Bpc", "rflag"], [("Xsel", ri)])
            xo_ = Xodd[:, ri].rearrange("p g (m c) -> p g m c", c=8)
            ts("dve", xo_, Bv[:, ri, :, :, 1, :], rflag[:, 1:2], None, ALU.mult, None, ["Bpc", "rflag"], [("Xodd", ri)])
            tt("dve", Xsel[:, ri], Xsel[:, ri], Xodd[:, ri], ALU.add, [("Xsel", ri), ("Xodd", ri)], [("Xsel", ri)])
        P.op("dve", lambda h: h.tensor_copy(out=Xsb, in_=Xsel), reads=[("Xsel", 0), ("Xsel", 1)], writes=["Xsb"])
        P.op("act", lambda h: h.copy(out=XownS[0:64, 0:16, :], in_=Xsb[0:64, 0]), reads=["Xsb"], writes=["XownS_a"])
        P.op("act", lambda h: h.copy(out=XownS[64:128, 16:32, :], in_=Xsb[64:128, 1]), reads=["Xsb"], writes=["XownS_b"])
        P.dma("sp", XownS[64:128, 0:16, :], Xsb[0:64, 1], reads=["Xsb"], writes=["XownS_c"])
        P.dma("sp", XownS[0:64, 16:32, :], Xsb[64:128, 0], reads=["Xsb"], writes=["XownS_d"])
        Xtoks = ["XownS_a", "XownS_b", "XownS_c", "XownS_d"]
        tap("XownS", XownS, [128, 32, 128], Xtoks, BF16)
        P.barrier()
        A.release()
        xTo = A.alloc([8, 2048], BF16, top=True)
        hi_xTo = A.hi
        ygT = A.alloc([4, 2048], BF16, top=True)
        hi_persist = A.hi
        W3 = A.alloc([32, 16, 32], BF16, top=True)
        W3b = A.alloc([4, 2, 16, 64], BF16, top=True)
        P.op("pool", lambda h: h.memset(W3, 0.0), writes=["W3"])
        P.op("pool", lambda h: h.memset(W3b, 0.0), writes=["W3b"])
        W3v = W3.rearrange("p (gp e) j (f c) -> p gp e j f c", e=2, f=2)
        Csv = Cs.rearrange("p j (gp e) c -> p gp e j c", e=2)
        for e in range(2):
            P.op("dve", lambda h, e=e: h.tensor_copy(out=W3v[:, :, e, :, e, :], in_=Csv[:, :, e, 1:17, :]), reads=["W3"], writes=["W3"])
        for T in range(4):
            P.op("dve", lambda h, T=T: h.tensor_copy(out=W3b[:, T, :, :, 32:64], in_=W3[:, T * 8 + 6:T * 8 + 8, :, :]), reads=["W3", "W3b"], writes=["W3b"])
        A.mark()
        xin_bufs = [A.alloc([4, 1024], BF16)]
        uTo = A.alloc([4, 16, 128], BF16)
        for blk in range(4):
            load_xT_block(x_own[blk * 512:(blk + 1) * 512, :], xTo, blk * 512, "xTo", xin_bufs, blk)
            for T in range(4):
                b_ = nps()
                for dt in range(8):
                    mm(ps[b_], w_u[:, dt, T * 128:(T + 1) * 128], xTo[:, dt, blk * 512:(blk + 1) * 512], dt == 0, dt == 7,
                       wutoks + [("xTo", blk * 4 + t) for t in range(4)], ["ps%d" % b_])
                evac(uTo[:, T, :, blk * 32:(blk + 1) * 32], ps[b_].rearrange("p (c j) -> p j c", j=16), ["ps%d" % b_], [("uTo", T)])
        yT = A.alloc([2, 2048], F32)
        gt = A.alloc([2048], F32)
        for T in range(4):
            yb = T % 2
            for j in range(16):
                b_ = nps()
                for i in range(j + 1):
                    mm(ps[b_][:, 0:128], KB[:, T, j - i, :], uTo[:, T, i, :], i == 0, False, [("KB", T), ("uTo", T)], ["ps%d" % b_])
                for gl in range(8):
                    g = T * 8 + gl
                    if gl < 6:
                        c0 = 32 * (gl // 2)
                        mm(ps[b_][c0:c0 + 32, 0:128], W3[:, g, j, :], XownS[:, g, :], False, False, ["W3"] + Xtoks, ["ps%d" % b_])
                    else:
                        mm(ps[b_][64:128, 0:128], W3b[:, T, gl - 6, j, :], XownS[:, g, :], False, gl == 7, ["W3b"] + Xtoks, ["ps%d" % b_])
                stt(yT[:, yb, j::16], uTo[:, T, j, :], sd[:, T:T + 1], ps[b_][:, 0:128], ALU.mult, ALU.add,
                    ["ps%d" % b_, "sd", ("uTo", T)], [("yT", yb)])
            if T == 0:
                tap("yT", yT[:, 0, :], [128, 2048], [("yT", 0)])
            y_ = yT[:, yb, :]
            tt("dve", gt, y_, y_, ALU.mult, [("yT", yb)], ["gt"])
            ts("dve", gt, gt, 0.044715, 1.0, ALU.mult, ALU.add, ["gt"], ["gt"])
            tt("dve", gt, gt, y_, ALU.mult, ["gt", ("yT", yb)], ["gt"])
            act(gt, gt, AF.Sigmoid, ["gt"], ["gt"], scale=1.5957691216057308)
            tt("dve", ygT[:, T, :], gt, y_, ALU.mult, ["gt", ("yT", yb)], [("ygT", T)])
        tap("ygT", ygT, [128, 4, 2048], [("ygT", T) for T in range(4)], BF16)
        P.barrier()
        A.release()
        A.release()
        A.hi = hi_persist
        if STAGE <= 1:
            P.dma("sp", out_d[0:128, 0:64], identf[:, 0:64], reads=["identf"], writes=["out"])
            P.barrier()
            P.emit()
            return nc, dbg


        attnT = A.alloc([4, 2048], BF16, top=True)
        A.mark()
        ckvTf = A.alloc([2, 4096], BF16)
        kropeT = A.alloc([4096], BF16)
        invf = A.alloc([1], F32)
        P.dma("sp", invf, invf_in, writes=["invf"])
        RP = slice(64, 96)
        sqj = A.alloc([256], F32)
        ssq = A.alloc([1], F32)
        cn = A.alloc([256], BF16)
        gkv = A.alloc([256], F32)
        gq = A.alloc([256], F32)
        P.dma("sp", gkv, kv_norm_g.partition_broadcast(128), writes=["gkv"])
        P.dma("sp", gq, q_norm_g.partition_broadcast(128), writes=["gq"])

        rp_posi = A.alloc([512], I32)
        rp_posf = A.alloc([512], F32)
        rp_t1 = A.alloc([512], F32)
        rp_t2 = A.alloc([512], F32)

        def rope_tables(pos_d, c0, n, cs_o, sn_o, nm):
            posi = rp_posi
            posf = rp_posf
            P.dma("sp", posi[RP], pos_d[c0:c0 + n].partition_broadcast(32), writes=["posi"])
            P.op("dve", lambda h: h.tensor_copy(out=posf[RP], in_=posi[RP]), reads=["posi"], writes=["posf"])
            ts("dve", posf[RP], posf[RP], invf[RP, 0:1], 1.0 / TWO_PI, ALU.mult, ALU.mult, ["posf", "invf"], ["posf"])
            sincos("dve", posf[RP], n, (64, 96), ["posf"], sn_o, cs_o, "rope", tmps=(rp_t1, rp_t2))

        def rms_to_T(ps_ap, ps_tok, g_ap, gtok, dstT, col0, tokw):
            act(sqj, ps_ap, AF.Square, [ps_tok], ["sqj"])
            P.op("dve", lambda h: h.tensor_reduce(out=ssq, in_=sqj, axis=AX.X, op=ALU.add), reads=["sqj"], writes=["ssq"])
            ts("dve", ssq, ssq, 1.0 / 256.0, 1e-6, ALU.mult, ALU.add, ["ssq"], ["ssq"])
            P.op("act", lambda h: h.sqrt(out=ssq, in_=ssq), reads=["ssq"], writes=["ssq"])
            P.op("dve", lambda h: h.reciprocal(out=ssq, in_=ssq), reads=["ssq"], writes=["ssq"])
            stt(cn, ps_ap, ssq[:, 0:1], g_ap, ALU.mult, ALU.mult, [ps_tok, "ssq", gtok], ["cn"])
            b2 = nps()
            for jt in range(2):
                tr(psb[b2][:, jt * 128:(jt + 1) * 128], cn[:, jt * 128:(jt + 1) * 128], identb, ["cn", "identb"], ["ps%d" % b2])
            evac(dstT[:, :, col0:col0 + 128], psb[b2][:, 0:256].rearrange("p (a b) -> p a b", a=2), ["ps%d" % b2], [tokw])

        A.mark()
        w_kv = A.alloc([8, 288], BF16)
        load_w_bf16(w_kv, w_in[:, 256:544], 8, "w_kv")
        wkvtoks = [("w_kv", k) for k in range(8)]
        w_krr = A.alloc([8, 32], BF16)
        P.op("act", lambda h: h.mul(out=w_krr[:, :, 0:16], in_=w_kv[:, :, 272:288], mul=-1.0), reads=wkvtoks, writes=["w_krr"])
        P.op("act", lambda h: h.copy(out=w_krr[:, :, 16:32], in_=w_kv[:, :, 256:272]), reads=wkvtoks, writes=["w_krr"])
        xin_bufs = [A.alloc([4, 1024], BF16)]
        xTblk = [A.alloc([8, 512], BF16) for _ in range(2)]
        krt = A.alloc([2, 512], F32)
        csb = A.alloc([512], F32)
        snb = A.alloc([512], F32)
        for blk in range(8):
            xt = xTblk[blk % 2]
            tb = "xTb%d" % (blk % 2)
            xtoks = [(tb, t) for t in range(4)]
            load_xT_block(x_full[blk * 512:(blk + 1) * 512, :], xt, 0, tb, xin_bufs, blk)
            for t in range(4):
                b_ = nps()
                for dt in range(8):
                    mm(ps[b_][:, 0:256], xt[:, dt, t * 128:(t + 1) * 128], w_kv[:, dt, 0:256], dt == 0, dt == 7, wkvtoks + xtoks, ["ps%d" % b_])
                rms_to_T(ps[b_][:, 0:256], "ps%d" % b_, gkv, "gkv", ckvTf, blk * 512 + t * 128, ("ckvT", blk))
            rope_tables(pos_full, blk * 512, 512, csb[RP], snb[RP], "K")
            ba, bb = nps(), nps()
            for dt in range(8):
                mm(ps[ba][RP, :], w_kv[:, dt, 256:288], xt[:, dt, :], dt == 0, dt == 7, wkvtoks + xtoks, ["ps%d" % ba])
            for dt in range(8):
                mm(ps[bb][RP, :], w_krr[:, dt, :], xt[:, dt, :], dt == 0, dt == 7, ["w_krr"] + xtoks, ["ps%d" % bb])
            tt("dve", krt[RP, 0, :], ps[ba][RP, :], csb[RP], ALU.mult, ["ps%d" % ba, "ropec"], ["krt0"])
            tt("dve", krt[RP, 1, :], ps[bb][RP, :], snb[RP], ALU.mult, ["ps%d" % bb, "ropes"], ["krt1"])
            tt("dve", kropeT[RP, blk * 512:(blk + 1) * 512], krt[RP, 0, :], krt[RP, 1, :], ALU.add, ["krt0", "krt1"], [("krope", blk)])
        tap("ckvTf", ckvTf, [128, 2, 4096], [("ckvT", b) for b in range(8)], BF16)
        P.barrier()
        A.release()
        KT = A.alloc([4, 4096], BF16)
        Vp = A.alloc([32, 4, 65], BF16)
        cqT = A.alloc([2, 2048], BF16)
        maskb = A.alloc([2, 128], BF16)
        P.dma("pool", maskb.rearrange("p a b -> p (a b)"), masks_in, writes=["maskb"])
        P.op("dve", lambda h: h.memset(Vp[:, :, :, 64:65], 1.0), writes=["Vp1"])
        cosQ = A.alloc([2048], BF16)
        sinQ = A.alloc([2048], BF16)
        for q4 in range(4):
            rope_tables(pos_own, q4 * 512, 512, cosQ[RP, q4 * 512:(q4 + 1) * 512], sinQ[RP, q4 * 512:(q4 + 1) * 512], "Q")
        w_ukv_s = A.alloc([2, 1024], BF16)
        load_w_bf16(w_ukv_s, w_ukv, 2, "w_ukv")
        wukvtoks = [("w_ukv", k) for k in range(2)]
        w_uk_v = w_ukv_s.rearrange("p k (h c) -> p k h c", c=128)
        A.mark()
        w_q = A.alloc([8, 256], BF16)
        load_w_bf16(w_q, w_in[:, 0:256], 8, "w_q")
        wqtoks = [("w_q", k) for k in range(8)]
        for t in range(16):
            b_ = nps()
            for dt in range(8):
                mm(ps[b_][:, 0:256], xTo[:, dt, t * 128:(t + 1) * 128], w_q[:, dt, :], dt == 0, dt == 7, wqtoks + [("xTo", t)], ["ps%d" % b_])
            rms_to_T(ps[b_][:, 0:256], "ps%d" % b_, gq, "gq", cqT, t * 128, ("cqT", t))
        P.barrier()
        A.release()
        w_uq_s = A.alloc([2, 768], BF16)
        load_w_bf16(w_uq_s, w_uq, 2, "w_uq")
        wuqtoks = [("w_uq", k) for k in range(2)]
        w_uq_v = w_uq_s.rearrange("p k (h c) -> p k h c", c=96)
        w_uqr = A.alloc([2, 8, 32], BF16)
        P.op("act", lambda h: h.mul(out=w_uqr[:, :, :, 0:16], in_=w_uq_v[:, :, :, 80:96], mul=-1.0), reads=wuqtoks, writes=["w_uqr"])
        P.op("act", lambda h: h.copy(out=w_uqr[:, :, :, 16:32], in_=w_uq_v[:, :, :, 64:80]), reads=wuqtoks, writes=["w_uqr"])
        QTb = [A.alloc([2048], BF16) for _ in range(2)]
        attn_tok = A.alloc([16, 512], BF16)
        PTb = [A.alloc([512], BF16) for _ in range(4)]
        pt_rr_box = [0]
        Osb = A.alloc([4, 65], F32)
        rinv = A.alloc([4], F32)
        cqtoks = [("cqT", t) for t in range(16)]
        SCALE = 96.0 ** -0.5
        sb_rr = [0]

        def sbank():
            i = 2 + sb_rr[0] % 6
            sb_rr[0] += 1
            return i

        def build_QT(h_):
            QT = QTb[h_ % 2]
            qtok = "QT%d" % (h_ % 2)
            for blk in range(4):
                cols = slice(blk * 512, (blk + 1) * 512)
                ba, bb = sbank(), sbank()
                for jt in range(2):
                    mm(ps[ba][0:96, :], w_uq_v[:, jt, h_, :], cqT[:, jt, cols], jt == 0, jt == 1, wuqtoks + cqtoks, ["ps%d" % ba])
                for jt in range(2):
                    mm(ps[bb][RP, :], w_uqr[:, jt, h_, :], cqT[:, jt, cols], jt == 0, jt == 1, ["w_uqr"] + cqtoks, ["ps%d" % bb])
                evac(QT[0:64, cols], ps[ba][0:64, :], ["ps%d" % ba], [(qtok, "n", blk)], eng="act")
                tt("dve", rp_t1[RP], ps[ba][RP, :], cosQ[RP, cols], ALU.mult, ["ps%d" % ba, "ropec"], ["qrt0"])
                tt("dve", rp_t2[RP], ps[bb][RP, :], sinQ[RP, cols], ALU.mult, ["ps%d" % bb, "ropes"], ["qrt1"])
                tt("dve", QT[RP, cols], rp_t1[RP], rp_t2[RP], ALU.add, ["qrt0", "qrt1"], [(qtok, "r", blk)])

        def build_KV(h0):
            for h2 in range(h0, h0 + 4):
                for blk in range(8):
                    b_ = sbank()
                    for jt in range(2):
                        mm(ps[b_][0:64, :], w_ukv_s[:, jt, h2 * 128:h2 * 128 + 64], ckvTf[:, jt, blk * 512:(blk + 1) * 512], jt == 0, jt == 1,
                           wukvtoks, ["ps%d" % b_])
                    evac(KT[0:64, h2 - h0, blk * 512:(blk + 1) * 512], ps[b_][0:64, :], ["ps%d" % b_], [("KTn", h2 - h0)])
                P.op("act", lambda h, h2=h2, h0=h0: h.copy(out=KT[RP, h2 - h0, :], in_=kropeT[RP, :]), reads=[], writes=[("KTr", h2 - h0)])
            for kb in range(32):
                b_ = sbank()
                for jt in range(2):
                    mm(ps[b_][:, 0:256], ckvTf[:, jt, kb * 128:(kb + 1) * 128], w_uk_v[:, jt, h0:h0 + 4, 64:128], jt == 0, jt == 1, wukvtoks, ["ps%d" % b_])
                evac(Vp[:, kb, :, 0:64], ps[b_][:, 0:256].rearrange("p (a b) -> p a b", a=4), ["ps%d" % b_], [("Vp", kb)])

        acc_rr = 0
        LOOK = 2
        build_QT(0)
        for h_ in range(8):
            hl = h_ % 4
            if hl == 0:
                build_KV(h_)
                if h_ == 0:
                    tap("KT", KT, [128, 4, 4096], [("KTn", i) for i in range(4)] + [("KTr", i) for i in range(4)], BF16)
                    tap("Vp", Vp, [128, 32, 4, 65], [("Vp", k) for k in range(32)] + ["Vp1"], BF16)
            if h_ + 1 < 8:
                build_QT(h_ + 1)
            QT = QTb[h_ % 2]
            qtok = "QT%d" % (h_ % 2)
            if h_ == 0:
                tap("QT0", QT, [128, 2048], [(qtok, "n", b) for b in range(4)] + [(qtok, "r", b) for b in range(4)], BF16)
            jobs = [(G, kb) for G in range(4) for kb in range(8 * G + 8)]
            st = {}
            bo_of = {}
            for G in range(4):
                bo_of[G] = acc_rr % 2
                acc_rr += 1

            def emit_S(i):
                G, kb = jobs[i]
                bs_ = sbank()
                mm(ps[bs_], KT[0:96, hl, kb * 128:(kb + 1) * 128], QT[0:96, G * 512:(G + 1) * 512], True, True,
                   [("KTn", hl), ("KTr", hl), (qtok, "n", G), (qtok, "r", G)], ["ps%d" % bs_])
                st[i] = (bs_, pt_rr_box[0] % 4)
                pt_rr_box[0] += 1

            def emit_rest(i):
                G, kb = jobs[i]
                bs_, pi = st.pop(i)
                bo = bo_of[G]
                nkb = 8 * G + 8
                PT = PTb[pi]
                ptok = "PT%d" % pi
                act(PT, ps[bs_], AF.Exp, ["ps%d" % bs_], [ptok], scale=SCALE)
                j = kb - 8 * G
                for qs in range(4):
                    if j > 2 * qs + 1:
                        continue
                    if (j == 2 * qs or j == 2 * qs + 1) and not NOMASK:
                        tt("dve", PT[:, qs * 128:(qs + 1) * 128], PT[:, qs * 128:(qs + 1) * 128], maskb[:, j - 2 * qs, :], ALU.mult, [ptok, "maskb"], [ptok])
                    last = (kb == nkb - 1 and qs == 3)
                    mm(ps[bo][:, qs * 65:(qs + 1) * 65], PT[:, qs * 128:(qs + 1) * 128], Vp[:, kb, hl, :], (kb == 0 and qs == 0), last,
                       [ptok, ("Vp", kb), "Vp1"], ["ps%d" % bo])
                if kb == nkb - 1:
                    evac(Osb, ps[bo][:, 0:260].rearrange("p (a b) -> p a b", a=4), ["ps%d" % bo], ["Osb"], eng="act")
                    P.op("dve", lambda h: h.reciprocal(out=rinv, in_=Osb[:, :, 64]), reads=["Osb"], writes=["rinv"])
                    tt("dve", attn_tok[:, G * 4:(G + 1) * 4, h_ * 64:(h_ + 1) * 64], Osb[:, :, 0:64], rinv.unsqueeze(2).to_broadcast([128, 4, 64]), ALU.mult,
                       ["Osb", "rinv"], [("attn_tok", G)])

            for i in range(min(LOOK, len(jobs))):
                emit_S(i)
            for i in range(len(jobs)):
                if i + LOOK < len(jobs):
                    emit_S(i + LOOK)
                emit_rest(i)
        for t in range(16):
            b_ = nps()
            for kt in range(4):
                tr(psb[b_][:, kt * 128:(kt + 1) * 128], attn_tok[:, t, kt * 128:(kt + 1) * 128], identb, [("attn_tok", t // 4), "identb"], ["ps%d" % b_])
            evac(attnT[:, :, t * 128:(t + 1) * 128], psb[b_][:, 0:512].rearrange("p (a b) -> p a b", a=4), ["ps%d" % b_], [("attnT", t)])
        tap("attnT", attnT, [128, 4, 2048], [("attnT", t) for t in range(16)], BF16)
        P.barrier()
        A.release()
        if STAGE <= 2:
            P.dma("sp", out_d[0:128, 0:64], identf[:, 0:64], reads=["identf"], writes=["out"])
            P.barrier()
            P.emit()
            return nc, dbg


        A.mark()
        w_g = A.alloc([8, 2048], BF16)
        P.dma("pool", w_g[:, :, 0:1024], w_in[:, 1056:2080].rearrange("(k p) c -> p k c", p=128), writes=[("w_g", 0)])
        P.dma("pool", w_g[:, :, 1024:2048], w_in[:, 2080:3104].rearrange("(k p) c -> p k c", p=128), writes=[("w_g", 1)])
        w_mo = A.alloc([4, 1024], BF16)
        P.dma("pool", w_mo, w_mla_o.rearrange("(k p) c -> p k c", p=128), writes=["w_mo"])
        w_glu = A.alloc([4, 2048], BF16)
        for k in range(4):
            P.dma("pool", w_glu[:, k, :], w_s5_glu[k * 128:(k + 1) * 128, :], writes=[("w_glu", k)])
        wglutoks = [("w_glu", k) for k in range(4)]
        w_o = A.alloc([8, 1024], BF16)
        P.dma("pool", w_o, w_out.rearrange("(k p) c -> p k c", p=128), writes=["w_o"])
        g1, b1 = ln_bcast_load("ln1")
        m1 = A.alloc([1024], F32)
        ys5 = A.alloc([1024], F32)
        sgt = [A.alloc([512], F32) for _ in range(2)]
        mbfs = [A.alloc([1024], BF16) for _ in range(2)]
        mTs = [A.alloc([8, 128], BF16) for _ in range(2)]
        xres = [A.alloc([1024], F32) for _ in range(3)]
        xs1 = [A.alloc([1024], F32) for _ in range(3)]
        xb1s = [A.alloc([1024], BF16) for _ in range(3)]
        st6 = A.alloc([4, 12], F32)
        mv = A.alloc([4, 2], F32)
        rstd = A.alloc([4, 1], F32)
        lnt = (st6, mv, rstd)

        ln_h = {}

        def merge_A(t):
            tc_ = slice(t * 128, (t + 1) * 128)
            xr = xres[t % 3]
            xrt = "xres%d" % (t % 3)
            mbf = mbfs[t % 2]
            P.dma("sp", xr, x_own[tc_, :], writes=[xrt])
            for hf in range(2):
                hc = slice(hf * 512, (hf + 1) * 512)
                by = nps()
                for kt in range(4):
                    mm(ps[by], attnT[:, kt, tc_], w_mo[:, kt, hc], kt == 0, kt == 3, [("attnT", t), "w_mo"], ["ps%d" % by])
                bg = nps()
                for dt in range(8):
                    mm(ps[bg], xTo[:, dt, tc_], w_g[:, dt, hc], dt == 0, dt == 7, [("xTo", t), ("w_g", 0)], ["ps%d" % bg])
                act(m1[:, hc], ps[bg], AF.Sigmoid, ["ps%d" % bg], [("m1", hf)])
                tt("dve", m1[:, hc], m1[:, hc], ps[by], ALU.mult, [("m1", hf), "ps%d" % by], [("m1", hf)])
                bv, bgt = nps(), nps()
                for T in range(4):
                    mm(ps[bv], ygT[:, T, tc_], w_glu[:, T, hc], T == 0, T == 3, [("ygT", T)] + wglutoks, ["ps%d" % bv])
                for T in range(4):
                    mm(ps[bgt], ygT[:, T, tc_], w_glu[:, T, 1024 + hf * 512:1024 + (hf + 1) * 512], T == 0, T == 3, [("ygT", T)] + wglutoks, ["ps%d" % bgt])
                sg_ = sgt[hf]
                act(sg_, ps[bgt], AF.Sigmoid, ["ps%d" % bgt], [("sgt", hf)])
                tt("dve", ys5[:, hc], sg_, ps[bv], ALU.mult, [("sgt", hf), "ps%d" % bv], [("ys5", hf)])
                bg2 = nps()
                for dt in range(8):
                    mm(ps[bg2], xTo[:, dt, tc_], w_g[:, dt, 1024 + hf * 512:1024 + (hf + 1) * 512], dt == 0, dt == 7, [("xTo", t), ("w_g", 1)], ["ps%d" % bg2])
                act(sg_, ps[bg2], AF.Sigmoid, ["ps%d" % bg2], [("sgt", hf)])
                tt("dve", ys5[:, hc], ys5[:, hc], sg_, ALU.mult, [("sgt", hf), ("ys5", hf)], [("ys5", hf)])
                tt("dve", mbf[:, hc], m1[:, hc], ys5[:, hc], ALU.add, [("m1", hf), ("ys5", hf)], [("mbf", t % 2, hf)])

        def merge_B(t):
            tc_ = slice(t * 128, (t + 1) * 128)
            xr = xres[t % 3]
            xrt = "xres%d" % (t % 3)
            mbf = mbfs[t % 2]
            mT = mTs[t % 2]
            mTt = "mT%d" % (t % 2)
            xb1 = xb1s[t % 3]
            xbt = "xb1_%d" % (t % 3)
            bt = nps()
            for dt in range(8):
                tr(psb[bt][:, dt * 128:(dt + 1) * 128], mbf[:, dt * 128:(dt + 1) * 128], identb, [("mbf", t % 2, 0), ("mbf", t % 2, 1), "identb"], ["ps%d" % bt])
            evac(mT, psb[bt].rearrange("p (a b) -> p a b", a=8), ["ps%d" % bt], [mTt])
            xs = xs1[t % 3]
            xst = "xs1_%d" % (t % 3)
            for hf in range(2):
                hc = slice(hf * 512, (hf + 1) * 512)
                bo_ = nps()
                for dt in range(8):
                    mm(ps[bo_], mT[:, dt, :], w_o[:, dt, hc], dt == 0, dt == 7, [mTt, "w_o"], ["ps%d" % bo_])
                stt(xs[:, hc], xr[:, hc], ALPHA, ps[bo_], ALU.mult, ALU.add, [xrt, "ps%d" % bo_], [(xst, hf)])
            ln_h[t] = layer_norm_a(xs, "ln1", [(xst, 0), (xst, 1)], lnt)

        def merge_B2a(t):
            tc_ = slice(t * 128, (t + 1) * 128)
            xb1 = xb1s[t % 3]
            xbt = "xb1_%d" % (t % 3)
            xs = xs1[t % 3]
            xst = "xs1_%d" % (t % 3)
            xh = [(xst, 0), (xst, 1)]
            layer_norm_b(ln_h[t], xs, xs, g1, b1, xh, xh)
            if t == 0:
                tap("x1_0", xs, [128, 1024], xh)
            P.dma(ST_Q, x1_d[tc_, :], xs, reads=xh, writes=[("x1d", t)])
            P.op("act", lambda h, xs=xs, xb1=xb1: h.copy(out=xb1, in_=xs), reads=xh, writes=[xbt])

        def merge_B2b(t):
            tc_ = slice(t * 128, (t + 1) * 128)
            xb1 = xb1s[t % 3]
            xbt = "xb1_%d" % (t % 3)
            bt = nps()
            for dt in range(8):
                tr(psb[bt][:, dt * 128:(dt + 1) * 128], xb1[:, dt * 128:(dt + 1) * 128], identb, [xbt, "identb"], ["ps%d" % bt])
            evac(xTo[:, :, tc_], psb[bt].rearrange("p (a b) -> p a b", a=8), ["ps%d" % bt], [("xTo", t)])

        merge_A(0)
        for t in range(16):
            if t >= 1:
                merge_B2a(t - 1)
            if t + 1 < 16:
                merge_A(t + 1)
            if t >= 1:
                merge_B2b(t - 1)
            merge_B(t)
        merge_B2a(15)
        merge_B2b(15)
        P.barrier()
        A.release()
        A.hi = hi_xTo
        if STAGE <= 3:
            P.dma("sp", out_d[0:128, 0:64], identf[:, 0:64], reads=["identf"], writes=["out"])
            P.barrier()
            P.emit()
            return nc, dbg


        gates = A.alloc([16, 64], F32, top=True)
        U32 = mybir.dt.uint32
        slotu = A.alloc([16, 8], U32, top=True)
        twv = A.alloc([16, 8, 2], F32, top=True)
        ovf = A.alloc([1], F32, top=True)
        hi_gates = A.hi
        A.mark()
        w_xq_s = A.alloc([8, 512], BF16)
        P.dma("pool", w_xq_s, w_xq.rearrange("(k p) c -> p k c", p=128), writes=["w_xq"])
        w_xo_s = A.alloc([4, 1024], BF16)
        P.dma("pool", w_xo_s, w_xo.rearrange("(k p) c -> p k c", p=128), writes=["w_xo"])
        w_rt = A.alloc([8, 64], F32)
        P.dma("sp", w_rt, w_router.rearrange("(k p) c -> p k c", p=128), writes=["w_rt"])
        rbias = A.alloc([64], F32)
        P.dma("sp", rbias, router_bias.partition_broadcast(128), writes=["rbias"])
        g2, b2 = ln_bcast_load("ln2")
        KxT = A.alloc([4, 256], BF16)
        Vx = A.alloc([2, 4, 129], BF16)
        P.op("dve", lambda h: h.memset(Vx[:, :, :, 128:129], 1.0), writes=["Vx1"])
        st6 = A.alloc([4, 12], F32)
        mv = A.alloc([4, 2], F32)
        rstd = A.alloc([4, 1], F32)
        lnt = (st6, mv, rstd)
        A.mark()
        w_xkv_s = A.alloc([8, 1024], BF16)
        P.dma("pool", w_xkv_s, w_xkv.rearrange("(k p) c -> p k c", p=128), writes=["w_xkv"])
        gm, bm = ln_bcast_load("mem_ln")
        memf = A.alloc([2, 1024], F32)
        memb = A.alloc([2, 1024], BF16)
        memT = A.alloc([8, 256], BF16)
        for mt in range(2):
            P.dma("sp", memf[:, mt, :], mem_in[mt * 128:(mt + 1) * 128, :], writes=[("memf", mt)])
            layer_norm(memf[:, mt, :], memf[:, mt, :], gm, bm, "mem_ln", [("memf", mt)], [("memf", mt)], lnt)
            P.op("act", lambda h, mt=mt: h.copy(out=memb[:, mt, :], in_=memf[:, mt, :]), reads=[("memf", mt)], writes=[("memb", mt)])
            bt = nps()
            for dt in range(8):
                tr(psb[bt][:, dt * 128:(dt + 1) * 128], memb[:, mt, dt * 128:(dt + 1) * 128], identb, [("memb", mt), "identb"], ["ps%d" % bt])
            evac(memT[:, :, mt * 128:(mt + 1) * 128], psb[bt].rearrange("p (a b) -> p a b", a=8), ["ps%d" % bt], [("memT", mt)])
        mtoks = [("memT", 0), ("memT", 1)]
        for h_ in range(4):
            b_ = nps()
            for dt in range(8):
                mm(ps[b_][:, 0:256], w_xkv_s[:, dt, h_ * 128:(h_ + 1) * 128], memT[:, dt, :], dt == 0, dt == 7, ["w_xkv"] + mtoks, ["ps%d" % b_])
            evac(KxT[:, h_, :], ps[b_][:, 0:256], ["ps%d" % b_], [("KxT", h_)])
        for mt in range(2):
            b_ = nps()
            for dt in range(8):
                mm(ps[b_], memT[:, dt, mt * 128:(mt + 1) * 128], w_xkv_s[:, dt, 512:1024], dt == 0, dt == 7, ["w_xkv"] + mtoks, ["ps%d" % b_])
            evac(Vx[:, mt, :, 0:128], ps[b_].rearrange("p (a b) -> p a b", a=4), ["ps%d" % b_], [("Vx", mt)])
        P.barrier()
        A.release()
        if XA_CUT == 1:
            tap("KxT", KxT, [128, 4, 256], [], BF16)
            tap("Vx", Vx, [128, 2, 4, 129], [], BF16)
            P.barrier(); P.emit(); return nc, dbg
        QxT = [A.alloc([512], BF16) for _ in range(2)]
        PTx = [A.alloc([512], BF16) for _ in range(4)]
        Osx = A.alloc([4, 129], F32)
        rinx = A.alloc([4], F32)
        xo_toks = [A.alloc([4, 512], BF16) for _ in range(2)]
        xoT = A.alloc([4, 128], BF16)
        x1r = [A.alloc([1024], F32) for _ in range(3)]
        xs2 = [A.alloc([1024], F32) for _ in range(3)]
        x2Tl = A.alloc([8, 128], BF16)
        xb_his = [A.alloc([1024], BF16) for _ in range(2)]
        xb_los = [A.alloc([1024], BF16) for _ in range(2)]
        w_rh = A.alloc([8, 64], BF16)
        w_rl = A.alloc([8, 64], BF16)
        P.op("act", lambda h: h.copy(out=w_rh, in_=w_rt), reads=["w_rt"], writes=["w_rhl"])
        tt("dve", w_rl, w_rt, w_rh, ALU.subtract, ["w_rt", "w_rhl"], ["w_rhl"])
        r_sc = A.alloc([64], F32)
        r_sel = A.alloc([64], F32)
        r_eq = A.alloc([64], F32)
        r_sel2 = A.alloc([64], F32)
        r_m1 = A.alloc([8], F32)
        r_m2 = A.alloc([8], F32)
        r_t8 = A.alloc([8], F32)
        r_gm = A.alloc([8], F32)
        r_pen = A.alloc([8], F32)
        r_den = A.alloc([1], F32)
        if sparse:
            P.op("dve", lambda h: h.memset(ovf, 0.0), writes=["ovf"])
            triu = A.alloc([128], BF16)
            P.dma("pool", triu, triu_in, writes=["triu"])
            iota64 = A.alloc([64], F32)
            P.dma("sp", iota64, iota_in, writes=["iota64"])
            dum8 = A.alloc([16, 8], F32)
            P.dma("sp", dum8.rearrange("p a b -> p (a b)"), dum_in, writes=["dum8"])
            tinit = A.alloc([NS // 128, 2], F32)
            P.op("dve", lambda h: h.memset(tinit[:, :, 0:1], 2048.0), writes=["tinit"])
            P.op("dve", lambda h: h.memset(tinit[:, :, 1:2], 0.0), writes=["tinit"])
            P.dma("sp", tbl_d[0:NS, :].rearrange("(p j) c -> p j c", p=128), tinit, reads=["tinit"], writes=["tbl_init"])
            zrow = A.alloc([1024], BF16)
            P.op("dve", lambda h: h.memset(zrow, 0.0), writes=["zrow"])
            P.dma("sp", x2b_d[2048:2049, :], zrow[0:1, :], reads=["zrow"], writes=["x2b_zero"])
            maskb16 = A.alloc([16, 64], BF16)
            v8s = [A.alloc([8], F32) for _ in range(2)]
            i8s = [A.alloc([8], U32) for _ in range(2)]
            i8f = A.alloc([8], F32)
            oh3 = A.alloc([8, 64], F32)
            pos_sbs = [A.alloc([64], F32) for _ in range(2)]
            pos8 = A.alloc([8], F32)
            ov8 = A.alloc([8], F32)
            dlt = A.alloc([8], F32)
            slotf = A.alloc([8], F32)
            tokf = A.alloc([16, 1], F32)
            ts("dve", tokf, dum8[:, :, 0:1], -float(NS), 0.125, ALU.add, ALU.mult, ["dum8"], ["tokf"])

        def slot_a(T):
            q = T % 2
            ts("dve", maskb16[:, T, :], gates[:, T, :], 0.0, None, ALU.is_gt, None, [("gates", T)], [("maskb16", T)])
            bp_ = nps()
            for T2 in range(T + 1):
                mm(ps[bp_][:, 0:64], ones_b if T2 < T else triu, maskb16[:, T2, :], T2 == 0, T2 == T,
                   ["ones_b", "triu", ("maskb16", T2)], ["ps%d" % bp_])
            evac(pos_sbs[q], ps[bp_][:, 0:64], ["ps%d" % bp_], [("pos_sb", q)], eng="act")
            P.op("dve", lambda h: h.max(out=v8s[q], in_=gates[:, T, :]), reads=[("gates", T)], writes=[("v8", q)])
            P.op("dve", lambda h: h.max_index(out=i8s[q], in_max=v8s[q], in_values=gates[:, T, :]), reads=[("v8", q), ("gates", T)], writes=[("i8", q)])

        def slot_b(T):
            q = T % 2
            pos_sb, v8, i8 = pos_sbs[q], v8s[q], i8s[q]
            P.op("dve", lambda h: h.tensor_copy(out=i8f, in_=i8), reads=[("i8", q)], writes=["i8f"])
            tt("dve", oh3, iota64.unsqueeze(1).to_broadcast([128, 8, 64]), i8f.unsqueeze(2).to_broadcast([128, 8, 64]), ALU.is_equal,
               ["iota64", "i8f"], ["oh3"])
            tt("dve", oh3, oh3, pos_sb.unsqueeze(1).to_broadcast([128, 8, 64]), ALU.mult, ["oh3", ("pos_sb", q)], ["oh3"])
            P.op("dve", lambda h: h.tensor_reduce(out=pos8, in_=oh3, axis=AX.X, op=ALU.add), reads=["oh3"], writes=["pos8"])
            ts("dve", ov8, pos8, float(CAP), None, ALU.is_ge, None, ["pos8"], ["ov8"])
            P.op("dve", lambda h: h.tensor_reduce(out=dlt[:, 0:1], in_=ov8, axis=AX.X, op=ALU.max), reads=["ov8"], writes=["dlt"])
            tt("dve", ovf, ovf, dlt[:, 0:1], ALU.max, ["ovf", "dlt"], ["ovf"])
            stt(slotf, i8f, float(CAP), pos8, ALU.mult, ALU.add, ["i8f", "pos8"], ["slotf"])
            tt("dve", dlt, dum8[:, T, :], slotf, ALU.subtract, ["dum8", "slotf"], ["dlt"])
            tt("dve", dlt, dlt, ov8, ALU.mult, ["dlt", "ov8"], ["dlt"])
            tt("dve", slotf, slotf, dlt, ALU.add, ["slotf", "dlt"], ["slotf"])
            P.op("dve", lambda h: h.tensor_copy(out=slotu[:, T, :], in_=slotf), reads=["slotf"], writes=[("slotu", T)])
            P.op("dve", lambda h: h.tensor_copy(out=twv[:, T, :, 0], in_=tokf[:, T, :].to_broadcast([128, 8])), reads=["tokf"], writes=[("twv", T)])
            P.op("dve", lambda h: h.tensor_copy(out=twv[:, T, :, 1], in_=v8), reads=[("v8", q), ("twv", T)], writes=[("twv", T)])
            for k in range(8):
                P.idma(tbl_d, twv[:, T, k, :], slotu[:, T, k:k + 1], gather=False,
                       reads=[("twv", T), ("slotu", T), "tbl_init"], writes=[("tbl", T, k)])

        XS = 128.0 ** -0.5
        ptx_rr = [0]
        xsb_rr = [0]

        def xs_bank():
            i = 4 + xsb_rr[0] % 4
            xsb_rr[0] += 1
            return i

        def xa_emit_Q(blk, h_):
            cols = slice(blk * 512, (blk + 1) * 512)
            xtoks_b = [("xTo", blk * 4 + i) for i in range(4)]
            Qx = QxT[h_ % 2]
            qxt = "QxT%d" % (h_ % 2)
            b_ = xs_bank()
            for dt in range(8):
                mm(ps[b_], w_xq_s[:, dt, h_ * 128:(h_ + 1) * 128], xTo[:, dt, cols], dt == 0, dt == 7, ["w_xq"] + xtoks_b, ["ps%d" % b_])
            evac(Qx, ps[b_], ["ps%d" % b_], [qxt], eng="act")

        def xa_head(blk, h_):
            xo_tok = xo_toks[blk % 2]
            if h_ == 0:
                xa_emit_Q(blk, 0)
            Qx = QxT[h_ % 2]
            qxt = "QxT%d" % (h_ % 2)
            boA, boB = (0, 1) if h_ % 2 == 0 else (2, 3)
            sb = []
            for mt in range(2):
                bs_ = xs_bank()
                mm(ps[bs_], KxT[:, h_, mt * 128:(mt + 1) * 128], Qx, True, True, [("KxT", h_), qxt], ["ps%d" % bs_])
                sb.append(bs_)
            if h_ + 1 < 4:
                xa_emit_Q(blk, h_ + 1)
            for mt in range(2):
                bs_ = sb[mt]
                PT = PTx[ptx_rr[0] % 4]
                ptok = "PTx%d" % (ptx_rr[0] % 4)
                ptx_rr[0] += 1
                act(PT, ps[bs_], AF.Exp, ["ps%d" % bs_], [ptok], scale=XS)
                for qs in range(4):
                    bo_ = boA if qs < 2 else boB
                    c0 = (qs % 2) * 129
                    mm(ps[bo_][:, c0:c0 + 129], PT[:, qs * 128:(qs + 1) * 128], Vx[:, mt, h_, :], (mt == 0 and qs % 2 == 0), (mt == 1 and qs % 2 == 1),
                       [ptok, ("Vx", mt), "Vx1"], ["ps%d" % bo_])
            evac(Osx[:, 0:2, :], ps[boA][:, 0:258].rearrange("p (a b) -> p a b", a=2), ["ps%d" % boA], [("Osx", 0)], eng="act")
            evac(Osx[:, 2:4, :], ps[boB][:, 0:258].rearrange("p (a b) -> p a b", a=2), ["ps%d" % boB], [("Osx", 1)], eng="act")
            P.op("dve", lambda h: h.reciprocal(out=rinx, in_=Osx[:, :, 128]), reads=[("Osx", 0), ("Osx", 1)], writes=["rinx"])
            tt("dve", xo_tok[:, :, h_ * 128:(h_ + 1) * 128], Osx[:, :, 0:128], rinx.unsqueeze(2).to_broadcast([128, 4, 128]), ALU.mult,
               [("Osx", 0), ("Osx", 1), "rinx"], [("xo_tok", blk % 2, h_)])

        ln2_h = {}

        def xa_post1(t):
            if True:
                blk, qs = t // 4, t % 4
                xo_tok = xo_toks[blk % 2]
                tc_ = slice(t * 128, (t + 1) * 128)
                xb_hi, xb_lo = xb_his[t % 2], xb_los[t % 2]
                xbh, xbl = "xb_hi%d" % (t % 2), "xb_lo%d" % (t % 2)
                bt = nps()
                for kt in range(4):
                    tr(psb[bt][:, kt * 128:(kt + 1) * 128], xo_tok[:, qs, kt * 128:(kt + 1) * 128], identb, [("xo_tok", blk % 2, kt), "identb"], ["ps%d" % bt])
                evac(xoT, psb[bt][:, 0:512].rearrange("p (a b) -> p a b", a=4), ["ps%d" % bt], ["xoT"])
                x1t = x1r[t % 3]
                x1tk = "x1r%d" % (t % 3)
                P.dma("sp", x1t, x1_d[tc_, :], reads=[("x1d", t)], writes=[x1tk])
                xs = xs2[t % 3]
                xst = "xs2_%d" % (t % 3)
                for hf in range(2):
                    hc = slice(hf * 512, (hf + 1) * 512)
                    bo_ = nps()
                    for kt in range(4):
                        mm(ps[bo_], xoT[:, kt, :], w_xo_s[:, kt, hc], kt == 0, kt == 3, ["xoT", "w_xo"], ["ps%d" % bo_])
                    stt(xs[:, hc], x1t[:, hc], ALPHA, ps[bo_], ALU.mult, ALU.add, [x1tk, "ps%d" % bo_], [(xst, hf)])
                ln2_h[t] = layer_norm_a(xs, "ln2", [(xst, 0), (xst, 1)], lnt)

        def xa_post2a(t):
            if True:
                tc_ = slice(t * 128, (t + 1) * 128)
                xb_hi, xb_lo = xb_his[t % 2], xb_los[t % 2]
                xbh, xbl = "xb_hi%d" % (t % 2), "xb_lo%d" % (t % 2)
                xs = xs2[t % 3]
                xst = "xs2_%d" % (t % 3)
                xh = [(xst, 0), (xst, 1)]
                layer_norm_b(ln2_h[t], xs, xs, g2, b2, xh, xh, gb_eng="dve")
                if t == 0:
                    tap("x2_0", xs, [128, 1024], xh)
                P.dma(ST_Q, x2_d[tc_, :], xs, reads=xh, writes=[("x2d", t)])
                P.op("act", lambda h, xs=xs: h.copy(out=xb_hi, in_=xs), reads=xh, writes=[xbh])
                if sparse:
                    P.dma("sp", x2b_d[tc_, :], xb_hi, reads=[xbh], writes=[("x2bd", t)])
                tt("dve", xb_lo, xs, xb_hi, ALU.subtract, xh + [xbh], [xbl])
        def xa_post2b(t):
            if True:
                tc_ = slice(t * 128, (t + 1) * 128)
                xb_hi, xb_lo = xb_his[t % 2], xb_los[t % 2]
                xbh, xbl = "xb_hi%d" % (t % 2), "xb_lo%d" % (t % 2)
                bA, bB = nps(), nps()
                for dt in range(8):
                    tr(psb[bA][:, dt * 128:(dt + 1) * 128], xb_hi[:, dt * 128:(dt + 1) * 128], identb, [xbh, "identb"], ["ps%d" % bA])
                evac(xTo[:, :, tc_], psb[bA].rearrange("p (a b) -> p a b", a=8), ["ps%d" % bA], [("xTo", t)])
                for dt in range(8):
                    tr(psb[bB][:, dt * 128:(dt + 1) * 128], xb_lo[:, dt * 128:(dt + 1) * 128], identb, [xbl, "identb"], ["ps%d" % bB])
                evac(x2Tl, psb[bB].rearrange("p (a b) -> p a b", a=8), ["ps%d" % bB], ["x2Tl"])
                br = nps()
                n_ = 0
                for dt in range(8):
                    for (l_, ltoks, w_) in ((xTo[:, dt, tc_], [("xTo", t)], w_rh), (x2Tl[:, dt, :], ["x2Tl"], w_rh), (xTo[:, dt, tc_], [("xTo", t)], w_rl)):
                        mm(ps[br][:, 0:64], l_, w_[:, dt, :], n_ == 0, n_ == 23, ltoks + ["w_rhl"], ["ps%d" % br])
                        n_ += 1
                act(r_sc, ps[br][:, 0:64], AF.Exp, ["ps%d" % br], ["r_sc"], scale=-1.0)
                ts("dve", r_sc, r_sc, 1.0, None, ALU.add, None, ["r_sc"], ["r_sc"])
                P.op("dve", lambda h: h.reciprocal(out=r_sc, in_=r_sc), reads=["r_sc"], writes=["r_sc"])
                tt("dve", r_sel, r_sc, rbias, ALU.add, ["r_sc", "rbias"], ["r_sel"])
                selv = r_sel.rearrange("p (g e) -> p g e", e=8)
                P.op("dve", lambda h: h.tensor_reduce(out=r_m1, in_=selv, axis=AX.X, op=ALU.max), reads=["r_sel"], writes=["r_m1"])
                tt("dve", r_eq.rearrange("p (g e) -> p g e", e=8), selv, r_m1.unsqueeze(2).to_broadcast([128, 8, 8]), ALU.is_equal, ["r_sel", "r_m1"], ["r_eq"])
                stt(r_sel2, r_eq, -1e9, r_sel, ALU.mult, ALU.add, ["r_eq", "r_sel"], ["r_sel2"])
                P.op("dve", lambda h: h.tensor_reduce(out=r_m2, in_=r_sel2.rearrange("p (g e) -> p g e", e=8), axis=AX.X, op=ALU.max), reads=["r_sel2"], writes=["r_m2"])
                tt("dve", r_m1, r_m1, r_m2, ALU.add, ["r_m1", "r_m2"], ["r_gs"])
                P.op("dve", lambda h: h.max(out=r_t8, in_=r_m1), reads=["r_gs"], writes=["r_t8"])
                ts("dve", r_gm, r_m1, r_t8[:, 3:4], None, ALU.is_ge, None, ["r_gs", "r_t8"], ["r_gm"])
                ts("dve", r_pen, r_gm, 1e9, -1e9, ALU.mult, ALU.add, ["r_gm"], ["r_pen"])
                tt("dve", r_sel2.rearrange("p (g e) -> p g e", e=8), selv, r_gm.unsqueeze(2).to_broadcast([128, 8, 8]), ALU.mult, ["r_sel", "r_gm"], ["r_sel2"])
                tt("dve", r_sel2.rearrange("p (g e) -> p g e", e=8), r_sel2.rearrange("p (g e) -> p g e", e=8), r_pen.unsqueeze(2).to_broadcast([128, 8, 8]), ALU.add,
                   ["r_sel2", "r_pen"], ["r_sel2"])
                P.op("dve", lambda h: h.max(out=r_t8, in_=r_sel2), reads=["r_sel2"], writes=["r_t8b"])
                ts("dve", r_eq, r_sel2, r_t8[:, 7:8], None, ALU.is_ge, None, ["r_sel2", "r_t8b"], ["r_eq"])
                tt("dve", r_eq, r_eq, r_sc, ALU.mult, ["r_eq", "r_sc"], ["r_eq"])
                P.op("dve", lambda h: h.tensor_reduce(out=r_den, in_=r_eq, axis=AX.X, op=ALU.add), reads=["r_eq"], writes=["r_den"])
                P.op("dve", lambda h: h.reciprocal(out=r_den, in_=r_den), reads=["r_den"], writes=["r_den"])
                ts("dve", gates[:, t, :], r_eq, r_den[:, 0:1], 2.5, ALU.mult, ALU.mult, ["r_eq", "r_den"], [("gates", t)])
                if sparse:
                    slot_a(t)
                    if t >= 1:
                        slot_b(t - 1)
        for h_ in range(4):
            xa_head(0, h_)
        for blk in range(4):
            for qs in range(4):
                t = blk * 4 + qs
                if t >= 1:
                    xa_post2a(t - 1)
                if blk + 1 < 4:
                    xa_head(blk + 1, qs)
                xa_post1(t)
                if t >= 1:
                    xa_post2b(t - 1)
        xa_post2a(15)
        xa_post2b(15)
        tap("gates", gates, [128, 16, 64], [("gates", t) for t in range(16)])
        if sparse:
            slot_b(15)
            P.dma("sp", ovf_d, ovf, reads=["ovf"], writes=["ovf_d"])
            tap("slotu", slotu, [128, 16, 8], [("slotu", T) for T in range(16)], U32)
        P.barrier()
        A.release()
        if STAGE <= 4:
            P.dma("sp", out_d[0:128, 0:64], identf[:, 0:64], reads=["identf"], writes=["out"])
            P.barrier()
            P.emit()
            return nc, dbg


        A.mark()
        acc = A.alloc([16, 1024], F32)
        HT = A.alloc([2, 2048], BF16)
        NB = 3
        wgu = [A.alloc([8, 512], BF16) for _ in range(NB)]
        wdn = [A.alloc([2, 1024], BF16) for _ in range(NB)]
        sgb = [A.alloc([512], BF16) for _ in range(2)]
        g3, b3 = ln_bcast_load("ln3")
        NFB = 2 if sparse else 4
        x2r = [A.alloc([1024], F32) for _ in range(NFB)]
        xs3 = [A.alloc([1024], F32) for _ in range(NFB)]
        st6 = A.alloc([4, 12], F32)
        mv = A.alloc([4, 2], F32)
        rstd = A.alloc([4, 1], F32)
        lnt = (st6, mv, rstd)
        n_exp = N_EXPERTS_RUN if not sparse else 0
        sg_rr = 0
        U32 = mybir.dt.uint32
        if not sparse:
            ovf = A.alloc([1], F32)
            P.op("dve", lambda h: h.memset(ovf, 0.0), writes=["ovf"])
            P.dma("sp", ovf_d, ovf, reads=["ovf"], writes=["ovf_d"])

        ln3_h = {}

        def final_a(t):
            tc_ = slice(t * 128, (t + 1) * 128)
            xr = x2r[t % NFB]
            xrt = "x2r%d" % (t % NFB)
            P.dma("sp", xr, x2_d[tc_, :], writes=[xrt])
            xs = xs3[t % NFB]
            xst = "xs3_%d" % (t % NFB)
            stt(xs, xr, ALPHA, acc[:, t, :], ALU.mult, ALU.add, [xrt, ("acc", t, 0), ("acc", t, 1)], [xst])
            ln3_h[t] = layer_norm_a(xs, "ln3", [xst], lnt)

        def final_b(t):
            tc_ = slice(t * 128, (t + 1) * 128)
            xs = xs3[t % NFB]
            xst = "xs3_%d" % (t % NFB)
            layer_norm_b(ln3_h[t], xs, xs, g3, b3, [xst], [xst], gb_eng=("dve" if sparse else None))
            P.dma(ST_Q, out_d[tc_, :], xs, reads=[xst], writes=[("out", t)])

        for ei in range(-1, n_exp):
            bi = (ei + 1) % NB
            gu_t, dn_t = ("wgu", bi), ("wdn", bi)
            if ei < 0:
                P.dma("pool", wgu[bi], w_sh_gu.rearrange("(k p) c -> p k c", p=128), writes=[gu_t])
                P.dma("pool", wdn[bi], w_sh_down.rearrange("(k p) c -> p k c", p=128), writes=[dn_t])
            else:
                P.dma("pool", wgu[bi], w_exp_gu[ei].rearrange("(k p) c -> p k c", p=128), writes=[gu_t])
                P.dma("pool", wdn[bi], w_exp_down[ei].rearrange("(k p) c -> p k c", p=128), writes=[dn_t])
            for tb in range(4):
                cols = slice(tb * 512, (tb + 1) * 512)
                xt_ = [("xTo", tb * 4 + i) for i in range(4)]
                for ft in range(2):
                    bgk, buk = nps(), nps()
                    for (bk, c0) in ((bgk, ft * 128), (buk, 256 + ft * 128)):
                        for dt in range(8):
                            mm(ps[bk], wgu[bi][:, dt, c0:c0 + 128], xTo[:, dt, cols], dt == 0, dt == 7, [gu_t] + xt_, ["ps%d" % bk])
                    sg_ = sgb[sg_rr % 2]
                    sgt_ = "sgb%d" % (sg_rr % 2)
                    sg_rr += 1
                    act(sg_, ps[bgk], AF.Silu, ["ps%d" % bgk], [sgt_])
                    tt("dve", HT[:, ft, cols], ps[buk], sg_, ALU.mult, ["ps%d" % buk, sgt_], [("HT", tb, ft)])
            for t in range(16):
                tc_ = slice(t * 128, (t + 1) * 128)
                for hf in range(2):
                    hc = slice(hf * 512, (hf + 1) * 512)
                    bd = nps()
                    for ft in range(2):
                        mm(ps[bd], HT[:, ft, tc_], wdn[bi][:, ft, hc], ft == 0, ft == 1, [("HT", t // 4, 0), ("HT", t // 4, 1), dn_t], ["ps%d" % bd])
                    if ei < 0:
                        evac(acc[:, t, hc], ps[bd], ["ps%d" % bd], [("acc", t, hf)])
                    else:
                        stt(acc[:, t, hc], ps[bd], gates[:, t, ei:ei + 1], acc[:, t, hc], ALU.mult, ALU.add,
                            ["ps%d" % bd, ("acc", t, hf)], [("acc", t, hf)])
                if ei == n_exp - 1 and not sparse:
                    if t == 0:
                        tap("acc0", acc[:, 0, :], [128, 1024], [("acc", 0, 0), ("acc", 0, 1)])
                    final_a(t)
                    if t >= 1:
                        final_b(t - 1)
            if ei == n_exp - 1 and not sparse:
                final_b(15)
        if sparse:
            P.barrier()
            xflat = xTo.rearrange("p a b -> p (a b)")
            Xg = [xflat[:, i * 4096:(i + 1) * 4096].rearrange("p (j d) -> p j d", j=4) for i in range(3)]
            XgT = xflat[:, 12288:16384].rearrange("p (a b) -> p a b", a=8)
            HTs = [A.alloc([2, 512], BF16) for _ in range(2)]
            Yb = [A.alloc([4, 1024], BF16) for _ in range(2)]
            twe = [A.alloc([4, 2], F32) for _ in range(4)]
            idxe = [A.alloc([4], U32) for _ in range(4)]
            ttoks = [("tbl", T, k) for T in range(16) for k in range(8)]
            x2btoks = [("x2bd", t) for t in range(16)] + ["x2b_zero"]

            def exp_tbl(e):
                b4 = e % 4
                P.dma("sp", twe[b4], tbl_d[e * CAP:(e + 1) * CAP, :].rearrange("(j p) c -> p j c", p=128), reads=ttoks + ["tbl_init"], writes=[("twe", b4)])

            def exp_gather(e):
                b3 = e % 3
                b4 = e % 4
                P.op("pool", lambda h: h.tensor_copy(out=idxe[b4], in_=twe[b4][:, :, 0]), reads=[("twe", b4)], writes=[("idxe", b4)])
                for j in range(4):
                    P.idma(Xg[b3][:, j, :], x2b_d, idxe[b4][:, j:j + 1], gather=True, reads=[("idxe", b4)] + x2btoks, writes=[("Xg", b3, j)])
                bi = (e + 1) % NB
                P.dma("pool", wgu[bi], w_exp_gu[e].rearrange("(k p) c -> p k c", p=128), writes=[("wgu", bi)])
                P.dma("pool", wdn[bi], w_exp_down[e].rearrange("(k p) c -> p k c", p=128), writes=[("wdn", bi)])

            def exp_compute(e):
                b3 = e % 3
                b4 = e % 4
                b2 = e % 2
                bi = (e + 1) % NB
                gu_t, dn_t = ("wgu", bi), ("wdn", bi)
                for j in range(4):
                    bt = nps()
                    for dt in range(8):
                        tr(psb[bt][:, dt * 128:(dt + 1) * 128], Xg[b3][:, j, dt * 128:(dt + 1) * 128], identb, [("Xg", b3, j), "identb"], ["ps%d" % bt])
                    evac(XgT[:, :, j * 128:(j + 1) * 128], psb[bt].rearrange("p (a b) -> p a b", a=8), ["ps%d" % bt], [("XgT", j)])
                xg_t = [("XgT", j) for j in range(4)]
                for ft in range(2):
                    bgk, buk = nps(), nps()
                    for (bk, c0) in ((bgk, ft * 128), (buk, 256 + ft * 128)):
                        for dt in range(8):
                            mm(ps[bk], wgu[bi][:, dt, c0:c0 + 128], XgT[:, dt, :], dt == 0, dt == 7, [gu_t] + xg_t, ["ps%d" % bk])
                    sg_ = sgb[ft]
                    act(sg_, ps[bgk], AF.Silu, ["ps%d" % bgk], [("sgb", ft)])
                    tt("dve", HTs[b2][:, ft, :], ps[buk], sg_, ALU.mult, ["ps%d" % buk, ("sgb", ft)], [("HTs", b2, ft)])
                for j in range(4):
                    for hf in range(2):
                        hc = slice(hf * 512, (hf + 1) * 512)
                        bd = nps()
                        for ft in range(2):
                            mm(ps[bd], HTs[b2][:, ft, j * 128:(j + 1) * 128], wdn[bi][:, ft, hc], ft == 0, ft == 1, [("HTs", b2, 0), ("HTs", b2, 1), dn_t], ["ps%d" % bd])
                        if (j * 2 + hf) % 2 == 0:
                            ts("dve", Yb[b2][:, j, hc], ps[bd], twe[b4][:, j, 1:2], None, ALU.mult, None, ["ps%d" % bd, ("twe", b4)], [("Yb", b2, j, hf)])
                        else:
                            act(Yb[b2][:, j, hc], ps[bd], AF.Copy, ["ps%d" % bd, ("twe", b4)], [("Yb", b2, j, hf)], scale=twe[b4][:, j, 1:2])
                P.dma("pool", y_d[e * CAP:(e + 1) * CAP, :].rearrange("(j p) d -> p j d", p=128), Yb[b2],
                      reads=[("Yb", b2, j, hf) for j in range(4) for hf in range(2)], writes=[("y_d", e)])

            for e0 in range(3):
                exp_tbl(e0)
            exp_gather(0)
            exp_gather(1)
            for e in range(64):
                if e + 3 < 64:
                    exp_tbl(e + 3)
                if e + 2 < 64:
                    exp_gather(e + 2)
                exp_compute(e)
            P.barrier()
            Yk = [xflat[:, i * 1024:(i + 1) * 1024] for i in range(16)]
            ytoks = [("y_d", e) for e in range(64)]
            def comb_gather(t):
                for k in range(8):
                    i_ = (t % 2) * 8 + k
                    P.idma(Yk[i_], y_d, slotu[:, t, k:k + 1], gather=True, reads=ytoks, writes=["Yk%d" % i_])

            comb_gather(0)
            for t in range(16):
                if t + 1 < 16:
                    comb_gather(t + 1)
                bA, bB = nps(), nps()
                for k in range(8):
                    i_ = (t % 2) * 8 + k
                    for hf, bb_ in ((0, bA), (1, bB)):
                        mm(ps[bb_], identb, Yk[i_][:, hf * 512:(hf + 1) * 512], k == 0, k == 7, ["Yk%d" % i_, "identb"], ["ps%d" % bb_])
                for hf, bb_ in ((0, bA), (1, bB)):
                    hc = slice(hf * 512, (hf + 1) * 512)
                    tt("dve", acc[:, t, hc], acc[:, t, hc], ps[bb_], ALU.add, ["ps%d" % bb_, ("acc", t, hf)], [("acc", t, hf)])
                if t == 0:
                    tap("acc0", acc[:, 0, :], [128, 1024], [("acc", 0, 0), ("acc", 0, 1)])
                final_a(t)
                if t >= 1:
                    final_b(t - 1)
            final_b(15)
        P.barrier()
        A.release()
        P.emit()
        return nc, dbg

        raise NotImplementedError
    return nc, dbg


def _consts(r):
    kk = np.arange(128)[:, None]
    qq = np.arange(128)[None, :]
    tri = (kk <= qq).astype(np.float32)
    if r == 0:
        masks = np.concatenate([tri, np.zeros((128, 128), np.float32)], axis=1)
    else:
        masks = np.concatenate([np.ones((128, 128), np.float32), tri], axis=1)
    rflag = np.zeros((128, 2), np.float32)
    rflag[:, 0] = 1.0 - r
    rflag[:, 1] = float(r)
    ident = np.eye(128, dtype=np.float32)
    gi = np.arange(128) // 16
    blockmask = (gi[:, None] == gi[None, :]).astype(np.float32)
    parity = np.stack([(gi % 2 == 0), (gi % 2 == 1)], axis=1).astype(np.float32)
    inv_freq = (10000.0 ** (-np.arange(0, 32, 2, dtype=np.float32) / 32.0)).astype(np.float32)
    invf = np.zeros((128, 1), np.float32)
    for p in range(64, 96):
        invf[p, 0] = inv_freq[(p - 64) % 16]
    tp = np.arange(128)
    triu = (tp[:, None] < tp[None, :]).astype(np.float32)
    iota64 = np.broadcast_to(np.arange(64, dtype=np.float32)[None, :], (128, 64)).copy()
    dum8 = np.zeros((128, 16, 8), np.float32)
    for T in range(16):
        for k in range(8):
            dum8[:, T, k] = NS + (T * 128 + tp) * 8 + k
    return dict(masks=masks, rflag=rflag, ident=ident, blockmask=blockmask, parity=parity, invf=invf,
                triu=triu, iota64=iota64, dum8=dum8.reshape(128, 128))


def make_in_maps(inp):
    x = np.asarray(inp["x"])
    mem = np.asarray(inp["mem"])
    pos = np.asarray(inp["positions"]).astype(np.int32)
    shared = {}
    for k, v in inp.items():
        if k in ("x", "mem", "positions"):
            continue
        v = np.asarray(v)
        shared[k] = np.ascontiguousarray(v[0])
    maps = []
    for c in range(8):
        b, r = c // 2, c % 2
        m = dict(shared)
        m["x_full"] = np.ascontiguousarray(x[b])
        m["x_own"] = np.ascontiguousarray(x[b].reshape(32, 128, 1024)[r::2].reshape(2048, 1024))
        m["mem"] = np.ascontiguousarray(mem[b])
        m["pos_full"] = np.ascontiguousarray(pos[b])
        m["pos_own"] = np.ascontiguousarray(pos[b].reshape(32, 128)[r::2].reshape(2048))
        m.update(_consts(r))
        maps.append(m)
    return maps


def kernel(**inputs):
    maps = make_in_maps(inputs)
    nc, _ = build(sparse=True)
    res = run_bass_kernel_spmd(nc, maps, core_ids=list(range(8)))
    if any(float(np.asarray(res.results[c]["ovf"]).max()) > 0.0 for c in range(8)):
        nc, _ = build(sparse=False)
        res = run_bass_kernel_spmd(nc, maps, core_ids=list(range(8)))
    out = np.zeros((4, 4096, 1024), np.float32)
    for c in range(8):
        b, r = c // 2, c % 2
        o = np.asarray(res.results[c]["out"]).reshape(16, 128, 1024)
        out[b].reshape(32, 128, 1024)[r::2] = o
    return out
```

```python
import numpy as np
from contextlib import ExitStack
import concourse.bass as bass
import concourse.mybir as mybir
from concourse.bass_utils import run_bass_kernel_spmd

F32 = mybir.dt.float32
BF16 = mybir.dt.bfloat16
I32 = mybir.dt.int32
U8 = mybir.dt.uint8
ALU = mybir.AluOpType
AF = mybir.ActivationFunctionType
AX = mybir.AxisListType

ENGS = ["pe", "act", "dve", "pool", "sp"]
NDMA_SLOTS = 12
DEBUG = False
STAGE = 99
NOMASK = False
XA_CUT = 99
N_EXPERTS_RUN = 64
LN_GB_ENG = "pool"
ST_Q = "pool"
CAP = 512
NS = 64 * CAP

ALPHA = 2.0 ** 0.25
MAGIC = 12582912.0
TWO_PI = 2.0 * np.pi


class Prog:
    def __init__(self, nc, stack):
        self.nc = nc
        self.lists = {e: [] for e in ENGS}
        self.cnt = {e: 0 for e in ENGS}
        self.seen = {e: {} for e in ENGS}
        self.tok = {}
        self.sem = {}
        for e in ENGS:
            self.sem[e] = stack.enter_context(nc.semaphore("s_" + e))
        self.dma_slots = {}
        self.dma_rr = {}
        self.dma_uses = {}
        for q in ["sp", "pool"]:
            self.dma_slots[q] = []
            for i in range(NDMA_SLOTS):
                k = "d_%s_%d" % (q, i)
                self.sem[k] = stack.enter_context(nc.semaphore(k))
                self.dma_slots[q].append(k)
                self.dma_uses[k] = 0
            self.dma_rr[q] = 0
        self.n_inst = 0

    def _need(self, E, deps):
        for (k, v) in deps:
            if k == E and E == "pe":
                continue
            if self.seen[E].get(k, 0) >= v:
                continue
            self.seen[E][k] = v
            sem = self.sem[k]
            self.lists[E].append(lambda h, sem=sem, v=v: h.wait_ge(sem, v))

    def _collect(self, reads, writes):
        deps = {}

        def add(d):
            if d is None:
                return
            k, v = d
            if deps.get(k, 0) < v:
                deps[k] = v
        for t in reads:
            st = self.tok.get(t)
            if st is not None:
                add(st["w"])
        for t in writes:
            st = self.tok.get(t)
            if st is not None:
                add(st["w"])
                for k, v in st["r"].items():
                    add((k, v))
        return list(deps.items())

    def _update(self, dep, reads, writes):
        k, v = dep
        for t in reads:
            st = self.tok.setdefault(t, {"w": None, "r": {}})
            if st["r"].get(k, 0) < v:
                st["r"][k] = v
        for t in writes:
            self.tok[t] = {"w": dep, "r": {}}

    def op(self, E, fn, reads=(), writes=()):
        deps = self._collect(reads, writes)
        self._need(E, deps)
        self.cnt[E] += 1
        idx = self.cnt[E]
        sem = self.sem[E]
        self.lists[E].append(lambda h, fn=fn, sem=sem: fn(h).then_inc(sem, 1))
        self._update((E, idx), reads, writes)
        self.n_inst += 1

    def dma(self, Q, out, in_, reads=(), writes=(), **kw):
        slots = self.dma_slots[Q]
        s = slots[self.dma_rr[Q] % len(slots)]
        self.dma_rr[Q] += 1
        deps = self._collect(reads, writes)
        if self.dma_uses[s] > 0:
            deps.append((s, 16 * self.dma_uses[s]))
        self._need(Q, deps)
        self.dma_uses[s] += 1
        v = 16 * self.dma_uses[s]
        sem = self.sem[s]
        self.lists[Q].append(lambda h, out=out, in_=in_, kw=kw, sem=sem: h.dma_start(out=out, in_=in_, allow_slow_non_contiguous=True, **kw).then_inc(sem, 16))
        self._update((s, v), reads, writes)
        self.n_inst += 1
        return (s, v)

    def idma(self, out, in_, idx_ap, gather, reads=(), writes=()):
        Q = "pool"
        slots = self.dma_slots[Q]
        s = slots[self.dma_rr[Q] % len(slots)]
        self.dma_rr[Q] += 1
        deps = self._collect(reads, writes)
        if self.dma_uses[s] > 0:
            deps.append((s, 16 * self.dma_uses[s]))
        self._need(Q, deps)
        self.dma_uses[s] += 1
        v = 16 * self.dma_uses[s]
        sem = self.sem[s]
        off = bass.IndirectOffsetOnAxis(idx_ap, 0)
        if gather:
            self.lists[Q].append(lambda h: h.indirect_dma_start(out=out, out_offset=None, in_=in_, in_offset=off).then_inc(sem, 16))
        else:
            self.lists[Q].append(lambda h: h.indirect_dma_start(out=out, out_offset=off, in_=in_, in_offset=None).then_inc(sem, 16))
        self._update((s, v), reads, writes)
        self.n_inst += 1

    def barrier(self):
        deps = [(e, self.cnt[e]) for e in ENGS if self.cnt[e] > 0]
        for s, u in self.dma_uses.items():
            if u > 0:
                deps.append((s, 16 * u))
        for E in ENGS:
            self._need(E, deps)
        self.tok = {}

    def emit(self):
        print("inst counts", self.cnt, {k: v for k, v in self.dma_uses.items() if v}, flush=True)
        nc = self.nc
        L = self.lists
        with nc.Block() as block:
            @block.tensor
            def _(h):
                for f in L["pe"]:
                    f(h)

            @block.scalar
            def _(h):
                for f in L["act"]:
                    f(h)

            @block.vector
            def _(h):
                for f in L["dve"]:
                    f(h)

            @block.gpsimd
            def _(h):
                for f in L["pool"]:
                    f(h)

            @block.sync
            def _(h):
                for f in L["sp"]:
                    f(h)


class Arena:
    def __init__(self, nc, stack, nbytes):
        self.t = stack.enter_context(nc.sbuf_tensor("arena", [128, nbytes], U8))
        self.nbytes = nbytes
        self.top = 0
        self.marks = []
        self.hi = nbytes

    def alloc(self, shape_free, dtype, top=False):
        esz = {F32: 4, BF16: 2, I32: 4, U8: 1, mybir.dt.uint32: 4}[dtype]
        n = int(np.prod(shape_free)) * esz
        if top:
            off = (self.hi - n) // 64 * 64
            assert off >= self.top, ("arena overflow(top)", off, n, self.top)
            self.hi = off
        else:
            off = (self.top + 63) // 64 * 64
            assert off + n <= self.hi, ("arena overflow", off, n, self.hi)
            self.top = off + n
        ap = self.t[:, off:off + n].bitcast(dtype)
        if len(shape_free) > 1:
            names = " ".join("d%d" % i for i in range(len(shape_free)))
            kw = {"d%d" % i: int(s) for i, s in enumerate(shape_free)}
            ap = ap.rearrange("p (%s) -> p %s" % (names, names), **kw)
        return ap

    def mark(self):
        self.marks.append(self.top)

    def release(self):
        self.top = self.marks.pop()


def build(debug_names=(), sparse=True):
    nc = bass.Bass("TRN2", target_bir_lowering=False)

    def din(name, shape, dt=F32):
        return nc.dram_tensor(name, list(shape), dt, kind="ExternalInput").ap()

    x_own = din("x_own", [2048, 1024])
    x_full = din("x_full", [4096, 1024])
    mem_in = din("mem", [256, 1024])
    pos_full = din("pos_full", [4096], I32)
    pos_own = din("pos_own", [2048], I32)
    masks_in = din("masks", [128, 256])
    rflag_in = din("rflag", [128, 2])
    ident_in = din("ident", [128, 128])
    blockmask_in = din("blockmask", [128, 128])
    parity_in = din("parity", [128, 2])
    invf_in = din("invf", [128, 1])
    w_in = din("w_in", [1024, 3104])
    q_norm_g = din("q_norm_g", [256])
    kv_norm_g = din("kv_norm_g", [256])
    w_uq = din("w_uq", [256, 768])
    w_ukv = din("w_ukv", [256, 1024])
    w_mla_o = din("w_mla_o", [512, 1024])
    s5_a_re = din("s5_a_re", [32, 64])
    s5_a_im = din("s5_a_im", [32, 64])
    s5_log_dt = din("s5_log_dt", [32])
    s5_b_re = din("s5_b_re", [32, 64, 16])
    s5_b_im = din("s5_b_im", [32, 64, 16])
    s5_c_re = din("s5_c_re", [32, 16, 64])
    s5_c_im = din("s5_c_im", [32, 16, 64])
    s5_d = din("s5_d", [512])
    w_s5_glu = din("w_s5_glu", [512, 2048])
    w_out = din("w_out", [1024, 1024])
    ln_g = {}
    ln_b = {}
    for nm in ["ln1", "mem_ln", "ln2", "ln3"]:
        ln_g[nm] = din(nm + "_g", [1024])
        ln_b[nm] = din(nm + "_b", [1024])
    w_xq = din("w_xq", [1024, 512])
    w_xkv = din("w_xkv", [1024, 1024])
    w_xo = din("w_xo", [512, 1024])
    w_router = din("w_router", [1024, 64])
    router_bias = din("router_bias", [64])
    w_exp_gu = din("w_exp_gu", [64, 1024, 512])
    w_exp_down = din("w_exp_down", [64, 256, 1024])
    w_sh_gu = din("w_sh_gu", [1024, 512])
    w_sh_down = din("w_sh_down", [256, 1024])

    out_d = nc.dram_tensor("out", [2048, 1024], F32, kind="ExternalOutput").ap()
    ovf_d = nc.dram_tensor("ovf", [128, 1], F32, kind="ExternalOutput").ap()
    x2b_d = nc.dram_tensor("x2b_scr", [2049, 1024], BF16, kind="Internal").ap()
    tbl_d = nc.dram_tensor("tbl_scr", [NS + 16384, 2], F32, kind="Internal").ap()
    y_d = nc.dram_tensor("y_scr", [NS + 1, 1024], BF16, kind="Internal").ap()
    triu_in = din("triu", [128, 128])
    iota_in = din("iota64", [128, 64])
    dum_in = din("dum8", [128, 16 * 8])
    x1_d = nc.dram_tensor("x1_scr", [2048, 1024], F32, kind="Internal").ap()
    x2_d = nc.dram_tensor("x2_scr", [2048, 1024], F32, kind="Internal").ap()
    dbg = {}

    with ExitStack() as stack:
        P = Prog(nc, stack)
        A = Arena(nc, stack, 206 * 1024)
        pst = [stack.enter_context(nc.psum_tensor("ps%d" % i, [128, 512], F32)) for i in range(8)]
        ps = [p_[:, :] for p_ in pst]
        psb = [p_.bitcast(BF16)[:, :] for p_ in pst]
        ps_rr = [0]

        def nps():
            i = ps_rr[0] % 8
            ps_rr[0] += 1
            return i

        def tap(name, ap, shape, reads, dt=F32):
            if name not in debug_names:
                return
            d = nc.dram_tensor("dbg_" + name, list(shape), dt, kind="ExternalOutput").ap()
            P.dma("sp", d, ap, reads=reads, writes=["dbg_" + name])
            dbg[name] = d

        def mm(out, lhsT, rhs, start, stop, reads, writes):
            P.op("pe", lambda h: h.matmul(out, lhsT=lhsT, rhs=rhs, start=start, stop=stop, skip_group_check=True), reads=reads, writes=writes)

        def tr(out, in_, ident, reads, writes):
            P.op("pe", lambda h: h.transpose(out=out, in_=in_, identity=ident), reads=reads, writes=writes)

        evac_rr = [0]

        def evac(out, in_, reads, writes, eng=None):
            if eng is None:
                eng = "act" if evac_rr[0] % 2 == 0 else "dve"
                evac_rr[0] += 1
            if eng == "act":
                P.op("act", lambda h: h.copy(out=out, in_=in_), reads=reads, writes=writes)
            else:
                P.op(eng, lambda h: h.tensor_copy(out=out, in_=in_), reads=reads, writes=writes)

        def tt(E, out, in0, in1, op, reads, writes):
            P.op(E, lambda h: h.tensor_tensor(out=out, in0=in0, in1=in1, op=op), reads=reads, writes=writes)

        def ts(E, out, in0, s1, s2, op0, op1, reads, writes):
            if op1 is None:
                P.op(E, lambda h: h.tensor_scalar(out=out, in0=in0, scalar1=s1, scalar2=None, op0=op0), reads=reads, writes=writes)
            else:
                P.op(E, lambda h: h.tensor_scalar(out=out, in0=in0, scalar1=s1, scalar2=s2, op0=op0, op1=op1), reads=reads, writes=writes)

        def stt(out, in0, scalar, in1, op0, op1, reads, writes):
            P.op("dve", lambda h: h.scalar_tensor_tensor(out=out, in0=in0, scalar=scalar, in1=in1, op0=op0, op1=op1), reads=reads, writes=writes)

        def act(out, in_, func, reads, writes, scale=1.0, bias=None, accum_out=None):
            kw = {}
            if bias is not None:
                kw["bias"] = bias
            if accum_out is not None:
                kw["accum_out"] = accum_out
            P.op("act", lambda h: h.activation(out=out, in_=in_, func=func, scale=scale, **kw), reads=reads, writes=writes)

        def load_w_bf16(dst, src, kt, tokname):
            for k in range(kt):
                P.dma("pool", dst[:, k, :], src[k * 128:(k + 1) * 128, :], writes=[(tokname, k)])

        def sincos(E_tmp, yin, n, parts, toks_in, sin_out, cos_out, tokname, tmps=None):
            p0, p1 = parts
            if tmps is None:
                tmps = (A.alloc([n], F32), A.alloc([n], F32))
            t1 = tmps[0][p0:p1]
            t2 = tmps[1][p0:p1]
            for which, outap, off in (("s", sin_out, 0.0), ("c", cos_out, 0.25)):
                ts("dve", t1, yin, off, MAGIC, ALU.add, ALU.add, toks_in, [tokname + "t1"])
                ts("dve", t2, t1, -MAGIC, None, ALU.add, None, [tokname + "t1"], [tokname + "t2"])
                stt(t1, yin, off, t2, ALU.add, ALU.subtract, toks_in + [tokname + "t2"], [tokname + "t1"])
                act(outap, t1, AF.Sin, [tokname + "t1"], [tokname + which], scale=TWO_PI)

        identf = A.alloc([128], F32)
        identb = A.alloc([128], BF16)
        P.dma("sp", identf, ident_in, writes=["identf"])
        P.dma("pool", identb, ident_in, writes=["identb"])
        rflag = A.alloc([2], F32)
        P.dma("sp", rflag, rflag_in, writes=["rflag"])
        ones_b = A.alloc([128], BF16)
        P.op("dve", lambda h: h.memset(ones_b, 1.0), writes=["ones_b"])
        eps_ln = A.alloc([1], F32)
        P.op("dve", lambda h: h.memset(eps_ln, 1e-5), writes=["eps"])

        def ln_bcast_load(nm):
            g = A.alloc([1024], F32)
            b = A.alloc([1024], F32)
            P.dma("sp", g, ln_g[nm].partition_broadcast(128), writes=[nm + "_g"])
            P.dma("sp", b, ln_b[nm].partition_broadcast(128), writes=[nm + "_b"])
            return g, b

        ln_par = [0]

        def layer_norm_a(src, nm0, reads, tmp_stats):
            par = ln_par[0] % 4
            ln_par[0] += 1
            st6a, mva, rstda = tmp_stats
            st6, mv, rstd = st6a[:, par, :], mva[:, par, :], rstda[:, par, :]
            nm = (nm0, par)
            for i in range(2):
                P.op("dve", lambda h, i=i: h.bn_stats(out=st6[:, i * 6:(i + 1) * 6], in_=src[:, i * 512:(i + 1) * 512]), reads=reads, writes=[("lnst", nm, i)])
            P.op("dve", lambda h: h.bn_aggr(out=mv, in_=st6), reads=[("lnst", nm, 0), ("lnst", nm, 1)], writes=[("lnmv", nm)])
            act(rstd, mv[:, 1:2], AF.Ln, [("lnmv", nm)], [("lnr", nm)], bias=eps_ln[:, 0:1])
            act(rstd, rstd, AF.Exp, [("lnr", nm)], [("lnr", nm)], scale=-0.5)
            return (mv, rstd, nm, nm0)

        def layer_norm_b(hnd, src, dst, g, b, reads, writes, gb_eng=None):
            mv, rstd, nm, nm0 = hnd
            gb_eng = gb_eng or LN_GB_ENG
            ts("dve", dst, src, mv[:, 0:1], rstd, ALU.subtract, ALU.mult, list(reads) + [("lnmv", nm), ("lnr", nm)], writes)
            tt(gb_eng, dst, dst, g, ALU.mult, list(writes) + [nm0 + "_g"], writes)
            tt(gb_eng, dst, dst, b, ALU.add, list(writes) + [nm0 + "_b"], writes)

        def layer_norm(src, dst, g, b, nm0, reads, writes, tmp_stats):
            hnd = layer_norm_a(src, nm0, reads, tmp_stats)
            layer_norm_b(hnd, src, dst, g, b, reads, writes)

        def load_xT_block(src_rows, dstT, col0, tokbase, xin_bufs, blk, eng=None):
            xb = xin_bufs[blk % len(xin_bufs)]
            xtok = ("xin", blk % len(xin_bufs))
            for t in range(4):
                P.dma("pool", xb[:, t, :], src_rows[t * 128:(t + 1) * 128, :], writes=[xtok + (t,)])
            for t in range(4):
                b_ = nps()
                for dt in range(8):
                    tr(psb[b_][:, dt * 128:(dt + 1) * 128], xb[:, t, dt * 128:(dt + 1) * 128], identb,
                       [xtok + (t,), "identb"], ["ps%d" % b_])
                evac(dstT[:, :, col0 + t * 128:col0 + (t + 1) * 128], psb[b_].rearrange("p (a b) -> p a b", a=8),
                     ["ps%d" % b_], [(tokbase, (col0 // 128) + t)], eng=eng)

        A.mark()
        XownS = A.alloc([32, 128], BF16)
        hi_save = A.hi
        W1 = A.alloc([4, 2, 16, 128], BF16, top=True)
        KB = A.alloc([4, 16, 128], BF16)
        Cs = A.alloc([17, 32, 16], BF16)
        w_u = A.alloc([8, 512], BF16)
        sd = A.alloc([4], F32)
        AA1 = A.alloc([2, 16], F32)
        AA2 = A.alloc([2, 16], F32)
        with nc.allow_non_contiguous_dma(reason="tiny param loads"):
            P.dma("sp", sd, s5_d.rearrange("(t r) -> r t", r=128), writes=["sd"])
        A.mark()
        load_w_bf16(w_u, w_in[:, 544:1056], 8, "w_u")
        wutoks = [("w_u", k) for k in range(8)]
        uTf = A.alloc([4, 16, 128], BF16)
        xin_bufs = [A.alloc([4, 1024], BF16)]
        xTblk = [A.alloc([8, 512], BF16) for _ in range(2)]

        def u_blocks(half, eng):
            for bl in range(4):
                blk = half * 4 + bl
                xt = xTblk[blk % 2]
                tb = "xTb%d" % (blk % 2)
                load_xT_block(x_full[blk * 512:(blk + 1) * 512, :], xt, 0, tb, xin_bufs, blk, eng=eng)
                for T in range(4):
                    b_ = nps()
                    for dt in range(8):
                        mm(ps[b_], w_u[:, dt, T * 128:(T + 1) * 128], xt[:, dt, :], dt == 0, dt == 7,
                           wutoks + [(tb, t) for t in range(4)], ["ps%d" % b_])
                    evac(uTf[:, T, :, bl * 32:(bl + 1) * 32], ps[b_].rearrange("p (c j) -> p j c", j=16), ["ps%d" % b_], [("uTf", T)], eng=eng)

        A.mark()
        are = A.alloc([32], F32)
        aim = A.alloc([32], F32)
        ldt = A.alloc([32], F32)
        bre = A.alloc([32, 16], F32)
        bim = A.alloc([32, 16], F32)
        cre = A.alloc([32, 16], F32)
        cim = A.alloc([32, 16], F32)
        craw = A.alloc([2, 4, 2, 64], F32)
        with nc.allow_non_contiguous_dma(reason="tiny param loads"):
            for hf in range(2):
                sl = slice(hf * 64, hf * 64 + 64)
                P.dma("sp", are[sl, :], s5_a_re.rearrange("g p -> p g"), writes=[("are", hf)])
                P.dma("sp", aim[sl, :], s5_a_im.rearrange("g p -> p g"), writes=[("aim", hf)])
                P.dma("sp", bre[sl], s5_b_re.rearrange("g p h -> p g h"), writes=[("bre", hf)])
                P.dma("sp", bim[sl], s5_b_im.rearrange("g p h -> p g h"), writes=[("bim", hf)])
            P.dma("sp", ldt, s5_log_dt.partition_broadcast(128), writes=["ldt"])
            for ri, src in enumerate((s5_c_re, s5_c_im)):
                for dup in range(2):
                    P.dma("sp", craw[:, ri, :, dup, :], src.rearrange("g h p -> (g h) p").rearrange("(t r) p -> r t p", r=128),
                          writes=[("craw", ri, dup)])
        for ri, dstc in enumerate((cre, cim)):
            for T in range(4):
                b_ = nps()
                tr(ps[b_][:, 0:128], craw[:, ri, T].rearrange("p a b -> p (a b)"), identf,
                   [("craw", ri, 0), ("craw", ri, 1), "identf"], ["ps%d" % b_])
                evac(dstc[:, T * 8:(T + 1) * 8, :], ps[b_][:, 0:128].rearrange("p (a b) -> p a b", a=8), ["ps%d" % b_], [("c%d" % ri, T)])
        ctoks = [("c0", T) for T in range(4)] + [("c1", T) for T in range(4)]
        atoks = [("are", 0), ("are", 1), ("aim", 0), ("aim", 1)]
        btoks = [("bre", 0), ("bre", 1), ("bim", 0), ("bim", 1)]
        dtv = A.alloc([32], F32)
        mag = A.alloc([32], F32)
        yang = A.alloc([32], F32)
        sn = A.alloc([32], F32)
        cs = A.alloc([32], F32)
        act(dtv, ldt, AF.Exp, ["ldt"], ["dtv"])
        tt("dve", mag, are, dtv, ALU.mult, atoks + ["dtv"], ["mag"])
        act(mag, mag, AF.Exp, ["mag"], ["mag"])
        tt("dve", yang, aim, dtv, ALU.mult, atoks + ["dtv"], ["yang"])
        ts("dve", yang, yang, 1.0 / TWO_PI, None, ALU.mult, None, ["yang"], ["yang"])
        A.mark()
        sincos("dve", yang, 32, (0, 128), ["yang"], sn, cs, "s5sc")
        A.release()
        abr = A.alloc([32], F32)
        abi = A.alloc([32], F32)
        tt("dve", abr, mag, cs, ALU.mult, ["mag", "s5scc"], ["abr"])
        tt("dve", abi, mag, sn, ALU.mult, ["mag", "s5scs"], ["abi"])
        u_blocks(0, "act")
        den = A.alloc([32], F32)
        tmpa = A.alloc([32], F32)
        tmpb = A.alloc([32], F32)
        cr = A.alloc([32], F32)
        ci = A.alloc([32], F32)
        tt("dve", den, are, are, ALU.mult, atoks, ["den"])
        tt("dve", tmpa, aim, aim, ALU.mult, atoks, ["tmpa"])
        tt("dve", den, den, tmpa, ALU.add, ["den", "tmpa"], ["den"])
        P.op("dve", lambda h: h.reciprocal(out=den, in_=den), reads=["den"], writes=["den"])
        nr = A.alloc([32], F32)
        ts("dve", nr, abr, -1.0, None, ALU.add, None, ["abr"], ["nr"])
        tt("dve", tmpa, nr, are, ALU.mult, ["nr"] + atoks, ["tmpa"])
        tt("dve", tmpb, abi, aim, ALU.mult, ["abi"] + atoks, ["tmpb"])
        tt("dve", tmpa, tmpa, tmpb, ALU.add, ["tmpa", "tmpb"], ["tmpa"])
        tt("dve", cr, tmpa, den, ALU.mult, ["tmpa", "den"], ["cr"])
        tt("dve", tmpa, abi, are, ALU.mult, ["abi"] + atoks, ["tmpa"])
        tt("dve", tmpb, nr, aim, ALU.mult, ["nr"] + atoks, ["tmpb"])
        tt("dve", tmpa, tmpa, tmpb, ALU.subtract, ["tmpa", "tmpb"], ["tmpa"])
        tt("dve", ci, tmpa, den, ALU.mult, ["tmpa", "den"], ["ci"])
        bbr = A.alloc([32, 16], F32)
        bbi = A.alloc([32, 16], F32)
        tb1 = A.alloc([32, 16], F32)
        crB = cr.unsqueeze(2).to_broadcast([128, 32, 16])
        ciB = ci.unsqueeze(2).to_broadcast([128, 32, 16])
        tt("dve", bbr, bre, crB, ALU.mult, btoks + ["cr"], ["bbr"])
        tt("dve", tb1, bim, ciB, ALU.mult, btoks + ["ci"], ["tb1"])
        tt("dve", bbr, bbr, tb1, ALU.subtract, ["bbr", "tb1"], ["bbr"])
        tt("dve", bbi, bim, crB, ALU.mult, btoks + ["cr"], ["bbi"])
        tt("dve", tb1, bre, ciB, ALU.mult, btoks + ["ci"], ["tb1"])
        tt("dve", bbi, bbi, tb1, ALU.add, ["bbi", "tb1"], ["bbi"])
        pwr = A.alloc([17, 32], F32)
        pwi = A.alloc([17, 32], F32)
        P.op("dve", lambda h: h.memset(pwr[:, 0, :], 1.0), writes=[("pw", 0)])
        P.op("dve", lambda h: h.memset(pwi[:, 0, :], 0.0), writes=[("pw", 0)])
        for k in range(1, 17):
            rd = [("pw", k - 1), "abr", "abi"]
            tt("dve", tmpa, pwr[:, k - 1, :], abr, ALU.mult, rd, ["tmpa"])
            tt("dve", tmpb, pwi[:, k - 1, :], abi, ALU.mult, rd, ["tmpb"])
            tt("dve", pwr[:, k, :], tmpa, tmpb, ALU.subtract, ["tmpa", "tmpb"], [("pw", k)])
            tt("dve", tmpa, pwr[:, k - 1, :], abi, ALU.mult, rd, ["tmpa"])
            tt("dve", tmpb, pwi[:, k - 1, :], abr, ALU.mult, rd, ["tmpb"])
            tt("dve", pwi[:, k, :], tmpa, tmpb, ALU.add, ["tmpa", "tmpb"], [("pw", k)])
        pwtoks = [("pw", k) for k in range(17)]
        for hp in range(2):
            pp = slice(hp * 64, hp * 64 + 64)
            gg = slice(hp * 16, hp * 16 + 16)
            for ri in range(2):
                P.op("dve", lambda h, pp=pp, gg=gg, ri=ri: h.tensor_copy(out=AA1[pp, ri, :], in_=pwr[pp, 16, gg]), reads=pwtoks, writes=["AA1"])
            ts("dve", AA2[pp, 0, :], pwi[pp, 16, gg], -1.0, None, ALU.mult, None, pwtoks, ["AA2"])
            P.op("dve", lambda h, pp=pp, gg=gg: h.tensor_copy(out=AA2[pp, 1, :], in_=pwi[pp, 16, gg]), reads=pwtoks, writes=["AA2"])
        Bs = A.alloc([16, 32, 16], BF16)
        t4a = A.alloc([2, 32, 16], F32)
        t4b = A.alloc([2, 32, 16], F32)
        lo, hi = slice(0, 64), slice(64, 128)

        def bc_pw(pw, k0, nk, sl):
            return pw[sl, k0:k0 + nk, :].unsqueeze(3).to_broadcast([64, nk, 32, 16])

        def bc_x(xx, nk, sl):
            return xx[sl].unsqueeze(1).to_broadcast([64, nk, 32, 16])

        for k0 in range(0, 16, 2):
            tt("dve", t4a[lo], bc_pw(pwr, k0, 2, lo), bc_x(bbr, 2, lo), ALU.mult, pwtoks + ["bbr"], ["t4a"])
            tt("dve", t4b[lo], bc_pw(pwi, k0, 2, lo), bc_x(bbi, 2, lo), ALU.mult, pwtoks + ["bbi"], ["t4b"])
            tt("dve", Bs[lo, k0:k0 + 2], t4a[lo], t4b[lo], ALU.subtract, ["t4a", "t4b"], [("Bs", k0)])
            tt("dve", t4a[hi], bc_pw(pwr, k0, 2, hi), bc_x(bbi, 2, hi), ALU.mult, pwtoks + ["bbi"], ["t4a"])
            tt("dve", t4b[hi], bc_pw(pwi, k0, 2, hi), bc_x(bbr, 2, hi), ALU.mult, pwtoks + ["bbr"], ["t4b"])
            tt("dve", Bs[hi, k0:k0 + 2], t4a[hi], t4b[hi], ALU.add, ["t4a", "t4b"], [("Bs", k0)])
        for k0, nk in ((0, 2), (2, 2), (4, 2), (6, 2), (8, 2), (10, 2), (12, 2), (14, 2), (16, 1)):
            tt("dve", t4a[lo, 0:nk], bc_pw(pwr, k0, nk, lo), bc_x(cre, nk, lo), ALU.mult, pwtoks + ctoks, ["t4a"])
            tt("dve", t4b[lo, 0:nk], bc_pw(pwi, k0, nk, lo), bc_x(cim, nk, lo), ALU.mult, pwtoks + ctoks, ["t4b"])
            tt("dve", Cs[lo, k0:k0 + nk], t4a[lo, 0:nk], t4b[lo, 0:nk], ALU.subtract, ["t4a", "t4b"], [("Cs", k0)])
            tt("dve", t4a[hi, 0:nk], bc_pw(pwi, k0, nk, hi), bc_x(cre, nk, hi), ALU.mult, pwtoks + ctoks, ["t4a"])
            tt("dve", t4b[hi, 0:nk], bc_pw(pwr, k0, nk, hi), bc_x(cim, nk, hi), ALU.mult, pwtoks + ctoks, ["t4b"])
            stt(Cs[hi, k0:k0 + nk], t4a[hi, 0:nk], -1.0, t4b[hi, 0:nk], ALU.mult, ALU.subtract, ["t4a", "t4b"], [("Cs", k0)])
        Bstoks = [("Bs", k0) for k0 in range(0, 16, 2)]
        Cstoks = [("Cs", k0) for k0 in range(0, 17, 2)]
        parity = A.alloc([2], F32)
        blockmask = A.alloc([128], F32)
        P.dma("sp", parity, parity_in, writes=["parity"])
        P.dma("sp", blockmask, blockmask_in, writes=["blockmask"])
        for T in range(4):
            for j in range(16):
                b_ = nps()
                tr(psb[b_][:, 0:128], Bs[:, 15 - j, T * 8:(T + 1) * 8, :].rearrange("p a b -> p (a b)"), identb, Bstoks + ["identb"], ["ps%d" % b_])
                for e in range(2):
                    ts("dve", W1[:, T, e, j, :], psb[b_][:, 0:128], parity[:, e:e + 1], None, ALU.mult, None,
                       ["ps%d" % b_, "parity"], [("W1", T, e, j)])
        for T in range(4):
            for tau in range(16):
                b_ = nps()
                mm(ps[b_][:, 0:128], Bs[:, tau, T * 8:(T + 1) * 8, :].rearrange("p a b -> p (a b)"),
                   Cs[:, 0, T * 8:(T + 1) * 8, :].rearrange("p a b -> p (a b)"), True, True, Bstoks + Cstoks, ["ps%d" % b_])
                tt("dve", KB[:, T, tau, :], ps[b_][:, 0:128], blockmask, ALU.mult, ["ps%d" % b_, "blockmask"], [("KB", T)])
        tap("KB", KB, [128, 4, 16, 128], [("KB", T) for T in range(4)], BF16)
        tap("W1", W1, [128, 4, 2, 16, 128], [("W1", T, e, j) for T in range(4) for e in range(2) for j in range(16)], BF16)
        P.barrier()
        A.release()

        Cstoks = ["CsB"]
        Bp = A.alloc([2, 16, 257], F32)
        P.op("pool", lambda h: h.memset(Bp[:, :, :, 0:1], 0.0), writes=["Bp0"])
        for half in range(2):
            if half == 1:
                u_blocks(1, None)
            if half == 0:
                tap("uTf", uTf, [128, 4, 16, 128], [("uTf", T) for T in range(4)], BF16)
            for T in range(4):
                for s_ in range(4):
                    for e in range(2):
                        g = T * 8 + 2 * s_ + e
                        hp, gg = g // 16, g % 16
                        b_ = nps()
                        rows = slice(32 * s_, 32 * s_ + 32) if s_ < 3 else slice(64, 128)
                        for j in range(16):
                            for hf in range(2):
                                mm(ps[b_][hp * 64:hp * 64 + 64, hf * 128:(hf + 1) * 128], W1[rows, T, e, j, hf * 64:(hf + 1) * 64], uTf[rows, T, j, :],
                                   (j == 0 and hf == 0), (j == 15 and hf == 1), [("W1", T, e, j), ("uTf", T)], ["ps%d" % b_])
                        dst = Bp[hp * 64:hp * 64 + 64, :, gg, 1 + half * 128:1 + (half + 1) * 128]
                        src = ps[b_][hp * 64:hp * 64 + 64, 0:256].rearrange("p (a b) -> p a b", a=2)
                        if s_ < 3:
                            evac(dst, src, ["ps%d" % b_], [("Bp", g)])
                        else:
                            oth = Bp[hp * 64:hp * 64 + 64, :, gg - 2, 1 + half * 128:1 + (half + 1) * 128]
                            tt("dve", dst, src, oth, ALU.subtract, ["ps%d" % b_, ("Bp", g - 2)], [("Bp", g)])
        P.barrier()
        A.hi = hi_save
        Bq = Bp[:, :, :, 1:257].rearrange("p r g (s k) -> p r g s k", k=16)
        l1a = A.alloc([2, 16, 16], F32)
        l1b = A.alloc([2, 16, 16], F32)
        AA1b = AA1.unsqueeze(3).to_broadcast([128, 2, 16, 16])
        for k in range(1, 16):
            tt("dve", l1a, AA1b, Bq[:, :, :, :, k - 1], ALU.mult, ["Bq"], ["l1a"])
            tt("dve", l1b[:, 0], AA2[:, 0, :].unsqueeze(2).to_broadcast([128, 16, 16]), Bq[:, 1, :, :, k - 1], ALU.mult, ["Bq"], ["l1b"])
            tt("dve", l1b[:, 1], AA2[:, 1, :].unsqueeze(2).to_broadcast([128, 16, 16]), Bq[:, 0, :, :, k - 1], ALU.mult, ["Bq"], ["l1b"])
            tt("dve", l1a, l1a, l1b, ALU.add, ["l1a", "l1b"], ["l1a"])
            tt("dve", Bq[:, :, :, :, k], Bq[:, :, :, :, k], l1a, ALU.add, ["l1a", "Bq"], ["Bq"])
        pwj = A.alloc([5, 2, 16], F32)
        sq1 = A.alloc([16], F32)
        sq2 = A.alloc([16], F32)
        P.op("dve", lambda h: h.tensor_copy(out=pwj[:, 0, 0, :], in_=AA1[:, 0, :]), reads=["AA1"], writes=[("pwj", 0)])
        P.op("dve", lambda h: h.tensor_copy(out=pwj[:, 0, 1, :], in_=AA2[:, 1, :]), reads=["AA2"], writes=[("pwj", 0)])
        for j in range(1, 5):
            r_, i_ = pwj[:, j - 1, 0, :], pwj[:, j - 1, 1, :]
            tt("dve", sq1, r_, r_, ALU.mult, [("pwj", j - 1)], ["sq1"])
            tt("dve", sq2, i_, i_, ALU.mult, [("pwj", j - 1)], ["sq2"])
            tt("dve", pwj[:, j, 0, :], sq1, sq2, ALU.subtract, ["sq1", "sq2"], [("pwj", j)])
            tt("dve", sq1, r_, i_, ALU.mult, [("pwj", j - 1)], ["sq1"])
            ts("dve", pwj[:, j, 1, :], sq1, 2.0, None, ALU.mult, None, ["sq1"], [("pwj", j)])
        BB1 = A.alloc([2, 16], F32)
        BB2 = A.alloc([2, 16], F32)
        for ri in range(2):
            P.op("dve", lambda h, ri=ri: h.tensor_copy(out=BB1[:, ri, :], in_=pwj[:, 4, 0, :]), reads=[("pwj", 4)], writes=["BB1"])
        ts("dve", BB2[:, 0, :], pwj[:, 4, 1, :], -1.0, None, ALU.mult, None, [("pwj", 4)], ["BB2"])
        P.op("dve", lambda h: h.tensor_copy(out=BB2[:, 1, :], in_=pwj[:, 4, 1, :]), reads=[("pwj", 4)], writes=["BB2"])
        Sb = A.alloc([2, 16, 17], F32)
        P.op("dve", lambda h: h.memset(Sb[:, :, :, 0:1], 0.0), writes=["Sb"])
        s2a = A.alloc([2, 16], F32)
        s2b = A.alloc([2, 16], F32)
        for s_ in range(15):
            tt("dve", s2a, BB1, Sb[:, :, :, s_], ALU.mult, ["Sb", "BB1"], ["s2a"])
            tt("dve", s2b[:, 0, :], BB2[:, 0, :], Sb[:, 1, :, s_], ALU.mult, ["Sb", "BB2"], ["s2b"])
            tt("dve", s2b[:, 1, :], BB2[:, 1, :], Sb[:, 0, :, s_], ALU.mult, ["Sb", "BB2"], ["s2b"])
            tt("dve", s2a, s2a, s2b, ALU.add, ["s2a", "s2b"], ["s2a"])
            tt("dve", Sb[:, :, :, s_ + 1], s2a, Bq[:, :, :, s_, 15], ALU.add, ["s2a", "Bq"], ["Sb"])
        Ap = A.alloc([2, 16, 16], F32)
        d1 = A.alloc([16, 8], F32)
        d2 = A.alloc([16, 8], F32)
        P.op("dve", lambda h: h.tensor_copy(out=Ap[:, :, :, 0], in_=pwj[:, 0]), reads=[("pwj", 0)], writes=["Ap"])
        for j in range(4):
            n = 1 << j
            pr = pwj[:, j, 0, :].unsqueeze(2).to_broadcast([128, 16, n])
            pi = pwj[:, j, 1, :].unsqueeze(2).to_broadcast([128, 16, n])
            o_r, o_i = Ap[:, 0, :, 0:n], Ap[:, 1, :, 0:n]
            n_r, n_i = Ap[:, 0, :, n:2 * n], Ap[:, 1, :, n:2 * n]
            rd = ["Ap", ("pwj", j)]
            tt("dve", d1[:, :, 0:n], o_r, pr, ALU.mult, rd, ["d1"])
            tt("dve", d2[:, :, 0:n], o_i, pi, ALU.mult, rd, ["d2"])
            tt("dve", n_r, d1[:, :, 0:n], d2[:, :, 0:n], ALU.subtract, ["d1", "d2", "Ap"], ["Ap"])
            tt("dve", d1[:, :, 0:n], o_r, pi, ALU.mult, rd, ["d1"])
            tt("dve", d2[:, :, 0:n], o_i, pr, ALU.mult, rd, ["d2"])
            tt("dve", n_i, d1[:, :, 0:n], d2[:, :, 0:n], ALU.add, ["d1", "d2", "Ap"], ["Ap"])
        tA = A.alloc([16, 16, 16], F32)
        tB = A.alloc([16, 16, 16], F32)
        Apr_b = Ap[:, 0].unsqueeze(2).to_broadcast([128, 16, 16, 16])
        Api_b = Ap[:, 1].unsqueeze(2).to_broadcast([128, 16, 16, 16])
        Sr_b = Sb[:, 0, :, 0:16].unsqueeze(3).to_broadcast([128, 16, 16, 16])
        Si_b = Sb[:, 1, :, 0:16].unsqueeze(3).to_broadcast([128, 16, 16, 16])
        tt("dve", tA, Apr_b, Sr_b, ALU.mult, ["Ap", "Sb"], ["tA"])
        tt("dve", tB, Api_b, Si_b, ALU.mult, ["Ap", "Sb"], ["tB"])
        tt("dve", tA, tA, tB, ALU.subtract, ["tA", "tB"], ["tA"])
        tt("dve", Bq[:, 0], Bq[:, 0], tA, ALU.add, ["tA", "Bq"], ["Bq"])
        tt("dve", tA, Apr_b, Si_b, ALU.mult, ["Ap", "Sb"], ["tA"])
        tt("dve", tB, Api_b, Sr_b, ALU.mult, ["Ap", "Sb"], ["tB"])
        tt("dve", tA, tA, tB, ALU.add, ["tA", "tB"], ["tA"])
        tt("dve", Bq[:, 1], Bq[:, 1], tA, ALU.add, ["tA", "Bq"], ["Bpc"])
        tap("Bp", Bp, [128, 2, 16, 257], ["Bpc"])
        Xsel = A.alloc([2, 16, 128], F32)
        Xodd = A.alloc([2, 16, 128], F32)
        Xsb = A.alloc([2, 16, 128], BF16)
        Bv = Bp[:, :, :, 0:256].rearrange("p r g (m two c) -> p r g m two c", two=2, c=8)
        for ri in range(2):
            xs = Xsel[:, ri].rearrange("p g (m c) -> p g m c", c=8)
            ts("dve", xs, Bv[:, ri, :, :, 0, :], rflag[:, 0:1], None, ALU.mult, None, ["Bpc", "rflag"], [("Xsel", ri)])
            xo_ = Xodd[:, ri].rearrange("p g (m c) -> p g m c", c=8)
            ts("dve", xo_, Bv[:, ri, :, :, 1, :], rflag[:, 1:2], None, ALU.mult, None, ["Bpc", "rflag"], [("Xodd", ri)])
            tt("dve", Xsel[:, ri], Xsel[:, ri], Xodd[:, ri], ALU.add, [("Xsel", ri), ("Xodd", ri)], [("Xsel", ri)])
        P.op("dve", lambda h: h.tensor_copy(out=Xsb, in_=Xsel), reads=[("Xsel", 0), ("Xsel", 1)], writes=["Xsb"])
        P.op("act", lambda h: h.copy(out=XownS[0:64, 0:16, :], in_=Xsb[0:64, 0]), reads=["Xsb"], writes=["XownS_a"])
        P.op("act", lambda h: h.copy(out=XownS[64:128, 16:32, :], in_=Xsb[64:128, 1]), reads=["Xsb"], writes=["XownS_b"])
        P.dma("sp", XownS[64:128, 0:16, :], Xsb[0:64, 1], reads=["Xsb"], writes=["XownS_c"])
        P.dma("sp", XownS[0:64, 16:32, :], Xsb[64:128, 0], reads=["Xsb"], writes=["XownS_d"])
        Xtoks = ["XownS_a", "XownS_b", "XownS_c", "XownS_d"]
        tap("XownS", XownS, [128, 32, 128], Xtoks, BF16)
        P.barrier()
        A.release()
        xTo = A.alloc([8, 2048], BF16, top=True)
        hi_xTo = A.hi
        ygT = A.alloc([4, 2048], BF16, top=True)
        hi_persist = A.hi
        W3 = A.alloc([32, 16, 32], BF16, top=True)
        W3b = A.alloc([4, 2, 16, 64], BF16, top=True)
        P.op("pool", lambda h: h.memset(W3, 0.0), writes=["W3"])
        P.op("pool", lambda h: h.memset(W3b, 0.0), writes=["W3b"])
        W3v = W3.rearrange("p (gp e) j (f c) -> p gp e j f c", e=2, f=2)
        Csv = Cs.rearrange("p j (gp e) c -> p gp e j c", e=2)
        for e in range(2):
            P.op("dve", lambda h, e=e: h.tensor_copy(out=W3v[:, :, e, :, e, :], in_=Csv[:, :, e, 1:17, :]), reads=["W3"], writes=["W3"])
        for T in range(4):
            P.op("dve", lambda h, T=T: h.tensor_copy(out=W3b[:, T, :, :, 32:64], in_=W3[:, T * 8 + 6:T * 8 + 8, :, :]), reads=["W3", "W3b"], writes=["W3b"])
        A.mark()
        xin_bufs = [A.alloc([4, 1024], BF16)]
        uTo = A.alloc([4, 16, 128], BF16)
        for blk in range(4):
            load_xT_block(x_own[blk * 512:(blk + 1) * 512, :], xTo, blk * 512, "xTo", xin_bufs, blk)
            for T in range(4):
                b_ = nps()
                for dt in range(8):
                    mm(ps[b_], w_u[:, dt, T * 128:(T + 1) * 128], xTo[:, dt, blk * 512:(blk + 1) * 512], dt == 0, dt == 7,
                       wutoks + [("xTo", blk * 4 + t) for t in range(4)], ["ps%d" % b_])
                evac(uTo[:, T, :, blk * 32:(blk + 1) * 32], ps[b_].rearrange("p (c j) -> p j c", j=16), ["ps%d" % b_], [("uTo", T)])
        yT = A.alloc([2, 2048], F32)
        gt = A.alloc([2048], F32)
        for T in range(4):
            yb = T % 2
            for j in range(16):
                b_ = nps()
                for i in range(j + 1):
                    mm(ps[b_][:, 0:128], KB[:, T, j - i, :], uTo[:, T, i, :], i == 0, False, [("KB", T), ("uTo", T)], ["ps%d" % b_])
                for gl in range(8):
                    g = T * 8 + gl
                    if gl < 6:
                        c0 = 32 * (gl // 2)
                        mm(ps[b_][c0:c0 + 32, 0:128], W3[:, g, j, :], XownS[:, g, :], False, False, ["W3"] + Xtoks, ["ps%d" % b_])
                    else:
                        mm(ps[b_][64:128, 0:128], W3b[:, T, gl - 6, j, :], XownS[:, g, :], False, gl == 7, ["W3b"] + Xtoks, ["ps%d" % b_])
                stt(yT[:, yb, j::16], uTo[:, T, j, :], sd[:, T:T + 1], ps[b_][:, 0:128], ALU.mult, ALU.add,
                    ["ps%d" % b_, "sd", ("uTo", T)], [("yT", yb)])
            if T == 0:
                tap("yT", yT[:, 0, :], [128, 2048], [("yT", 0)])
            y_ = yT[:, yb, :]
            tt("dve", gt, y_, y_, ALU.mult, [("yT", yb)], ["gt"])
            ts("dve", gt, gt, 0.044715, 1.0, ALU.mult, ALU.add, ["gt"], ["gt"])
            tt("dve", gt, gt, y_, ALU.mult, ["gt", ("yT", yb)], ["gt"])
            act(gt, gt, AF.Sigmoid, ["gt"], ["gt"], scale=1.5957691216057308)
            tt("dve", ygT[:, T, :], gt, y_, ALU.mult, ["gt", ("yT", yb)], [("ygT", T)])
        tap("ygT", ygT, [128, 4, 2048], [("ygT", T) for T in range(4)], BF16)
        P.barrier()
        A.release()
        A.release()
        A.hi = hi_persist
        if STAGE <= 1:
            P.dma("sp", out_d[0:128, 0:64], identf[:, 0:64], reads=["identf"], writes=["out"])
            P.barrier()
            P.emit()
            return nc, dbg


        attnT = A.alloc([4, 2048], BF16, top=True)
        A.mark()
        ckvTf = A.alloc([2, 4096], BF16)
        kropeT = A.alloc([4096], BF16)
        invf = A.alloc([1], F32)
        P.dma("sp", invf, invf_in, writes=["invf"])
        RP = slice(64, 96)
        sqj = A.alloc([256], F32)
        ssq = A.alloc([1], F32)
        cn = A.alloc([256], BF16)
        gkv = A.alloc([256], F32)
        gq = A.alloc([256], F32)
        P.dma("sp", gkv, kv_norm_g.partition_broadcast(128), writes=["gkv"])
        P.dma("sp", gq, q_norm_g.partition_broadcast(128), writes=["gq"])

        rp_posi = A.alloc([512], I32)
        rp_posf = A.alloc([512], F32)
        rp_t1 = A.alloc([512], F32)
        rp_t2 = A.alloc([512], F32)

        def rope_tables(pos_d, c0, n, cs_o, sn_o, nm):
            posi = rp_posi
            posf = rp_posf
            P.dma("sp", posi[RP], pos_d[c0:c0 + n].partition_broadcast(32), writes=["posi"])
            P.op("dve", lambda h: h.tensor_copy(out=posf[RP], in_=posi[RP]), reads=["posi"], writes=["posf"])
            ts("dve", posf[RP], posf[RP], invf[RP, 0:1], 1.0 / TWO_PI, ALU.mult, ALU.mult, ["posf", "invf"], ["posf"])
            sincos("dve", posf[RP], n, (64, 96), ["posf"], sn_o, cs_o, "rope", tmps=(rp_t1, rp_t2))

        def rms_to_T(ps_ap, ps_tok, g_ap, gtok, dstT, col0, tokw):
            act(sqj, ps_ap, AF.Square, [ps_tok], ["sqj"])
            P.op("dve", lambda h: h.tensor_reduce(out=ssq, in_=sqj, axis=AX.X, op=ALU.add), reads=["sqj"], writes=["ssq"])
            ts("dve", ssq, ssq, 1.0 / 256.0, 1e-6, ALU.mult, ALU.add, ["ssq"], ["ssq"])
            P.op("act", lambda h: h.sqrt(out=ssq, in_=ssq), reads=["ssq"], writes=["ssq"])
            P.op("dve", lambda h: h.reciprocal(out=ssq, in_=ssq), reads=["ssq"], writes=["ssq"])
            stt(cn, ps_ap, ssq[:, 0:1], g_ap, ALU.mult, ALU.mult, [ps_tok, "ssq", gtok], ["cn"])
            b2 = nps()
            for jt in range(2):
                tr(psb[b2][:, jt * 128:(jt + 1) * 128], cn[:, jt * 128:(jt + 1) * 128], identb, ["cn", "identb"], ["ps%d" % b2])
            evac(dstT[:, :, col0:col0 + 128], psb[b2][:, 0:256].rearrange("p (a b) -> p a b", a=2), ["ps%d" % b2], [tokw])

        A.mark()
        w_kv = A.alloc([8, 288], BF16)
        load_w_bf16(w_kv, w_in[:, 256:544], 8, "w_kv")
        wkvtoks = [("w_kv", k) for k in range(8)]
        w_krr = A.alloc([8, 32], BF16)
        P.op("act", lambda h: h.mul(out=w_krr[:, :, 0:16], in_=w_kv[:, :, 272:288], mul=-1.0), reads=wkvtoks, writes=["w_krr"])
        P.op("act", lambda h: h.copy(out=w_krr[:, :, 16:32], in_=w_kv[:, :, 256:272]), reads=wkvtoks, writes=["w_krr"])
        xin_bufs = [A.alloc([4, 1024], BF16)]
        xTblk = [A.alloc([8, 512], BF16) for _ in range(2)]
        krt = A.alloc([2, 512], F32)
        csb = A.alloc([512], F32)
        snb = A.alloc([512], F32)
        for blk in range(8):
            xt = xTblk[blk % 2]
            tb = "xTb%d" % (blk % 2)
            xtoks = [(tb, t) for t in range(4)]
            load_xT_block(x_full[blk * 512:(blk + 1) * 512, :], xt, 0, tb, xin_bufs, blk)
            for t in range(4):
                b_ = nps()
                for dt in range(8):
                    mm(ps[b_][:, 0:256], xt[:, dt, t * 128:(t + 1) * 128], w_kv[:, dt, 0:256], dt == 0, dt == 7, wkvtoks + xtoks, ["ps%d" % b_])
                rms_to_T(ps[b_][:, 0:256], "ps%d" % b_, gkv, "gkv", ckvTf, blk * 512 + t * 128, ("ckvT", blk))
            rope_tables(pos_full, blk * 512, 512, csb[RP], snb[RP], "K")
            ba, bb = nps(), nps()
            for dt in range(8):
                mm(ps[ba][RP, :], w_kv[:, dt, 256:288], xt[:, dt, :], dt == 0, dt == 7, wkvtoks + xtoks, ["ps%d" % ba])
            for dt in range(8):
                mm(ps[bb][RP, :], w_krr[:, dt, :], xt[:, dt, :], dt == 0, dt == 7, ["w_krr"] + xtoks, ["ps%d" % bb])
            tt("dve", krt[RP, 0, :], ps[ba][RP, :], csb[RP], ALU.mult, ["ps%d" % ba, "ropec"], ["krt0"])
            tt("dve", krt[RP, 1, :], ps[bb][RP, :], snb[RP], ALU.mult, ["ps%d" % bb, "ropes"], ["krt1"])
            tt("dve", kropeT[RP, blk * 512:(blk + 1) * 512], krt[RP, 0, :], krt[RP, 1, :], ALU.add, ["krt0", "krt1"], [("krope", blk)])
        tap("ckvTf", ckvTf, [128, 2, 4096], [("ckvT", b) for b in range(8)], BF16)
        P.barrier()
        A.release()
        KT = A.alloc([4, 4096], BF16)
        Vp = A.alloc([32, 4, 65], BF16)
        cqT = A.alloc([2, 2048], BF16)
        maskb = A.alloc([2, 128], BF16)
        P.dma("pool", maskb.rearrange("p a b -> p (a b)"), masks_in, writes=["maskb"])
        P.op("dve", lambda h: h.memset(Vp[:, :, :, 64:65], 1.0), writes=["Vp1"])
        cosQ = A.alloc([2048], BF16)
        sinQ = A.alloc([2048], BF16)
        for q4 in range(4):
            rope_tables(pos_own, q4 * 512, 512, cosQ[RP, q4 * 512:(q4 + 1) * 512], sinQ[RP, q4 * 512:(q4 + 1) * 512], "Q")
        w_ukv_s = A.alloc([2, 1024], BF16)
        load_w_bf16(w_ukv_s, w_ukv, 2, "w_ukv")
        wukvtoks = [("w_ukv", k) for k in range(2)]
        w_uk_v = w_ukv_s.rearrange("p k (h c) -> p k h c", c=128)
        A.mark()
        w_q = A.alloc([8, 256], BF16)
        load_w_bf16(w_q, w_in[:, 0:256], 8, "w_q")
        wqtoks = [("w_q", k) for k in range(8)]
        for t in range(16):
            b_ = nps()
            for dt in range(8):
                mm(ps[b_][:, 0:256], xTo[:, dt, t * 128:(t + 1) * 128], w_q[:, dt, :], dt == 0, dt == 7, wqtoks + [("xTo", t)], ["ps%d" % b_])
            rms_to_T(ps[b_][:, 0:256], "ps%d" % b_, gq, "gq", cqT, t * 128, ("cqT", t))
        P.barrier()
        A.release()
        w_uq_s = A.alloc([2, 768], BF16)
        load_w_bf16(w_uq_s, w_uq, 2, "w_uq")
        wuqtoks = [("w_uq", k) for k in range(2)]
        w_uq_v = w_uq_s.rearrange("p k (h c) -> p k h c", c=96)
        w_uqr = A.alloc([2, 8, 32], BF16)
        P.op("act", lambda h: h.mul(out=w_uqr[:, :, :, 0:16], in_=w_uq_v[:, :, :, 80:96], mul=-1.0), reads=wuqtoks, writes=["w_uqr"])
        P.op("act", lambda h: h.copy(out=w_uqr[:, :, :, 16:32], in_=w_uq_v[:, :, :, 64:80]), reads=wuqtoks, writes=["w_uqr"])
        QTb = [A.alloc([2048], BF16) for _ in range(2)]
        attn_tok = A.alloc([16, 512], BF16)
        PTb = [A.alloc([512], BF16) for _ in range(4)]
        pt_rr_box = [0]
        Osb = A.alloc([4, 65], F32)
        rinv = A.alloc([4], F32)
        cqtoks = [("cqT", t) for t in range(16)]
        SCALE = 96.0 ** -0.5
        sb_rr = [0]

        def sbank():
            i = 2 + sb_rr[0] % 6
            sb_rr[0] += 1
            return i

        def build_QT(h_):
            QT = QTb[h_ % 2]
            qtok = "QT%d" % (h_ % 2)
            for blk in range(4):
                cols = slice(blk * 512, (blk + 1) * 512)
                ba, bb = sbank(), sbank()
                for jt in range(2):
                    mm(ps[ba][0:96, :], w_uq_v[:, jt, h_, :], cqT[:, jt, cols], jt == 0, jt == 1, wuqtoks + cqtoks, ["ps%d" % ba])
                for jt in range(2):
                    mm(ps[bb][RP, :], w_uqr[:, jt, h_, :], cqT[:, jt, cols], jt == 0, jt == 1, ["w_uqr"] + cqtoks, ["ps%d" % bb])
                evac(QT[0:64, cols], ps[ba][0:64, :], ["ps%d" % ba], [(qtok, "n", blk)], eng="act")
                tt("dve", rp_t1[RP], ps[ba][RP, :], cosQ[RP, cols], ALU.mult, ["ps%d" % ba, "ropec"], ["qrt0"])
                tt("dve", rp_t2[RP], ps[bb][RP, :], sinQ[RP, cols], ALU.mult, ["ps%d" % bb, "ropes"], ["qrt1"])
                tt("dve", QT[RP, cols], rp_t1[RP], rp_t2[RP], ALU.add, ["qrt0", "qrt1"], [(qtok, "r", blk)])

        def build_KV(h0):
            for h2 in range(h0, h0 + 4):
                for blk in range(8):
                    b_ = sbank()
                    for jt in range(2):
                        mm(ps[b_][0:64, :], w_ukv_s[:, jt, h2 * 128:h2 * 128 + 64], ckvTf[:, jt, blk * 512:(blk + 1) * 512], jt == 0, jt == 1,
                           wukvtoks, ["ps%d" % b_])
                    evac(KT[0:64, h2 - h0, blk * 512:(blk + 1) * 512], ps[b_][0:64, :], ["ps%d" % b_], [("KTn", h2 - h0)])
                P.op("act", lambda h, h2=h2, h0=h0: h.copy(out=KT[RP, h2 - h0, :], in_=kropeT[RP, :]), reads=[], writes=[("KTr", h2 - h0)])
            for kb in range(32):
                b_ = sbank()
                for jt in range(2):
                    mm(ps[b_][:, 0:256], ckvTf[:, jt, kb * 128:(kb + 1) * 128], w_uk_v[:, jt, h0:h0 + 4, 64:128], jt == 0, jt == 1, wukvtoks, ["ps%d" % b_])
                evac(Vp[:, kb, :, 0:64], ps[b_][:, 0:256].rearrange("p (a b) -> p a b", a=4), ["ps%d" % b_], [("Vp", kb)])

        acc_rr = 0
        LOOK = 2
        build_QT(0)
        for h_ in range(8):
            hl = h_ % 4
            if hl == 0:
                build_KV(h_)
                if h_ == 0:
                    tap("KT", KT, [128, 4, 4096], [("KTn", i) for i in range(4)] + [("KTr", i) for i in range(4)], BF16)
                    tap("Vp", Vp, [128, 32, 4, 65], [("Vp", k) for k in range(32)] + ["Vp1"], BF16)
            if h_ + 1 < 8:
                build_QT(h_ + 1)
            QT = QTb[h_ % 2]
            qtok = "QT%d" % (h_ % 2)
            if h_ == 0:
                tap("QT0", QT, [128, 2048], [(qtok, "n", b) for b in range(4)] + [(qtok, "r", b) for b in range(4)], BF16)
            jobs = [(G, kb) for G in range(4) for kb in range(8 * G + 8)]
            st = {}
            bo_of = {}
            for G in range(4):
                bo_of[G] = acc_rr % 2
                acc_rr += 1

            def emit_S(i):
                G, kb = jobs[i]
                bs_ = sbank()
                mm(ps[bs_], KT[0:96, hl, kb * 128:(kb + 1) * 128], QT[0:96, G * 512:(G + 1) * 512], True, True,
                   [("KTn", hl), ("KTr", hl), (qtok, "n", G), (qtok, "r", G)], ["ps%d" % bs_])
                st[i] = (bs_, pt_rr_box[0] % 4)
                pt_rr_box[0] += 1

            def emit_rest(i):
                G, kb = jobs[i]
                bs_, pi = st.pop(i)
                bo = bo_of[G]
                nkb = 8 * G + 8
                PT = PTb[pi]
                ptok = "PT%d" % pi
                act(PT, ps[bs_], AF.Exp, ["ps%d" % bs_], [ptok], scale=SCALE)
                j = kb - 8 * G
                for qs in range(4):
                    if j > 2 * qs + 1:
                        continue
                    if (j == 2 * qs or j == 2 * qs + 1) and not NOMASK:
                        tt("dve", PT[:, qs * 128:(qs + 1) * 128], PT[:, qs * 128:(qs + 1) * 128], maskb[:, j - 2 * qs, :], ALU.mult, [ptok, "maskb"], [ptok])
                    last = (kb == nkb - 1 and qs == 3)
                    mm(ps[bo][:, qs * 65:(qs + 1) * 65], PT[:, qs * 128:(qs + 1) * 128], Vp[:, kb, hl, :], (kb == 0 and qs == 0), last,
                       [ptok, ("Vp", kb), "Vp1"], ["ps%d" % bo])
                if kb == nkb - 1:
                    evac(Osb, ps[bo][:, 0:260].rearrange("p (a b) -> p a b", a=4), ["ps%d" % bo], ["Osb"], eng="act")
                    P.op("dve", lambda h: h.reciprocal(out=rinv, in_=Osb[:, :, 64]), reads=["Osb"], writes=["rinv"])
                    tt("dve", attn_tok[:, G * 4:(G + 1) * 4, h_ * 64:(h_ + 1) * 64], Osb[:, :, 0:64], rinv.unsqueeze(2).to_broadcast([128, 4, 64]), ALU.mult,
                       ["Osb", "rinv"], [("attn_tok", G)])

            for i in range(min(LOOK, len(jobs))):
                emit_S(i)
            for i in range(len(jobs)):
                if i + LOOK < len(jobs):
                    emit_S(i + LOOK)
                emit_rest(i)
        for t in range(16):
            b_ = nps()
            for kt in range(4):
                tr(psb[b_][:, kt * 128:(kt + 1) * 128], attn_tok[:, t, kt * 128:(kt + 1) * 128], identb, [("attn_tok", t // 4), "identb"], ["ps%d" % b_])
            evac(attnT[:, :, t * 128:(t + 1) * 128], psb[b_][:, 0:512].rearrange("p (a b) -> p a b", a=4), ["ps%d" % b_], [("attnT", t)])
        tap("attnT", attnT, [128, 4, 2048], [("attnT", t) for t in range(16)], BF16)
        P.barrier()
        A.release()
        if STAGE <= 2:
            P.dma("sp", out_d[0:128, 0:64], identf[:, 0:64], reads=["identf"], writes=["out"])
            P.barrier()
            P.emit()
            return nc, dbg


        A.mark()
        w_g = A.alloc([8, 2048], BF16)
        P.dma("pool", w_g[:, :, 0:1024], w_in[:, 1056:2080].rearrange("(k p) c -> p k c", p=128), writes=[("w_g", 0)])
        P.dma("pool", w_g[:, :, 1024:2048], w_in[:, 2080:3104].rearrange("(k p) c -> p k c", p=128), writes=[("w_g", 1)])
        w_mo = A.alloc([4, 1024], BF16)
        P.dma("pool", w_mo, w_mla_o.rearrange("(k p) c -> p k c", p=128), writes=["w_mo"])
        w_glu = A.alloc([4, 2048], BF16)
        for k in range(4):
            P.dma("pool", w_glu[:, k, :], w_s5_glu[k * 128:(k + 1) * 128, :], writes=[("w_glu", k)])
        wglutoks = [("w_glu", k) for k in range(4)]
        w_o = A.alloc([8, 1024], BF16)
        P.dma("pool", w_o, w_out.rearrange("(k p) c -> p k c", p=128), writes=["w_o"])
        g1, b1 = ln_bcast_load("ln1")
        m1 = A.alloc([1024], F32)
        ys5 = A.alloc([1024], F32)
        sgt = [A.alloc([512], F32) for _ in range(2)]
        mbfs = [A.alloc([1024], BF16) for _ in range(2)]
        mTs = [A.alloc([8, 128], BF16) for _ in range(2)]
        xres = [A.alloc([1024], F32) for _ in range(3)]
        xs1 = [A.alloc([1024], F32) for _ in range(3)]
        xb1s = [A.alloc([1024], BF16) for _ in range(3)]
        st6 = A.alloc([4, 12], F32)
        mv = A.alloc([4, 2], F32)
        rstd = A.alloc([4, 1], F32)
        lnt = (st6, mv, rstd)

        ln_h = {}

        def merge_A(t):
            tc_ = slice(t * 128, (t + 1) * 128)
            xr = xres[t % 3]
            xrt = "xres%d" % (t % 3)
            mbf = mbfs[t % 2]
            P.dma("sp", xr, x_own[tc_, :], writes=[xrt])
            for hf in range(2):
                hc = slice(hf * 512, (hf + 1) * 512)
                by = nps()
                for kt in range(4):
                    mm(ps[by], attnT[:, kt, tc_], w_mo[:, kt, hc], kt == 0, kt == 3, [("attnT", t), "w_mo"], ["ps%d" % by])
                bg = nps()
                for dt in range(8):
                    mm(ps[bg], xTo[:, dt, tc_], w_g[:, dt, hc], dt == 0, dt == 7, [("xTo", t), ("w_g", 0)], ["ps%d" % bg])
                act(m1[:, hc], ps[bg], AF.Sigmoid, ["ps%d" % bg], [("m1", hf)])
                tt("dve", m1[:, hc], m1[:, hc], ps[by], ALU.mult, [("m1", hf), "ps%d" % by], [("m1", hf)])
                bv, bgt = nps(), nps()
                for T in range(4):
                    mm(ps[bv], ygT[:, T, tc_], w_glu[:, T, hc], T == 0, T == 3, [("ygT", T)] + wglutoks, ["ps%d" % bv])
                for T in range(4):
                    mm(ps[bgt], ygT[:, T, tc_], w_glu[:, T, 1024 + hf * 512:1024 + (hf + 1) * 512], T == 0, T == 3, [("ygT", T)] + wglutoks, ["ps%d" % bgt])
                sg_ = sgt[hf]
                act(sg_, ps[bgt], AF.Sigmoid, ["ps%d" % bgt], [("sgt", hf)])
                tt("dve", ys5[:, hc], sg_, ps[bv], ALU.mult, [("sgt", hf), "ps%d" % bv], [("ys5", hf)])
                bg2 = nps()
                for dt in range(8):
                    mm(ps[bg2], xTo[:, dt, tc_], w_g[:, dt, 1024 + hf * 512:1024 + (hf + 1) * 512], dt == 0, dt == 7, [("xTo", t), ("w_g", 1)], ["ps%d" % bg2])
                act(sg_, ps[bg2], AF.Sigmoid, ["ps%d" % bg2], [("sgt", hf)])
                tt("dve", ys5[:, hc], ys5[:, hc], sg_, ALU.mult, [("sgt", hf), ("ys5", hf)], [("ys5", hf)])
                tt("dve", mbf[:, hc], m1[:, hc], ys5[:, hc], ALU.add, [("m1", hf), ("ys5", hf)], [("mbf", t % 2, hf)])

        def merge_B(t):
            tc_ = slice(t * 128, (t + 1) * 128)
            xr = xres[t % 3]
            xrt = "xres%d" % (t % 3)
            mbf = mbfs[t % 2]
            mT = mTs[t % 2]
            mTt = "mT%d" % (t % 2)
            xb1 = xb1s[t % 3]
            xbt = "xb1_%d" % (t % 3)
            bt = nps()
            for dt in range(8):
                tr(psb[bt][:, dt * 128:(dt + 1) * 128], mbf[:, dt * 128:(dt + 1) * 128], identb, [("mbf", t % 2, 0), ("mbf", t % 2, 1), "identb"], ["ps%d" % bt])
            evac(mT, psb[bt].rearrange("p (a b) -> p a b", a=8), ["ps%d" % bt], [mTt])
            xs = xs1[t % 3]
            xst = "xs1_%d" % (t % 3)
            for hf in range(2):
                hc = slice(hf * 512, (hf + 1) * 512)
                bo_ = nps()
                for dt in range(8):
                    mm(ps[bo_], mT[:, dt, :], w_o[:, dt, hc], dt == 0, dt == 7, [mTt, "w_o"], ["ps%d" % bo_])
                stt(xs[:, hc], xr[:, hc], ALPHA, ps[bo_], ALU.mult, ALU.add, [xrt, "ps%d" % bo_], [(xst, hf)])
            ln_h[t] = layer_norm_a(xs, "ln1", [(xst, 0), (xst, 1)], lnt)

        def merge_B2a(t):
            tc_ = slice(t * 128, (t + 1) * 128)
            xb1 = xb1s[t % 3]
            xbt = "xb1_%d" % (t % 3)
            xs = xs1[t % 3]
            xst = "xs1_%d" % (t % 3)
            xh = [(xst, 0), (xst, 1)]
            layer_norm_b(ln_h[t], xs, xs, g1, b1, xh, xh)
            if t == 0:
                tap("x1_0", xs, [128, 1024], xh)
            P.dma(ST_Q, x1_d[tc_, :], xs, reads=xh, writes=[("x1d", t)])
            P.op("act", lambda h, xs=xs, xb1=xb1: h.copy(out=xb1, in_=xs), reads=xh, writes=[xbt])

        def merge_B2b(t):
            tc_ = slice(t * 128, (t + 1) * 128)
            xb1 = xb1s[t % 3]
            xbt = "xb1_%d" % (t % 3)
            bt = nps()
            for dt in range(8):
                tr(psb[bt][:, dt * 128:(dt + 1) * 128], xb1[:, dt * 128:(dt + 1) * 128], identb, [xbt, "identb"], ["ps%d" % bt])
            evac(xTo[:, :, tc_], psb[bt].rearrange("p (a b) -> p a b", a=8), ["ps%d" % bt], [("xTo", t)])

        merge_A(0)
        for t in range(16):
            if t >= 1:
                merge_B2a(t - 1)
            if t + 1 < 16:
                merge_A(t + 1)
            if t >= 1:
                merge_B2b(t - 1)
            merge_B(t)
        merge_B2a(15)
        merge_B2b(15)
        P.barrier()
        A.release()
        A.hi = hi_xTo
        if STAGE <= 3:
            P.dma("sp", out_d[0:128, 0:64], identf[:, 0:64], reads=["identf"], writes=["out"])
            P.barrier()
            P.emit()
            return nc, dbg


        gates = A.alloc([16, 64], F32, top=True)
        U32 = mybir.dt.uint32
        slotu = A.alloc([16, 8], U32, top=True)
        slotgu = A.alloc([16, 8], U32, top=True)
        twv = A.alloc([16, 8, 2], F32, top=True)
        ovf = A.alloc([1], F32, top=True)
        hi_gates = A.hi
        A.mark()
        w_xq_s = A.alloc([8, 512], BF16)
        P.dma("pool", w_xq_s, w_xq.rearrange("(k p) c -> p k c", p=128), writes=["w_xq"])
        w_xo_s = A.alloc([4, 1024], BF16)
        P.dma("pool", w_xo_s, w_xo.rearrange("(k p) c -> p k c", p=128), writes=["w_xo"])
        w_rt = A.alloc([8, 64], F32)
        P.dma("sp", w_rt, w_router.rearrange("(k p) c -> p k c", p=128), writes=["w_rt"])
        rbias = A.alloc([64], F32)
        P.dma("sp", rbias, router_bias.partition_broadcast(128), writes=["rbias"])
        g2, b2 = ln_bcast_load("ln2")
        KxT = A.alloc([4, 256], BF16)
        Vx = A.alloc([2, 4, 129], BF16)
        P.op("dve", lambda h: h.memset(Vx[:, :, :, 128:129], 1.0), writes=["Vx1"])
        st6 = A.alloc([4, 12], F32)
        mv = A.alloc([4, 2], F32)
        rstd = A.alloc([4, 1], F32)
        lnt = (st6, mv, rstd)
        A.mark()
        w_xkv_s = A.alloc([8, 1024], BF16)
        P.dma("pool", w_xkv_s, w_xkv.rearrange("(k p) c -> p k c", p=128), writes=["w_xkv"])
        gm, bm = ln_bcast_load("mem_ln")
        memf = A.alloc([2, 1024], F32)
        memb = A.alloc([2, 1024], BF16)
        memT = A.alloc([8, 256], BF16)
        for mt in range(2):
            P.dma("sp", memf[:, mt, :], mem_in[mt * 128:(mt + 1) * 128, :], writes=[("memf", mt)])
            layer_norm(memf[:, mt, :], memf[:, mt, :], gm, bm, "mem_ln", [("memf", mt)], [("memf", mt)], lnt)
            P.op("act", lambda h, mt=mt: h.copy(out=memb[:, mt, :], in_=memf[:, mt, :]), reads=[("memf", mt)], writes=[("memb", mt)])
            bt = nps()
            for dt in range(8):
                tr(psb[bt][:, dt * 128:(dt + 1) * 128], memb[:, mt, dt * 128:(dt + 1) * 128], identb, [("memb", mt), "identb"], ["ps%d" % bt])
            evac(memT[:, :, mt * 128:(mt + 1) * 128], psb[bt].rearrange("p (a b) -> p a b", a=8), ["ps%d" % bt], [("memT", mt)])
        mtoks = [("memT", 0), ("memT", 1)]
        for h_ in range(4):
            b_ = nps()
            for dt in range(8):
                mm(ps[b_][:, 0:256], w_xkv_s[:, dt, h_ * 128:(h_ + 1) * 128], memT[:, dt, :], dt == 0, dt == 7, ["w_xkv"] + mtoks, ["ps%d" % b_])
            evac(KxT[:, h_, :], ps[b_][:, 0:256], ["ps%d" % b_], [("KxT", h_)])
        for mt in range(2):
            b_ = nps()
            for dt in range(8):
                mm(ps[b_], memT[:, dt, mt * 128:(mt + 1) * 128], w_xkv_s[:, dt, 512:1024], dt == 0, dt == 7, ["w_xkv"] + mtoks, ["ps%d" % b_])
            evac(Vx[:, mt, :, 0:128], ps[b_].rearrange("p (a b) -> p a b", a=4), ["ps%d" % b_], [("Vx", mt)])
        P.barrier()
        A.release()
        if XA_CUT == 1:
            tap("KxT", KxT, [128, 4, 256], [], BF16)
            tap("Vx", Vx, [128, 2, 4, 129], [], BF16)
            P.barrier(); P.emit(); return nc, dbg
        QxT = [A.alloc([512], BF16) for _ in range(2)]
        PTx = [A.alloc([512], BF16) for _ in range(4)]
        Osx = A.alloc([4, 129], F32)
        rinx = A.alloc([4], F32)
        xo_toks = [A.alloc([4, 512], BF16) for _ in range(2)]
        xoT = A.alloc([4, 128], BF16)
        x1r = [A.alloc([1024], F32) for _ in range(3)]
        xs2 = [A.alloc([1024], F32) for _ in range(3)]
        x2Tl = A.alloc([8, 128], BF16)
        xb_his = [A.alloc([1024], BF16) for _ in range(2)]
        xb_los = [A.alloc([1024], BF16) for _ in range(2)]
        w_rh = A.alloc([8, 64], BF16)
        w_rl = A.alloc([8, 64], BF16)
        P.op("act", lambda h: h.copy(out=w_rh, in_=w_rt), reads=["w_rt"], writes=["w_rhl"])
        tt("dve", w_rl, w_rt, w_rh, ALU.subtract, ["w_rt", "w_rhl"], ["w_rhl"])
        r_sc = A.alloc([64], F32)
        r_sel = A.alloc([64], F32)
        r_eq = A.alloc([64], F32)
        r_sel2 = A.alloc([64], F32)
        r_m1 = A.alloc([8], F32)
        r_m2 = A.alloc([8], F32)
        r_t8 = A.alloc([8], F32)
        r_gm = A.alloc([8], F32)
        r_pen = A.alloc([8], F32)
        r_den = A.alloc([1], F32)
        if sparse:
            P.op("dve", lambda h: h.memset(ovf, 0.0), writes=["ovf"])
            triu = A.alloc([128], BF16)
            P.dma("pool", triu, triu_in, writes=["triu"])
            iota64 = A.alloc([64], F32)
            P.dma("sp", iota64, iota_in, writes=["iota64"])
            dum8 = A.alloc([16, 8], F32)
            P.dma("sp", dum8.rearrange("p a b -> p (a b)"), dum_in, writes=["dum8"])
            tinit = A.alloc([NS // 128, 2], F32)
            P.op("dve", lambda h: h.memset(tinit[:, :, 0:1], 2048.0), writes=["tinit"])
            P.op("dve", lambda h: h.memset(tinit[:, :, 1:2], 0.0), writes=["tinit"])
            P.dma("sp", tbl_d[0:NS, :].rearrange("(p j) c -> p j c", p=128), tinit, reads=["tinit"], writes=["tbl_init"])
            zrow = A.alloc([1024], BF16)
            P.op("dve", lambda h: h.memset(zrow, 0.0), writes=["zrow"])
            P.dma("sp", x2b_d[2048:2049, :], zrow[0:1, :], reads=["zrow"], writes=["x2b_zero"])
            P.dma("sp", y_d[NS:NS + 1, :], zrow[0:1, :], reads=["zrow"], writes=["y_zero"])
            P.dma("sp", tbl_d[NS:NS + 16384, :].rearrange("(p j) c -> p (j c)", p=128), zrow.bitcast(F32)[:, 0:256], reads=["zrow"], writes=["tbl_init2"])
            slotg = A.alloc([8], F32)
            maskb16 = A.alloc([16, 64], BF16)
            v8s = [A.alloc([8], F32) for _ in range(2)]
            i8s = [A.alloc([8], U32) for _ in range(2)]
            i8f = A.alloc([8], F32)
            oh3 = A.alloc([8, 64], F32)
            pos_sbs = [A.alloc([64], F32) for _ in range(2)]
            pos8 = A.alloc([8], F32)
            ov8 = A.alloc([8], F32)
            dlt = A.alloc([8], F32)
            slotf = A.alloc([8], F32)
            tokf = A.alloc([16, 1], F32)
            ts("dve", tokf, dum8[:, :, 0:1], -float(NS), 0.125, ALU.add, ALU.mult, ["dum8"], ["tokf"])

        def slot_a(T):
            q = T % 2
            ts("dve", maskb16[:, T, :], gates[:, T, :], 0.0, None, ALU.is_gt, None, [("gates", T)], [("maskb16", T)])
            bp_ = nps()
            for T2 in range(T + 1):
                mm(ps[bp_][:, 0:64], ones_b if T2 < T else triu, maskb16[:, T2, :], T2 == 0, T2 == T,
                   ["ones_b", "triu", ("maskb16", T2)], ["ps%d" % bp_])
            evac(pos_sbs[q], ps[bp_][:, 0:64], ["ps%d" % bp_], [("pos_sb", q)], eng="act")
            P.op("dve", lambda h: h.max(out=v8s[q], in_=gates[:, T, :]), reads=[("gates", T)], writes=[("v8", q)])
            P.op("dve", lambda h: h.max_index(out=i8s[q], in_max=v8s[q], in_values=gates[:, T, :]), reads=[("v8", q), ("gates", T)], writes=[("i8", q)])

        def slot_b(T):
            q = T % 2
            pos_sb, v8, i8 = pos_sbs[q], v8s[q], i8s[q]
            P.op("dve", lambda h: h.tensor_copy(out=i8f, in_=i8), reads=[("i8", q)], writes=["i8f"])
            tt("dve", oh3, iota64.unsqueeze(1).to_broadcast([128, 8, 64]), i8f.unsqueeze(2).to_broadcast([128, 8, 64]), ALU.is_equal,
               ["iota64", "i8f"], ["oh3"])
            tt("dve", oh3, oh3, pos_sb.unsqueeze(1).to_broadcast([128, 8, 64]), ALU.mult, ["oh3", ("pos_sb", q)], ["oh3"])
            P.op("dve", lambda h: h.tensor_reduce(out=pos8, in_=oh3, axis=AX.X, op=ALU.add), reads=["oh3"], writes=["pos8"])
            ts("dve", ov8, pos8, float(CAP), None, ALU.is_ge, None, ["pos8"], ["ov8"])
            P.op("dve", lambda h: h.tensor_reduce(out=dlt[:, 0:1], in_=ov8, axis=AX.X, op=ALU.max), reads=["ov8"], writes=["dlt"])
            tt("dve", ovf, ovf, dlt[:, 0:1], ALU.max, ["ovf", "dlt"], ["ovf"])
            stt(slotf, i8f, float(CAP), pos8, ALU.mult, ALU.add, ["i8f", "pos8"], ["slotf"])
            ts("dve", slotg, slotf, -1.0, float(NS), ALU.mult, ALU.add, ["slotf"], ["slotg"])
            tt("dve", slotg, slotg, ov8, ALU.mult, ["slotg", "ov8"], ["slotg"])
            tt("dve", slotg, slotg, slotf, ALU.add, ["slotg", "slotf"], ["slotg"])
            P.op("dve", lambda h: h.tensor_copy(out=slotgu[:, T, :], in_=slotg), reads=["slotg"], writes=[("slotgu", T)])
            tt("dve", dlt, dum8[:, T, :], slotf, ALU.subtract, ["dum8", "slotf", "slotg"], ["dlt"])
            tt("dve", dlt, dlt, ov8, ALU.mult, ["dlt", "ov8"], ["dlt"])
            tt("dve", slotf, slotf, dlt, ALU.add, ["slotf", "dlt"], ["slotf"])
            P.op("dve", lambda h: h.tensor_copy(out=slotu[:, T, :], in_=slotf), reads=["slotf"], writes=[("slotu", T)])
            P.op("dve", lambda h: h.tensor_copy(out=twv[:, T, :, 0], in_=tokf[:, T, :].to_broadcast([128, 8])), reads=["tokf"], writes=[("twv", T)])
            P.op("dve", lambda h: h.tensor_copy(out=twv[:, T, :, 1], in_=v8), reads=[("v8", q), ("twv", T)], writes=[("twv", T)])
            for k in range(8):
                P.idma(tbl_d, twv[:, T, k, :], slotu[:, T, k:k + 1], gather=False,
                       reads=[("twv", T), ("slotu", T), "tbl_init", "tbl_init2"], writes=[("tbl", T, k)])

        XS = 128.0 ** -0.5
        ptx_rr = [0]
        xsb_rr = [0]

        def xs_bank():
            i = 4 + xsb_rr[0] % 4
            xsb_rr[0] += 1
            return i

        def xa_emit_Q(blk, h_):
            cols = slice(blk * 512, (blk + 1) * 512)
            xtoks_b = [("xTo", blk * 4 + i) for i in range(4)]
            Qx = QxT[h_ % 2]
            qxt = "QxT%d" % (h_ % 2)
            b_ = xs_bank()
            for dt in range(8):
                mm(ps[b_], w_xq_s[:, dt, h_ * 128:(h_ + 1) * 128], xTo[:, dt, cols], dt == 0, dt == 7, ["w_xq"] + xtoks_b, ["ps%d" % b_])
            evac(Qx, ps[b_], ["ps%d" % b_], [qxt], eng="act")

        def xa_head(blk, h_):
            xo_tok = xo_toks[blk % 2]
            if h_ == 0:
                xa_emit_Q(blk, 0)
            Qx = QxT[h_ % 2]
            qxt = "QxT%d" % (h_ % 2)
            boA, boB = (0, 1) if h_ % 2 == 0 else (2, 3)
            sb = []
            for mt in range(2):
                bs_ = xs_bank()
                mm(ps[bs_], KxT[:, h_, mt * 128:(mt + 1) * 128], Qx, True, True, [("KxT", h_), qxt], ["ps%d" % bs_])
                sb.append(bs_)
            if h_ + 1 < 4:
                xa_emit_Q(blk, h_ + 1)
            for mt in range(2):
                bs_ = sb[mt]
                PT = PTx[ptx_rr[0] % 4]
                ptok = "PTx%d" % (ptx_rr[0] % 4)
                ptx_rr[0] += 1
                act(PT, ps[bs_], AF.Exp, ["ps%d" % bs_], [ptok], scale=XS)
                for qs in range(4):
                    bo_ = boA if qs < 2 else boB
                    c0 = (qs % 2) * 129
                    mm(ps[bo_][:, c0:c0 + 129], PT[:, qs * 128:(qs + 1) * 128], Vx[:, mt, h_, :], (mt == 0 and qs % 2 == 0), (mt == 1 and qs % 2 == 1),
                       [ptok, ("Vx", mt), "Vx1"], ["ps%d" % bo_])
            evac(Osx[:, 0:2, :], ps[boA][:, 0:258].rearrange("p (a b) -> p a b", a=2), ["ps%d" % boA], [("Osx", 0)], eng="act")
            evac(Osx[:, 2:4, :], ps[boB][:, 0:258].rearrange("p (a b) -> p a b", a=2), ["ps%d" % boB], [("Osx", 1)], eng="act")
            P.op("dve", lambda h: h.reciprocal(out=rinx, in_=Osx[:, :, 128]), reads=[("Osx", 0), ("Osx", 1)], writes=["rinx"])
            tt("dve", xo_tok[:, :, h_ * 128:(h_ + 1) * 128], Osx[:, :, 0:128], rinx.unsqueeze(2).to_broadcast([128, 4, 128]), ALU.mult,
               [("Osx", 0), ("Osx", 1), "rinx"], [("xo_tok", blk % 2, h_)])

        ln2_h = {}

        def xa_post1(t):
            if True:
                blk, qs = t // 4, t % 4
                xo_tok = xo_toks[blk % 2]
                tc_ = slice(t * 128, (t + 1) * 128)
                xb_hi, xb_lo = xb_his[t % 2], xb_los[t % 2]
                xbh, xbl = "xb_hi%d" % (t % 2), "xb_lo%d" % (t % 2)
                bt = nps()
                for kt in range(4):
                    tr(psb[bt][:, kt * 128:(kt + 1) * 128], xo_tok[:, qs, kt * 128:(kt + 1) * 128], identb, [("xo_tok", blk % 2, kt), "identb"], ["ps%d" % bt])
                evac(xoT, psb[bt][:, 0:512].rearrange("p (a b) -> p a b", a=4), ["ps%d" % bt], ["xoT"])
                x1t = x1r[t % 3]
                x1tk = "x1r%d" % (t % 3)
                P.dma("sp", x1t, x1_d[tc_, :], reads=[("x1d", t)], writes=[x1tk])
                xs = xs2[t % 3]
                xst = "xs2_%d" % (t % 3)
                for hf in range(2):
                    hc = slice(hf * 512, (hf + 1) * 512)
                    bo_ = nps()
                    for kt in range(4):
                        mm(ps[bo_], xoT[:, kt, :], w_xo_s[:, kt, hc], kt == 0, kt == 3, ["xoT", "w_xo"], ["ps%d" % bo_])
                    stt(xs[:, hc], x1t[:, hc], ALPHA, ps[bo_], ALU.mult, ALU.add, [x1tk, "ps%d" % bo_], [(xst, hf)])
                ln2_h[t] = layer_norm_a(xs, "ln2", [(xst, 0), (xst, 1)], lnt)

        def xa_post2a(t):
            if True:
                tc_ = slice(t * 128, (t + 1) * 128)
                xb_hi, xb_lo = xb_his[t % 2], xb_los[t % 2]
                xbh, xbl = "xb_hi%d" % (t % 2), "xb_lo%d" % (t % 2)
                xs = xs2[t % 3]
                xst = "xs2_%d" % (t % 3)
                xh = [(xst, 0), (xst, 1)]
                layer_norm_b(ln2_h[t], xs, xs, g2, b2, xh, xh, gb_eng="dve")
                if t == 0:
                    tap("x2_0", xs, [128, 1024], xh)
                P.dma(ST_Q, x2_d[tc_, :], xs, reads=xh, writes=[("x2d", t)])
                P.op("act", lambda h, xs=xs: h.copy(out=xb_hi, in_=xs), reads=xh, writes=[xbh])
                if sparse:
                    P.dma("sp", x2b_d[tc_, :], xb_hi, reads=[xbh], writes=[("x2bd", t)])
                tt("dve", xb_lo, xs, xb_hi, ALU.subtract, xh + [xbh], [xbl])
        def xa_post2b(t):
            if True:
                tc_ = slice(t * 128, (t + 1) * 128)
                xb_hi, xb_lo = xb_his[t % 2], xb_los[t % 2]
                xbh, xbl = "xb_hi%d" % (t % 2), "xb_lo%d" % (t % 2)
                bA, bB = nps(), nps()
                for dt in range(8):
                    tr(psb[bA][:, dt * 128:(dt + 1) * 128], xb_hi[:, dt * 128:(dt + 1) * 128], identb, [xbh, "identb"], ["ps%d" % bA])
                evac(xTo[:, :, tc_], psb[bA].rearrange("p (a b) -> p a b", a=8), ["ps%d" % bA], [("xTo", t)])
                for dt in range(8):
                    tr(psb[bB][:, dt * 128:(dt + 1) * 128], xb_lo[:, dt * 128:(dt + 1) * 128], identb, [xbl, "identb"], ["ps%d" % bB])
                evac(x2Tl, psb[bB].rearrange("p (a b) -> p a b", a=8), ["ps%d" % bB], ["x2Tl"])
                br = nps()
                n_ = 0
                for dt in range(8):
                    for (l_, ltoks, w_) in ((xTo[:, dt, tc_], [("xTo", t)], w_rh), (x2Tl[:, dt, :], ["x2Tl"], w_rh), (xTo[:, dt, tc_], [("xTo", t)], w_rl)):
                        mm(ps[br][:, 0:64], l_, w_[:, dt, :], n_ == 0, n_ == 23, ltoks + ["w_rhl"], ["ps%d" % br])
                        n_ += 1
                act(r_sc, ps[br][:, 0:64], AF.Exp, ["ps%d" % br], ["r_sc"], scale=-1.0)
                ts("dve", r_sc, r_sc, 1.0, None, ALU.add, None, ["r_sc"], ["r_sc"])
                P.op("dve", lambda h: h.reciprocal(out=r_sc, in_=r_sc), reads=["r_sc"], writes=["r_sc"])
                tt("dve", r_sel, r_sc, rbias, ALU.add, ["r_sc", "rbias"], ["r_sel"])
                selv = r_sel.rearrange("p (g e) -> p g e", e=8)
                P.op("dve", lambda h: h.tensor_reduce(out=r_m1, in_=selv, axis=AX.X, op=ALU.max), reads=["r_sel"], writes=["r_m1"])
                tt("dve", r_eq.rearrange("p (g e) -> p g e", e=8), selv, r_m1.unsqueeze(2).to_broadcast([128, 8, 8]), ALU.is_equal, ["r_sel", "r_m1"], ["r_eq"])
                stt(r_sel2, r_eq, -1e9, r_sel, ALU.mult, ALU.add, ["r_eq", "r_sel"], ["r_sel2"])
                P.op("dve", lambda h: h.tensor_reduce(out=r_m2, in_=r_sel2.rearrange("p (g e) -> p g e", e=8), axis=AX.X, op=ALU.max), reads=["r_sel2"], writes=["r_m2"])
                tt("dve", r_m1, r_m1, r_m2, ALU.add, ["r_m1", "r_m2"], ["r_gs"])
                P.op("dve", lambda h: h.max(out=r_t8, in_=r_m1), reads=["r_gs"], writes=["r_t8"])
                ts("dve", r_gm, r_m1, r_t8[:, 3:4], None, ALU.is_ge, None, ["r_gs", "r_t8"], ["r_gm"])
                ts("dve", r_pen, r_gm, 1e9, -1e9, ALU.mult, ALU.add, ["r_gm"], ["r_pen"])
                tt("dve", r_sel2.rearrange("p (g e) -> p g e", e=8), selv, r_gm.unsqueeze(2).to_broadcast([128, 8, 8]), ALU.mult, ["r_sel", "r_gm"], ["r_sel2"])
                tt("dve", r_sel2.rearrange("p (g e) -> p g e", e=8), r_sel2.rearrange("p (g e) -> p g e", e=8), r_pen.unsqueeze(2).to_broadcast([128, 8, 8]), ALU.add,
                   ["r_sel2", "r_pen"], ["r_sel2"])
                P.op("dve", lambda h: h.max(out=r_t8, in_=r_sel2), reads=["r_sel2"], writes=["r_t8b"])
                ts("dve", r_eq, r_sel2, r_t8[:, 7:8], None, ALU.is_ge, None, ["r_sel2", "r_t8b"], ["r_eq"])
                tt("dve", r_eq, r_eq, r_sc, ALU.mult, ["r_eq", "r_sc"], ["r_eq"])
                P.op("dve", lambda h: h.tensor_reduce(out=r_den, in_=r_eq, axis=AX.X, op=ALU.add), reads=["r_eq"], writes=["r_den"])
                P.op("dve", lambda h: h.reciprocal(out=r_den, in_=r_den), reads=["r_den"], writes=["r_den"])
                ts("dve", gates[:, t, :], r_eq, r_den[:, 0:1], 2.5, ALU.mult, ALU.mult, ["r_eq", "r_den"], [("gates", t)])
                if sparse:
                    slot_a(t)
                    if t >= 1:
                        slot_b(t - 1)
        for h_ in range(4):
            xa_head(0, h_)
        for blk in range(4):
            for qs in range(4):
                t = blk * 4 + qs
                if t >= 1:
                    xa_post2a(t - 1)
                if blk + 1 < 4:
                    xa_head(blk + 1, qs)
                xa_post1(t)
                if t >= 1:
                    xa_post2b(t - 1)
        xa_post2a(15)
        xa_post2b(15)
        tap("gates", gates, [128, 16, 64], [("gates", t) for t in range(16)])
        if sparse:
            slot_b(15)
            P.dma("sp", ovf_d, ovf, reads=["ovf"], writes=["ovf_d"])
            tap("slotu", slotu, [128, 16, 8], [("slotu", T) for T in range(16)], U32)
        P.barrier()
        A.release()
        if STAGE <= 4:
            P.dma("sp", out_d[0:128, 0:64], identf[:, 0:64], reads=["identf"], writes=["out"])
            P.barrier()
            P.emit()
            return nc, dbg


        A.mark()
        acc = A.alloc([16, 1024], F32)
        HT = A.alloc([2, 2048], BF16)
        NB = 3
        wgu = [A.alloc([8, 512], BF16) for _ in range(NB)]
        wdn = [A.alloc([2, 1024], BF16) for _ in range(NB)]
        sgb = [A.alloc([512], BF16) for _ in range(2)]
        g3, b3 = ln_bcast_load("ln3")
        NFB = 2 if sparse else 4
        x2r = [A.alloc([1024], F32) for _ in range(NFB)]
        xs3 = [A.alloc([1024], F32) for _ in range(NFB)]
        st6 = A.alloc([4, 12], F32)
        mv = A.alloc([4, 2], F32)
        rstd = A.alloc([4, 1], F32)
        lnt = (st6, mv, rstd)
        n_exp = N_EXPERTS_RUN if not sparse else 0
        sg_rr = 0
        U32 = mybir.dt.uint32
        if not sparse:
            ovf = A.alloc([1], F32)
            P.op("dve", lambda h: h.memset(ovf, 0.0), writes=["ovf"])
            P.dma("sp", ovf_d, ovf, reads=["ovf"], writes=["ovf_d"])

        ln3_h = {}

        def final_a(t):
            tc_ = slice(t * 128, (t + 1) * 128)
            xr = x2r[t % NFB]
            xrt = "x2r%d" % (t % NFB)
            P.dma("sp", xr, x2_d[tc_, :], writes=[xrt])
            xs = xs3[t % NFB]
            xst = "xs3_%d" % (t % NFB)
            stt(xs, xr, ALPHA, acc[:, t, :], ALU.mult, ALU.add, [xrt, ("acc", t, 0), ("acc", t, 1)], [xst])
            ln3_h[t] = layer_norm_a(xs, "ln3", [xst], lnt)

        def final_b(t):
            tc_ = slice(t * 128, (t + 1) * 128)
            xs = xs3[t % NFB]
            xst = "xs3_%d" % (t % NFB)
            layer_norm_b(ln3_h[t], xs, xs, g3, b3, [xst], [xst], gb_eng=("dve" if sparse else None))
            P.dma(ST_Q, out_d[tc_, :], xs, reads=[xst], writes=[("out", t)])

        for ei in range(-1, n_exp):
            bi = (ei + 1) % NB
            gu_t, dn_t = ("wgu", bi), ("wdn", bi)
            if ei < 0:
                P.dma("pool", wgu[bi], w_sh_gu.rearrange("(k p) c -> p k c", p=128), writes=[gu_t])
                P.dma("pool", wdn[bi], w_sh_down.rearrange("(k p) c -> p k c", p=128), writes=[dn_t])
            else:
                P.dma("pool", wgu[bi], w_exp_gu[ei].rearrange("(k p) c -> p k c", p=128), writes=[gu_t])
                P.dma("pool", wdn[bi], w_exp_down[ei].rearrange("(k p) c -> p k c", p=128), writes=[dn_t])
            for tb in range(4):
                cols = slice(tb * 512, (tb + 1) * 512)
                xt_ = [("xTo", tb * 4 + i) for i in range(4)]
                for ft in range(2):
                    bgk, buk = nps(), nps()
                    for (bk, c0) in ((bgk, ft * 128), (buk, 256 + ft * 128)):
                        for dt in range(8):
                            mm(ps[bk], wgu[bi][:, dt, c0:c0 + 128], xTo[:, dt, cols], dt == 0, dt == 7, [gu_t] + xt_, ["ps%d" % bk])
                    sg_ = sgb[sg_rr % 2]
                    sgt_ = "sgb%d" % (sg_rr % 2)
                    sg_rr += 1
                    act(sg_, ps[bgk], AF.Silu, ["ps%d" % bgk], [sgt_])
                    tt("dve", HT[:, ft, cols], ps[buk], sg_, ALU.mult, ["ps%d" % buk, sgt_], [("HT", tb, ft)])
            for t in range(16):
                tc_ = slice(t * 128, (t + 1) * 128)
                for hf in range(2):
                    hc = slice(hf * 512, (hf + 1) * 512)
                    bd = nps()
                    for ft in range(2):
                        mm(ps[bd], HT[:, ft, tc_], wdn[bi][:, ft, hc], ft == 0, ft == 1, [("HT", t // 4, 0), ("HT", t // 4, 1), dn_t], ["ps%d" % bd])
                    if ei < 0:
                        evac(acc[:, t, hc], ps[bd], ["ps%d" % bd], [("acc", t, hf)])
                    else:
                        stt(acc[:, t, hc], ps[bd], gates[:, t, ei:ei + 1], acc[:, t, hc], ALU.mult, ALU.add,
                            ["ps%d" % bd, ("acc", t, hf)], [("acc", t, hf)])
                if ei == n_exp - 1 and not sparse:
                    if t == 0:
                        tap("acc0", acc[:, 0, :], [128, 1024], [("acc", 0, 0), ("acc", 0, 1)])
                    final_a(t)
                    if t >= 1:
                        final_b(t - 1)
            if ei == n_exp - 1 and not sparse:
                final_b(15)
        if sparse:
            P.barrier()
            xflat = xTo.rearrange("p a b -> p (a b)")
            Xg = [xflat[:, i * 4096:(i + 1) * 4096].rearrange("p (j d) -> p j d", j=4) for i in range(3)]
            XgT = xflat[:, 12288:16384].rearrange("p (a b) -> p a b", a=8)
            HTs = [A.alloc([2, 512], BF16) for _ in range(2)]
            Yb = [A.alloc([4, 1024], BF16) for _ in range(2)]
            twe = [A.alloc([4, 2], F32) for _ in range(4)]
            idxe = [A.alloc([4], U32) for _ in range(4)]
            ttoks = [("tbl", T, k) for T in range(16) for k in range(8)]
            x2btoks = [("x2bd", t) for t in range(16)] + ["x2b_zero"]

            def exp_tbl(e):
                b4 = e % 4
                P.dma("sp", twe[b4], tbl_d[e * CAP:(e + 1) * CAP, :].rearrange("(j p) c -> p j c", p=128), reads=ttoks + ["tbl_init"], writes=[("twe", b4)])

            def exp_gather(e):
                b3 = e % 3
                b4 = e % 4
                P.op("pool", lambda h: h.tensor_copy(out=idxe[b4], in_=twe[b4][:, :, 0]), reads=[("twe", b4)], writes=[("idxe", b4)])
                for j in range(4):
                    P.idma(Xg[b3][:, j, :], x2b_d, idxe[b4][:, j:j + 1], gather=True, reads=[("idxe", b4)] + x2btoks, writes=[("Xg", b3, j)])
                bi = (e + 1) % NB
                P.dma("pool", wgu[bi], w_exp_gu[e].rearrange("(k p) c -> p k c", p=128), writes=[("wgu", bi)])
                P.dma("pool", wdn[bi], w_exp_down[e].rearrange("(k p) c -> p k c", p=128), writes=[("wdn", bi)])

            def exp_compute(e):
                b3 = e % 3
                b4 = e % 4
                b2 = e % 2
                bi = (e + 1) % NB
                gu_t, dn_t = ("wgu", bi), ("wdn", bi)
                for j in range(4):
                    bt = nps()
                    for dt in range(8):
                        tr(psb[bt][:, dt * 128:(dt + 1) * 128], Xg[b3][:, j, dt * 128:(dt + 1) * 128], identb, [("Xg", b3, j), "identb"], ["ps%d" % bt])
                    evac(XgT[:, :, j * 128:(j + 1) * 128], psb[bt].rearrange("p (a b) -> p a b", a=8), ["ps%d" % bt], [("XgT", j)])
                xg_t = [("XgT", j) for j in range(4)]
                for ft in range(2):
                    bgk, buk = nps(), nps()
                    for (bk, c0) in ((bgk, ft * 128), (buk, 256 + ft * 128)):
                        for dt in range(8):
                            mm(ps[bk], wgu[bi][:, dt, c0:c0 + 128], XgT[:, dt, :], dt == 0, dt == 7, [gu_t] + xg_t, ["ps%d" % bk])
                    sg_ = sgb[ft]
                    act(sg_, ps[bgk], AF.Silu, ["ps%d" % bgk], [("sgb", ft)])
                    tt("dve", HTs[b2][:, ft, :], ps[buk], sg_, ALU.mult, ["ps%d" % buk, ("sgb", ft)], [("HTs", b2, ft)])
                for j in range(4):
                    for hf in range(2):
                        hc = slice(hf * 512, (hf + 1) * 512)
                        bd = nps()
                        for ft in range(2):
                            mm(ps[bd], HTs[b2][:, ft, j * 128:(j + 1) * 128], wdn[bi][:, ft, hc], ft == 0, ft == 1, [("HTs", b2, 0), ("HTs", b2, 1), dn_t], ["ps%d" % bd])
                        if (j * 2 + hf) % 2 == 0:
                            ts("dve", Yb[b2][:, j, hc], ps[bd], twe[b4][:, j, 1:2], None, ALU.mult, None, ["ps%d" % bd, ("twe", b4)], [("Yb", b2, j, hf)])
                        else:
                            act(Yb[b2][:, j, hc], ps[bd], AF.Copy, ["ps%d" % bd, ("twe", b4)], [("Yb", b2, j, hf)], scale=twe[b4][:, j, 1:2])
                P.dma("pool", y_d[e * CAP:(e + 1) * CAP, :].rearrange("(j p) d -> p j d", p=128), Yb[b2],
                      reads=[("Yb", b2, j, hf) for j in range(4) for hf in range(2)], writes=[("y_d", e)])

            for e0 in range(3):
                exp_tbl(e0)
            exp_gather(0)
            exp_gather(1)
            for e in range(64):
                if e + 3 < 64:
                    exp_tbl(e + 3)
                if e + 2 < 64:
                    exp_gather(e + 2)
                exp_compute(e)
            P.barrier()
            Yk = [xflat[:, i * 1024:(i + 1) * 1024] for i in range(16)]
            ytoks = [("y_d", e) for e in range(64)]
            def comb_gather(t):
                for k in range(8):
                    i_ = (t % 2) * 8 + k
                    P.idma(Yk[i_], y_d, slotgu[:, t, k:k + 1], gather=True, reads=ytoks, writes=["Yk%d" % i_])

            comb_gather(0)
            for t in range(16):
                if t + 1 < 16:
                    comb_gather(t + 1)
                bA, bB = nps(), nps()
                for k in range(8):
                    i_ = (t % 2) * 8 + k
                    for hf, bb_ in ((0, bA), (1, bB)):
                        mm(ps[bb_], identb, Yk[i_][:, hf * 512:(hf + 1) * 512], k == 0, k == 7, ["Yk%d" % i_, "identb"], ["ps%d" % bb_])
                for hf, bb_ in ((0, bA), (1, bB)):
                    hc = slice(hf * 512, (hf + 1) * 512)
                    tt("dve", acc[:, t, hc], acc[:, t, hc], ps[bb_], ALU.add, ["ps%d" % bb_, ("acc", t, hf)], [("acc", t, hf)])
                if t == 0:
                    tap("acc0", acc[:, 0, :], [128, 1024], [("acc", 0, 0), ("acc", 0, 1)])
                final_a(t)
                if t >= 1:
                    final_b(t - 1)
            final_b(15)
        P.barrier()
        A.release()
        P.emit()
        return nc, dbg

        raise NotImplementedError
    return nc, dbg


def _consts(r):
    kk = np.arange(128)[:, None]
    qq = np.arange(128)[None, :]
    tri = (kk <= qq).astype(np.float32)
    if r == 0:
        masks = np.concatenate([tri, np.zeros((128, 128), np.float32)], axis=1)
    else:
        masks = np.concatenate([np.ones((128, 128), np.float32), tri], axis=1)
    rflag = np.zeros((128, 2), np.float32)
    rflag[:, 0] = 1.0 - r
    rflag[:, 1] = float(r)
    ident = np.eye(128, dtype=np.float32)
    gi = np.arange(128) // 16
    blockmask = (gi[:, None] == gi[None, :]).astype(np.float32)
    parity = np.stack([(gi % 2 == 0), (gi % 2 == 1)], axis=1).astype(np.float32)
    inv_freq = (10000.0 ** (-np.arange(0, 32, 2, dtype=np.float32) / 32.0)).astype(np.float32)
    invf = np.zeros((128, 1), np.float32)
    for p in range(64, 96):
        invf[p, 0] = inv_freq[(p - 64) % 16]
    tp = np.arange(128)
    triu = (tp[:, None] < tp[None, :]).astype(np.float32)
    iota64 = np.broadcast_to(np.arange(64, dtype=np.float32)[None, :], (128, 64)).copy()
    dum8 = np.zeros((128, 16, 8), np.float32)
    for T in range(16):
        for k in range(8):
            dum8[:, T, k] = NS + (T * 128 + tp) * 8 + k
    return dict(masks=masks, rflag=rflag, ident=ident, blockmask=blockmask, parity=parity, invf=invf,
                triu=triu, iota64=iota64, dum8=dum8.reshape(128, 128))


def make_in_maps(inp):
    x = np.asarray(inp["x"])
    mem = np.asarray(inp["mem"])
    pos = np.asarray(inp["positions"]).astype(np.int32)
    shared = {}
    for k, v in inp.items():
        if k in ("x", "mem", "positions"):
            continue
        v = np.asarray(v)
        shared[k] = np.ascontiguousarray(v[0])
    maps = []
    for c in range(8):
        b, r = c // 2, c % 2
        m = dict(shared)
        m["x_full"] = np.ascontiguousarray(x[b])
        m["x_own"] = np.ascontiguousarray(x[b].reshape(32, 128, 1024)[r::2].reshape(2048, 1024))
        m["mem"] = np.ascontiguousarray(mem[b])
        m["pos_full"] = np.ascontiguousarray(pos[b])
        m["pos_own"] = np.ascontiguousarray(pos[b].reshape(32, 128)[r::2].reshape(2048))
        m.update(_consts(r))
        maps.append(m)
    return maps


def kernel(**inputs):
    maps = make_in_maps(inputs)
    nc, _ = build(sparse=True)
    res = run_bass_kernel_spmd(nc, maps, core_ids=list(range(8)))
    if any(float(np.asarray(res.results[c]["ovf"]).max()) > 0.0 for c in range(8)):
        nc, _ = build(sparse=False)
        res = run_bass_kernel_spmd(nc, maps, core_ids=list(range(8)))
    out = np.zeros((4, 4096, 1024), np.float32)
    for c in range(8):
        b, r = c // 2, c % 2
        o = np.asarray(res.results[c]["out"]).reshape(16, 128, 1024)
        out[b].reshape(32, 128, 1024)[r::2] = o
    return out
```
